# Optimizing a Trainium2 kernel written in Bass

```python
import math
import jax, jax.numpy as jnp
from jax import lax
import numpy as np

D_MODEL = 2048
BATCH = 16
SEQ = 2048
DEPTH = 4

BLOCK = 128
ROPE_THETA = 10000.0
NORM_EPS = 1e-6

DA_HEADS = 4
DA_HEAD_DIM = 128
DA_QK_DIM = 64
DL_HEADS = 6
DL_HEAD_DIM = 128
DL_PATTERNS = ((128, 1), (512, 4), (2048, 16))
RW_HEADS = 12
RW_HEAD_DIM = 64
RW_DECAY_RANK = 64
RW_A_RANK = 64
RW_GATE_RANK = 128
RW_LN_EPS = 64e-5

DA_W = DA_HEADS * DA_HEAD_DIM
DL_W = DL_HEADS * DL_HEAD_DIM
RW_W = RW_HEADS * RW_HEAD_DIM
D_MIX = DA_W + DL_W + RW_W
ATT_SIZES = (DA_W, DA_W, DA_W, DL_W, DL_W, DL_W)
RW_SIZES = (RW_W, RW_W, RW_W, RW_DECAY_RANK, RW_A_RANK, RW_GATE_RANK)
ATT_IN = sum(ATT_SIZES)
RW_IN = sum(RW_SIZES)
D_IN = ATT_IN + RW_IN

D_FF = 5632
N_EXPERTS = 8
TOP_K = 2
D_FF_EXPERT = 5632
N_DENSE = (DEPTH + 1) // 2
N_MOE = DEPTH // 2

kernel_name = "hybrid_diffattn_rwkv7_dilated_moe"


def _rms_norm(x, g, eps=NORM_EPS):
    xf = x.astype(jnp.float32)
    y = xf * lax.rsqrt(jnp.mean(xf * xf, axis=-1, keepdims=True) + eps)
    return (y * g.astype(jnp.float32)).astype(x.dtype)


def _split_cols(p, sizes):
    out, start = [], 0
    for s in sizes:
        out.append(p[..., start:start + s])
        start += s
    return out


def _rope(x, pos):
    d = x.shape[-1]
    half = d // 2
    inv = ROPE_THETA ** (-jnp.arange(half, dtype=jnp.float32) / half)
    ang = pos.astype(jnp.float32)[:, None] * inv[None, :]
    bshape = (1, pos.shape[0]) + (1,) * (x.ndim - 3) + (half,)
    cos, sin = jnp.cos(ang).reshape(bshape), jnp.sin(ang).reshape(bshape)
    xf = x.astype(jnp.float32)
    x1, x2 = xf[..., :half], xf[..., half:]
    return jnp.concatenate([x1 * cos - x2 * sin, x2 * cos + x1 * sin], axis=-1).astype(x.dtype)


def _diff_attention(q, k, v, lam):
    b, s, h, _, dk = q.shape
    nb = s // BLOCK
    scale = dk ** -0.5
    q_blocks = jnp.moveaxis(q.reshape(b, nb, BLOCK, h, 2, dk), 1, 0)
    k_pos = jnp.arange(s)

    def attend_block(args):
        q_blk, blk = args
        sc = jnp.einsum("bqhcd,bkhcd->bhcqk", q_blk, k).astype(jnp.float32) * scale
        q_pos = blk * BLOCK + jnp.arange(BLOCK)
        causal = k_pos[None, :] <= q_pos[:, None]
        pr = jax.nn.softmax(jnp.where(causal, sc, -jnp.inf), axis=-1)
        diff = pr[:, :, 0] - lam * pr[:, :, 1]
        return jnp.einsum("bhqk,bkhd->bqhd", diff.astype(v.dtype), v)

    o = lax.map(attend_block, (q_blocks, jnp.arange(nb)))
    return jnp.moveaxis(o, 0, 1).reshape(b, s, h, v.shape[-1])


def _dilated_branch(q, k, v, window, dilation):
    b, s, h, d = q.shape
    steps = window // dilation
    length = s // dilation
    nb = -(-length // BLOCK)
    lp = nb * BLOCK

    def to_blocks(t):
        t = jnp.swapaxes(t.reshape(b, length, dilation, h, d), 1, 2)
        t = jnp.pad(t, ((0, 0), (0, 0), (0, lp - length), (0, 0), (0, 0)))
        return t.reshape(b, dilation, nb, BLOCK, h, d)

    def with_prev(t):
        prev = jnp.pad(t, ((0, 0), (0, 0), (1, 0), (0, 0), (0, 0), (0, 0)))[:, :, :-1]
        return jnp.concatenate([prev, t], axis=3)

    qb = to_blocks(q)
    kb, vb = with_prev(to_blocks(k)), with_prev(to_blocks(v))
    sc = jnp.einsum("brnqhd,brnkhd->brnhqk", qb, kb).astype(jnp.float32) * (d ** -0.5)
    qi = jnp.arange(BLOCK)[:, None]
    kj = jnp.arange(2 * BLOCK)[None, :]
    offset = qi - kj + BLOCK
    band = (offset >= 0) & (offset <= steps)
    key_idx = jnp.arange(nb)[:, None] * BLOCK - BLOCK + kj
    valid = band[None] & (key_idx >= 0)[:, None, :]
    sc = jnp.where(valid[None, None, :, None], sc, -jnp.inf)
    lse = jax.nn.logsumexp(sc, axis=-1)
    pr = jnp.exp(sc - lse[..., None])
    o = jnp.einsum("brnhqk,brnkhd->brnqhd", pr.astype(v.dtype), vb)
    lse = jnp.swapaxes(lse, 3, 4)

    def from_blocks(t):
        t = t.reshape((b, dilation, lp) + t.shape[4:])[:, :, :length]
        return jnp.swapaxes(t, 1, 2).reshape((b, s) + t.shape[3:])

    return from_blocks(o), from_blocks(lse)


def _dilated_attention(q, k, v):
    outs, lses = [], []
    for window, dilation in DL_PATTERNS:
        o, lse = _dilated_branch(q, k, v, window, dilation)
        outs.append(o.astype(jnp.float32))
        lses.append(lse)
    wts = jax.nn.softmax(jnp.stack(lses, axis=-1), axis=-1)
    return jnp.einsum("bshp,pbshd->bshd", wts, jnp.stack(outs, axis=0))


def _rwkv7_scan(r, w, k, v, a, b):
    bsz, _, h, n = r.shape

    def step(state, inp):
        r_t, w_t, k_t, v_t, a_t, b_t = inp
        sa = jnp.einsum("bhvk,bhk->bhv", state, a_t)
        state = (state * w_t[:, :, None, :] + sa[..., None] * b_t[:, :, None, :]
                 + v_t[..., None] * k_t[:, :, None, :])
        return state, jnp.einsum("bhvk,bhk->bhv", state, r_t)

    xs = tuple(jnp.moveaxis(t.astype(jnp.float32), 1, 0) for t in (r, w, k, v, a, b))
    init = jnp.zeros((bsz, h, n, n), jnp.float32)
    _, y = lax.scan(step, init, xs)
    return jnp.moveaxis(y, 0, 1)


def _swiglu(h, w_gate, w_up, w_down):
    return (jax.nn.silu(h @ w_gate) * (h @ w_up)) @ w_down


def _moe(h, router, w_gate, w_up, w_down):
    b, s, d = h.shape
    t = h.reshape(b * s, d)
    logits = (t @ router).astype(jnp.float32)
    top_v, top_i = lax.top_k(logits, TOP_K)
    top_w = jax.nn.softmax(top_v, axis=-1)
    combine = jnp.einsum("tk,tke->te", top_w, jax.nn.one_hot(top_i, N_EXPERTS, dtype=jnp.float32))
    y = jnp.zeros((b * s, d), jnp.float32)
    for e in range(N_EXPERTS):
        y = y + combine[:, e:e + 1] * _swiglu(t, w_gate[e], w_up[e], w_down[e]).astype(jnp.float32)
    return y.reshape(b, s, d).astype(h.dtype)


def setup_inputs(seed: int = 0) -> dict:
    key = jax.random.key(seed)
    ks = iter(jax.random.split(key, 40))
    f32 = jnp.float32

    def nrm(shape, scale):
        return jax.random.normal(next(ks), shape, f32) * scale

    def gain(shape):
        return 1.0 + 0.02 * jax.random.normal(next(ks), shape, f32)

    out_scale = (2 * DEPTH) ** -0.5
    return {
        "x": nrm((BATCH, SEQ, D_MODEL), 1.0),
        "norm1_g": gain((DEPTH, D_MODEL)),
        "w_in": nrm((DEPTH, D_MODEL, D_IN), D_MODEL ** -0.5),
        "da_q_norm": gain((DEPTH, DA_QK_DIM)),
        "da_k_norm": gain((DEPTH, DA_QK_DIM)),
        "da_lambda": nrm((DEPTH, 4, DA_QK_DIM), 0.1),
        "da_out_norm": gain((DEPTH, DA_HEAD_DIM)),
        "dl_q_norm": gain((DEPTH, DL_HEAD_DIM)),
        "dl_k_norm": gain((DEPTH, DL_HEAD_DIM)),
        "rw_mu": jax.random.uniform(next(ks), (DEPTH, RW_IN), f32),
        "rw_w0": jax.random.uniform(next(ks), (DEPTH, RW_W), f32, -6.0, -1.0),
        "rw_w2": nrm((DEPTH, RW_DECAY_RANK, RW_W), 0.1),
        "rw_a0": nrm((DEPTH, RW_W), 0.1),
        "rw_a2": nrm((DEPTH, RW_A_RANK, RW_W), 0.1),
        "rw_g2": nrm((DEPTH, RW_GATE_RANK, RW_W), RW_GATE_RANK ** -0.5),
        "rw_k_k": 0.85 + 0.02 * jax.random.normal(next(ks), (DEPTH, RW_W), f32),
        "rw_k_a": gain((DEPTH, RW_W)),
        "rw_r_k": nrm((DEPTH, RW_HEADS, RW_HEAD_DIM), 0.1),
        "rw_ln_g": gain((DEPTH, RW_W)),
        "rw_ln_b": nrm((DEPTH, RW_W), 0.02),
        "w_out": nrm((DEPTH, D_MIX, D_MODEL), D_MIX ** -0.5 * out_scale),
        "norm2_g": gain((DEPTH, D_MODEL)),
        "ffn_w_gate": nrm((N_DENSE, D_MODEL, D_FF), D_MODEL ** -0.5),
        "ffn_w_up": nrm((N_DENSE, D_MODEL, D_FF), D_MODEL ** -0.5),
        "ffn_w_down": nrm((N_DENSE, D_FF, D_MODEL), D_FF ** -0.5 * out_scale),
        "moe_router": nrm((N_MOE, D_MODEL, N_EXPERTS), D_MODEL ** -0.5),
        "moe_w_gate": nrm((N_MOE, N_EXPERTS, D_MODEL, D_FF_EXPERT), D_MODEL ** -0.5),
        "moe_w_up": nrm((N_MOE, N_EXPERTS, D_MODEL, D_FF_EXPERT), D_MODEL ** -0.5),
        "moe_w_down": nrm((N_MOE, N_EXPERTS, D_FF_EXPERT, D_MODEL), D_FF_EXPERT ** -0.5 * out_scale),
    }


def reference(x, norm1_g, w_in, da_q_norm, da_k_norm, da_lambda, da_out_norm,
              dl_q_norm, dl_k_norm, rw_mu, rw_w0, rw_w2, rw_a0, rw_a2, rw_g2,
              rw_k_k, rw_k_a, rw_r_k, rw_ln_g, rw_ln_b, w_out, norm2_g,
              ffn_w_gate, ffn_w_up, ffn_w_down, moe_router, moe_w_gate, moe_w_up,
              moe_w_down):
    b, s, _ = x.shape
    f32 = jnp.float32
    pos = jnp.arange(s)
    for l in range(DEPTH):
        h = _rms_norm(x, norm1_g[l])
        p = jnp.einsum("bsd,dc->bsc", h, w_in[l])
        da_q, da_k, da_v, dl_q, dl_k, dl_v = _split_cols(p[..., :ATT_IN], ATT_SIZES)

        qa = _rope(_rms_norm(da_q.reshape(b, s, DA_HEADS, 2, DA_QK_DIM), da_q_norm[l]), pos)
        ka = _rope(_rms_norm(da_k.reshape(b, s, DA_HEADS, 2, DA_QK_DIM), da_k_norm[l]), pos)
        va = da_v.reshape(b, s, DA_HEADS, DA_HEAD_DIM)
        lam_init = 0.8 - 0.6 * math.exp(-0.3 * l)
        lq1, lk1, lq2, lk2 = da_lambda[l].astype(f32)
        lam = jnp.exp(jnp.sum(lq1 * lk1)) - jnp.exp(jnp.sum(lq2 * lk2)) + lam_init
        oa = _diff_attention(qa, ka, va, lam)
        oa = _rms_norm(oa, da_out_norm[l]).astype(f32) * (1.0 - lam_init)

        qc = _rope(_rms_norm(dl_q.reshape(b, s, DL_HEADS, DL_HEAD_DIM), dl_q_norm[l]), pos)
        kc = _rope(_rms_norm(dl_k.reshape(b, s, DL_HEADS, DL_HEAD_DIM), dl_k_norm[l]), pos)
        vc = dl_v.reshape(b, s, DL_HEADS, DL_HEAD_DIM)
        oc = _dilated_attention(qc, kc, vc)

        rw_p = p[..., ATT_IN:]
        prev = jnp.pad(rw_p, ((0, 0), (1, 0), (0, 0)))[:, :-1]
        rw_p = rw_p + rw_mu[l] * (prev - rw_p)
        rr, kr, vr, wl, al, gl = _split_cols(rw_p, RW_SIZES)
        w_log = -jax.nn.softplus(-(rw_w0[l] + jnp.tanh(wl) @ rw_w2[l])) - 0.5
        decay = jnp.exp(-jnp.exp(w_log.astype(f32)))
        a = jax.nn.sigmoid(rw_a0[l] + al @ rw_a2[l])
        g = jax.nn.sigmoid(gl) @ rw_g2[l]

        def heads(t):
            return t.reshape(b, s, RW_HEADS, RW_HEAD_DIM).astype(f32)

        kk = heads(kr * rw_k_k[l])
        kk = kk / jnp.maximum(jnp.sqrt(jnp.sum(kk * kk, axis=-1, keepdims=True)), 1e-12)
        kr = kr * (1.0 + (a - 1.0) * rw_k_a[l])
        r_h, k_h, v_h, a_h = heads(rr), heads(kr), heads(vr), heads(a)
        y = _rwkv7_scan(r_h, heads(decay), k_h, v_h, -kk, kk * a_h)
        mu = jnp.mean(y, axis=-1, keepdims=True)
        var = jnp.mean(jnp.square(y - mu), axis=-1, keepdims=True)
        y = ((y - mu) * lax.rsqrt(var + RW_LN_EPS) * rw_ln_g[l].reshape(RW_HEADS, RW_HEAD_DIM)
             + rw_ln_b[l].reshape(RW_HEADS, RW_HEAD_DIM))
        y = y + jnp.sum(r_h * k_h * rw_r_k[l], axis=-1, keepdims=True) * v_h
        ob = y.reshape(b, s, RW_W) * g.astype(f32)

        mix = jnp.concatenate([oa.reshape(b, s, DA_W), oc.reshape(b, s, DL_W), ob],
                              axis=-1).astype(x.dtype)
        x = x + jnp.einsum("bsc,cd->bsd", mix, w_out[l])

        h2 = _rms_norm(x, norm2_g[l])
        if l % 2 == 0:
            i = l // 2
            x = x + _swiglu(h2, ffn_w_gate[i], ffn_w_up[i], ffn_w_down[i])
        else:
            i = l // 2
            x = x + _moe(h2, moe_router[i], moe_w_gate[i], moe_w_up[i], moe_w_down[i])
    return x
```

```python
import math
import numpy as np
import ml_dtypes
import concourse.bass as bass
import concourse.mybir as mybir
from concourse.bass_utils import run_bass_kernel_spmd

F32 = mybir.dt.float32
BF16 = mybir.dt.bfloat16
AF = mybir.ActivationFunctionType
ALU = mybir.AluOpType
AX = mybir.AxisListType

D = 2048
S = 2048
NB = 2
T = NB * S
NT = T // 128
DEPTH = 4
D_IN = 6400
ATT_IN = 3840
RW_IN = 2560
D_FF = 5632
NE = 8
EPS = 1e-6
NCORES = 8
NBLK_MAX = (2 * T) // 512 + NE


class TT:
    __slots__ = ("w", "r", "name", "excl")

    def __init__(self, name="", excl=False):
        self.w = {}
        self.r = {}
        self.name = name
        self.excl = excl


def PT():
    return TT(excl=True)


class Sched:
    def __init__(self, nc, stack, ndma_sp=40, ndma_pool=24, ndma_act=8):
        self.nc = nc
        self.E = {}
        for name, obj in (("pe", nc.tensor), ("act", nc.scalar), ("dve", nc.vector),
                          ("pool", nc.gpsimd), ("sp", nc.sync)):
            sem = stack.enter_context(nc.semaphore("e_" + name))
            self.E[name] = {"obj": obj, "sem": sem, "count": 0, "waited": {}, "dma_i": 0, "dma_sems": []}
        for name, n in (("sp", ndma_sp), ("pool", ndma_pool), ("act", ndma_act)):
            self.E[name]["dma_sems"] = [stack.enter_context(nc.semaphore("d_%s%d" % (name, i))) for i in range(n)]

    def _wait(self, e, tok):
        key, sem, val, _ = tok
        if e["waited"].get(key, 0) >= val:
            return
        e["obj"].wait_ge(sem, val)
        e["waited"][key] = val

    def _deps(self, eng, outs, ins, same_engine, accs=()):
        e = self.E[eng]
        toks = []
        for t in ins:
            toks.extend(t.w.values())
            if t.excl:
                toks.extend(t.r.values())
        for t in outs:
            toks.extend(t.w.values())
            toks.extend(t.r.values())
        for t in accs:
            toks.extend(t.r.values())
        for tok in toks:
            if tok[3] == eng and not same_engine:
                continue
            self._wait(e, tok)

    def op(self, eng, fn, outs=(), ins=(), same_engine=True, mark=True):
        e = self.E[eng]
        if eng == "pe":
            same_engine = False
        self._deps(eng, outs, ins, same_engine)
        ins_ = fn()
        if mark:
            e["count"] += 1
            ins_.then_inc(e["sem"], 1)
            val = e["count"]
        else:
            val = e["count"] + 1
        tok = ("E" + eng, e["sem"], val, eng)
        for t in outs:
            t.w = {tok[0]: tok}
            t.r = {}
        for t in ins:
            t.r[tok[0]] = tok
        return ins_

    def dma(self, q, out, in_, outs=(), ins=(), accs=(), **kw):
        e = self.E[q]
        self._deps(q, outs, ins, True, accs)
        n = len(e["dma_sems"])
        slot, gen = e["dma_i"] % n, e["dma_i"] // n
        e["dma_i"] += 1
        sem = e["dma_sems"][slot]
        key = "D%s%d" % (q, slot)
        if gen > 0:
            self._wait(e, (key, sem, 16 * gen, "dma"))
        e["obj"].dma_start(out=out, in_=in_, **kw).then_inc(sem, 16)
        tok = (key, sem, 16 * (gen + 1), "dma")
        for t in outs:
            t.w = {key: tok}
            t.r = {}
        for t in accs:
            t.w[key] = tok
            t.r = {}
        for t in ins:
            t.r[key] = tok
        return tok

    def idma(self, out, in_, outs=(), ins=(), accs=(), **kw):
        q = "pool"
        e = self.E[q]
        self._deps(q, outs, ins, True, accs)
        n = len(e["dma_sems"])
        slot, gen = e["dma_i"] % n, e["dma_i"] // n
        e["dma_i"] += 1
        sem = e["dma_sems"][slot]
        key = "D%s%d" % (q, slot)
        if gen > 0:
            self._wait(e, (key, sem, 16 * gen, "dma"))
        e["obj"].indirect_dma_start(out=out, in_=in_, **kw).then_inc(sem, 16)
        tok = (key, sem, 16 * (gen + 1), "dma")
        for t in outs:
            t.w = {key: tok}
            t.r = {}
        for t in accs:
            t.w[key] = tok
            t.r = {}
        for t in ins:
            t.r[key] = tok
        return tok

    def wait_all(self, eng, tts):
        e = self.E[eng]
        for t in tts:
            for tok in list(t.w.values()) + list(t.r.values()):
                self._wait(e, tok)


from contextlib import ExitStack


BF16_INPUTS = ("ident_b", "maskA", "maskB", "triU", "ones_b", "maskU2", "maskL")


def host_constants(Tn=T):
    bf = ml_dtypes.bfloat16
    C = {"ident_f": np.eye(128, dtype=np.float32), "ident_b": np.eye(128, dtype=np.float32).astype(bf)}
    pos = np.arange(S, dtype=np.float64)[:, None]
    tabs = []
    for half in (32, 64):
        inv = 10000.0 ** (-np.arange(half, dtype=np.float64) / half)
        ang = (pos.astype(np.float32) * inv.astype(np.float32)[None, :]).astype(np.float32)
        tabs += [np.cos(ang), np.sin(ang)]
    C["rope"] = np.concatenate(tabs, axis=1).astype(np.float32)
    ki = np.arange(128)[:, None]
    qi = np.arange(128)[None, :]
    mA = np.zeros((128, 7, 128), np.float32)
    mB = np.zeros((128, 19, 128), np.float32)
    for j in range(19):
        delta = (j - 3) * 128 + qi - ki
        cnt = (delta >= 0) * ((delta <= 128).astype(np.int64) + ((delta % 4 == 0) & (delta <= 512)) + ((delta % 16 == 0) & (delta <= 2048)))
        mB[:, j, :] = cnt
        if j < 7:
            mA[:, j, :] = (delta >= 0)
    C["maskA"] = mA.astype(bf)
    C["maskB"] = mB.astype(bf)
    C["triU"] = (ki <= qi).astype(np.float32).astype(bf)
    C["ones_b"] = np.ones((128, 128), np.float32).astype(bf)
    C["maskU2"] = np.concatenate([(ki < qi), (ki <= qi)], axis=1).astype(np.float32).astype(bf)
    C["maskL"] = (ki > qi).astype(np.float32).astype(bf)
    pcol = np.arange(128, dtype=np.float32)[:, None]
    C["iota8"] = np.tile(np.arange(8, dtype=np.float32)[None, :], (128, 1))
    C["jj"] = np.tile(np.arange(NBLK_MAX, dtype=np.float32)[None, :], (128, 1))
    C["constAD"] = np.concatenate([np.arange(11, dtype=np.float32)[None, :] * 128 + pcol,
                                   np.arange(16, dtype=np.float32)[None, :] * 128 + pcol], axis=1).astype(np.float32)
    C["tokid"] = (np.arange(NT, dtype=np.float32)[None, :] * 128 + pcol).astype(np.float32)
    si = np.zeros((NBLK_MAX * 512 + 128, 2), np.float32)
    si[:, 0] = Tn + (np.arange(si.shape[0]) % 128)
    C["slot_init"] = si
    C["cb4"] = np.tile(np.arange(4, dtype=np.float32)[None, :], (128, 4))
    return C


class Builder:
    def __init__(self, cfg):
        self.cfg = cfg
        self.nc = bass.Bass("TRN2", target_bir_lowering=False)
        self.uid = 0
        self.NB = cfg.get("NB", NB)
        self.T = self.NB * S
        self.NT = self.T // 128

    def sb(self, st, shape, dt, name=None):
        self.uid += 1
        return st.enter_context(self.nc.sbuf_tensor("%s_%d" % (name or "sb", self.uid), list(shape), dt))

    def ps(self, st, shape, dt, name=None):
        self.uid += 1
        return st.enter_context(self.nc.psum_tensor("%s_%d" % (name or "ps", self.uid), list(shape), dt))

    def din(self, name, shape, dt=F32):
        return self.nc.dram_tensor(name, list(shape), dt, kind="ExternalInput").ap()

    def dout(self, name, shape, dt=F32):
        return self.nc.dram_tensor(name, list(shape), dt, kind="ExternalOutput").ap()

    def dscr(self, name, shape, dt=F32):
        return self.nc.dram_tensor(name, list(shape), dt, kind="Internal").ap()

    def load_fm_vec(self, sc, st, dst, dst_t, vec_ap, n, ident, ident_t, pst, pst_t):
        nc = self.nc
        tmp = self.sb(st, [n, 128], F32, "fmtmp"); tmp_t = TT()
        sc.dma("sp", tmp[:], vec_ap.rearrange("(c p) -> c p", p=128), outs=[tmp_t])
        flat = pst[:].rearrange("p a b -> p (a b)") if len(pst.shape) == 3 else pst[:]
        sc.op("pe", lambda: nc.tensor.transpose(flat[:, 0:n], tmp[:], ident[0:n, 0:n]), outs=[pst_t], ins=[tmp_t, ident_t])
        sc.op("dve", lambda: nc.vector.tensor_copy(out=dst[:, 0:n], in_=flat[:, 0:n]), outs=[dst_t], ins=[pst_t])

    def rms_tile_to_hT(self, sc, x_src_ap, xtile, xt_t, xn, xn_t, junk, junk_t, stat, stat_t, g_fm, g_t,
                       psT, psT_t, hT, hT_t, col0, ident, ident_t, x_dram_t, k):
        nc = self.nc
        sc.dma("sp", xtile[:], x_src_ap, outs=[xt_t], ins=[x_dram_t])
        sc.op("dve", lambda: nc.vector.memset(stat[:, 0:1], 0.0), outs=[stat_t])
        sc.op("act", lambda: nc.scalar.activation(out=junk[:], in_=xtile[:], func=AF.Square, accum_out=stat[:, 0:1]),
              outs=[junk_t, stat_t], ins=[xt_t])
        sc.op("dve", lambda: nc.vector.tensor_scalar(out=stat[:, 1:2], in0=stat[:, 0:1], scalar1=1.0 / D, scalar2=EPS,
                                                      op0=ALU.mult, op1=ALU.add), outs=[stat_t], ins=[stat_t])
        sc.op("act", lambda: nc.scalar.activation(out=stat[:, 3:4], in_=stat[:, 1:2], func=AF.Sqrt),
              outs=[stat_t], ins=[stat_t])
        sc.op("dve", lambda: nc.vector.reciprocal(out=stat[:, 2:3], in_=stat[:, 3:4]), outs=[stat_t], ins=[stat_t])
        sc.op("act", lambda: nc.scalar.activation(out=xn[:], in_=xtile[:], func=AF.Copy, scale=stat[:, 2:3]),
              outs=[xn_t], ins=[xt_t, stat_t])
        for g4 in range(4):
            pt, pt_t = psT[(k * 4 + g4) % len(psT)], psT_t[(k * 4 + g4) % len(psT)]
            for j in range(4):
                c = g4 * 4 + j
                sc.op("pe", lambda c=c, j=j, pt=pt: nc.tensor.transpose(pt[:, j, :], xn[:, c * 128:(c + 1) * 128], ident[:]),
                      outs=[pt_t], ins=[xn_t, ident_t], mark=(j == 3))
            gb = g_fm[:, g4 * 4:g4 * 4 + 4].unsqueeze(2).to_broadcast([128, 4, 128])
            sc.op("dve", lambda pt=pt, gb=gb, g4=g4: nc.vector.tensor_tensor(
                out=hT[:, g4 * 4:g4 * 4 + 4, col0:col0 + 128], in0=pt[:], in1=gb, op=ALU.mult),
                outs=[hT_t], ins=[pt_t, g_t])

    def phase_inproj(self, sc, l, x_ap, x_t, w_in, g1, p_ap, p_t, ident, ident_t, TBLK=1024):
        nc = self.nc
        ntb = TBLK // 128
        with ExitStack() as st:
            xt = [self.sb(st, [128, D], F32, "xt") for _ in range(2)]
            xt_t = [TT() for _ in range(2)]
            xn = [self.sb(st, [128, D], F32, "xn") for _ in range(2)]
            xn_t = [TT() for _ in range(2)]
            junk = self.sb(st, [128, D], BF16, "junk"); junk_t = TT()
            stat = [self.sb(st, [128, 4], F32, "stat") for _ in range(2)]
            stat_t = [TT() for _ in range(2)]
            g_fm = self.sb(st, [128, 16], F32, "gfm"); g_t = TT()
            hT = self.sb(st, [128, 16, TBLK], BF16, "hT"); hT_t = TT()
            wt = [self.sb(st, [128, 16, 512], BF16, "wt") for _ in range(2)]
            wt_t = [TT() for _ in range(2)]
            ot = [self.sb(st, [128, 512], F32, "ot") for _ in range(3)]
            ot_t = [TT() for _ in range(3)]
            psT = [self.ps(st, [128, 4, 128], F32, "psT") for _ in range(3)]
            psT_t = [PT() for _ in range(3)]
            psO = [self.ps(st, [128, 512], F32, "psO") for _ in range(4)]
            psO_t = [PT() for _ in range(4)]
            self.load_fm_vec(sc, st, g_fm, g_t, g1[l], 16, ident, ident_t, psT[0], psT_t[0])
            k = 0
            oi = 0
            wi = 0
            for tb in range(self.T // TBLK):
                for j in range(ntb):
                    r0 = tb * TBLK + j * 128
                    self.rms_tile_to_hT(sc, x_ap[r0:r0 + 128, :], xt[k % 2], xt_t[k % 2], xn[k % 2], xn_t[k % 2],
                                        junk, junk_t, stat[k % 2], stat_t[k % 2], g_fm, g_t, psT, psT_t,
                                        hT, hT_t, j * 128, ident, ident_t, x_t[r0 // 128], k)
                    k += 1
                for cb in range((D_IN + 511) // 512):
                    c0 = cb * 512
                    ncol = min(512, D_IN - c0)
                    w, w_t = wt[wi % 2], wt_t[wi % 2]
                    wi += 1
                    sc.dma("pool", w[:, :, 0:ncol], w_in[l][:, c0:c0 + ncol].rearrange("(c p) n -> p c n", p=128),
                           outs=[w_t])
                    for j in range(ntb):
                        po, po_t = psO[oi % 4], psO_t[oi % 4]
                        o, o_t = ot[oi % 3], ot_t[oi % 3]
                        for c in range(16):
                            sc.op("pe", lambda c=c, j=j, po=po, w=w: nc.tensor.matmul(
                                po[:, 0:ncol], lhsT=hT[:, c, j * 128:(j + 1) * 128], rhs=w[:, c, 0:ncol],
                                start=(c == 0), stop=(c == 15)), outs=[po_t], ins=[hT_t, w_t], mark=(c == 15))
                        if oi % 2 == 0:
                            sc.op("act", lambda po=po, o=o: nc.scalar.copy(out=o[:, 0:ncol], in_=po[:, 0:ncol]),
                                  outs=[o_t], ins=[po_t])
                        else:
                            sc.op("dve", lambda po=po, o=o: nc.vector.tensor_copy(out=o[:, 0:ncol], in_=po[:, 0:ncol]),
                                  outs=[o_t], ins=[po_t])
                        r0 = tb * TBLK + j * 128
                        sc.dma("sp", p_ap[r0:r0 + 128, c0:c0 + ncol], o[:, 0:ncol], accs=[p_t], ins=[o_t])
                        oi += 1
            sc.wait_all("sp", [p_t])
            for e in ("pe", "act", "dve", "pool"):
                sc.wait_all(e, xt_t + xn_t + [junk_t, g_t, hT_t] + stat_t + wt_t + ot_t + psT_t + psO_t)

    def tile_to_hT(self, sc, src_ap, src_t, xtile, xt_t, xn, xn_t, stat, stat_t, g_fm, g_t,
                   psT, psT_t, hT, hT_t, col0, ident, ident_t, k, norm=True, router=None):
        nc = self.nc
        sc.dma("sp", xtile[:], src_ap, outs=[xt_t], ins=[src_t])
        src = xtile
        src_tt = xt_t
        if norm:
            sc.op("dve", lambda: nc.vector.memset(stat[:, 0:1], 0.0), outs=[stat_t])
            sc.op("act", lambda: nc.scalar.activation(out=xn[:], in_=xtile[:], func=AF.Square, accum_out=stat[:, 0:1]),
                  outs=[xn_t, stat_t], ins=[xt_t])
            sc.op("dve", lambda: nc.vector.tensor_scalar(out=stat[:, 1:2], in0=stat[:, 0:1], scalar1=1.0 / D, scalar2=EPS,
                                                          op0=ALU.mult, op1=ALU.add), outs=[stat_t], ins=[stat_t])
            sc.op("act", lambda: nc.scalar.activation(out=stat[:, 3:4], in_=stat[:, 1:2], func=AF.Sqrt),
                  outs=[stat_t], ins=[stat_t])
            sc.op("dve", lambda: nc.vector.reciprocal(out=stat[:, 2:3], in_=stat[:, 3:4]), outs=[stat_t], ins=[stat_t])
            sc.op("act", lambda: nc.scalar.activation(out=xn[:], in_=xtile[:], func=AF.Copy, scale=stat[:, 2:3]),
                  outs=[xn_t], ins=[xt_t, stat_t])
            src = xn
            src_tt = xn_t
        for g4 in range(4):
            pt, pt_t = psT[(k * 4 + g4) % len(psT)], psT_t[(k * 4 + g4) % len(psT)]
            for j in range(4):
                c = g4 * 4 + j
                sc.op("pe", lambda c=c, j=j, pt=pt: nc.tensor.transpose(pt[:, j, :], src[:, c * 128:(c + 1) * 128], ident[:]),
                      outs=[pt_t], ins=[src_tt, ident_t], mark=(j == 3))
            if g_fm is not None:
                gb = g_fm[:, g4 * 4:g4 * 4 + 4].unsqueeze(2).to_broadcast([128, 4, 128])
                sc.op("dve", lambda pt=pt, gb=gb, g4=g4: nc.vector.tensor_tensor(
                    out=hT[:, g4 * 4:g4 * 4 + 4, col0:col0 + 128], in0=pt[:], in1=gb, op=ALU.mult),
                    outs=[hT_t], ins=[pt_t, g_t])
                if router is not None:
                    rtr_f, rtr_ft, pl, pl_t, hTf, hTf_t = router
                    hf, hf_t = hTf[(k * 4 + g4) % 2], hTf_t[(k * 4 + g4) % 2]
                    if self.cfg.get("router_fp32", True):
                        hlo, hlo_t = self.hlo[(k * 4 + g4) % 2], self.hlo_t[(k * 4 + g4) % 2]
                        sc.op("dve", lambda pt=pt, gb=gb, hf=hf: nc.vector.tensor_tensor(out=hf[:], in0=pt[:], in1=gb, op=ALU.mult),
                              outs=[hf_t], ins=[pt_t, g_t])
                        sc.op("dve", lambda hf=hf, hlo=hlo, g4=g4: nc.vector.tensor_tensor(
                            out=hlo[:], in0=hf[:], in1=hT[:, g4 * 4:g4 * 4 + 4, col0:col0 + 128], op=ALU.subtract),
                            outs=[hlo_t], ins=[hf_t, hT_t])
                        for j in range(4):
                            c = g4 * 4 + j
                            sc.op("pe", lambda c=c: nc.tensor.matmul(
                                pl[:, 0:NE], lhsT=hT[:, c, col0:col0 + 128], rhs=self.rtr_b[:, c, :], start=(c == 0), stop=False),
                                outs=[pl_t], ins=[hT_t, self.rtr_bt], mark=False)
                            sc.op("pe", lambda c=c, j=j, hlo=hlo: nc.tensor.matmul(
                                pl[:, 0:NE], lhsT=hlo[:, j, :], rhs=self.rtr_b[:, c, :], start=False, stop=False),
                                outs=[pl_t], ins=[hlo_t, self.rtr_bt], mark=False)
                            sc.op("pe", lambda c=c: nc.tensor.matmul(
                                pl[:, 0:NE], lhsT=hT[:, c, col0:col0 + 128], rhs=self.rtr_lo[:, c, :], start=False, stop=(c == 15)),
                                outs=[pl_t], ins=[hT_t, self.rtr_bt], mark=(j == 3))
                    else:
                        for j in range(4):
                            c = g4 * 4 + j
                            sc.op("pe", lambda c=c, j=j: nc.tensor.matmul(
                                pl[:, 0:NE], lhsT=hT[:, c, col0:col0 + 128], rhs=self.rtr_b[:, c, :], start=(c == 0), stop=(c == 15)),
                                outs=[pl_t], ins=[hT_t, self.rtr_bt], mark=(j == 3))
            else:
                sc.op("dve", lambda pt=pt, g4=g4: nc.vector.tensor_copy(
                    out=hT[:, g4 * 4:g4 * 4 + 4, col0:col0 + 128], in_=pt[:]), outs=[hT_t], ins=[pt_t])

    def phase_outproj(self, sc, l, mix_ap, mix_t, w_out, y, y_tt, ident, ident_t):
        nc = self.nc
        with ExitStack() as st:
            xt = [self.sb(st, [128, D], F32, "xt") for _ in range(2)]
            xt_t = [TT() for _ in range(2)]
            hT = [self.sb(st, [128, 16, 128], BF16, "hT") for _ in range(2)]
            hT_t = [TT() for _ in range(2)]
            wt = self.sb(st, [128, 16, D], BF16, "wo")
            wt_t = [TT() for _ in range(4)]
            rt = [self.sb(st, [128, D], F32, "rt") for _ in range(2)]
            rt_t = [TT() for _ in range(2)]
            psT = [self.ps(st, [128, 4, 128], F32, "psT") for _ in range(3)]
            psT_t = [PT() for _ in range(3)]
            psO = [self.ps(st, [128, 512], F32, "psO") for _ in range(4)]
            psO_t = [PT() for _ in range(4)]
            for cb in range(4):
                sc.dma("pool", wt[:, :, cb * 512:(cb + 1) * 512],
                       w_out[l][:, cb * 512:(cb + 1) * 512].rearrange("(c p) n -> p c n", p=128), outs=[wt_t[cb]])
            oi = 0
            for k in range(self.NT):
                r0 = k * 128
                h, h_t = hT[k % 2], hT_t[k % 2]
                self.tile_to_hT(sc, mix_ap[r0:r0 + 128, :], mix_t, xt[k % 2], xt_t[k % 2], None, None, None, None,
                                None, None, psT, psT_t, h, h_t, 0, ident, ident_t, k, norm=False)
                r, r_t = rt[k % 2], rt_t[k % 2]
                sc.dma("sp", r[:], y[r0:r0 + 128, :], outs=[r_t], ins=[y_tt[k]])
                for cb in range(4):
                    po, po_t = psO[oi % 4], psO_t[oi % 4]
                    oi += 1
                    for c in range(16):
                        sc.op("pe", lambda c=c, po=po, cb=cb, h=h: nc.tensor.matmul(
                            po[:], lhsT=h[:, c, :], rhs=wt[:, c, cb * 512:(cb + 1) * 512],
                            start=(c == 0), stop=(c == 15)), outs=[po_t], ins=[h_t, wt_t[cb]], mark=(c == 15))
                    sc.op("dve", lambda po=po, cb=cb, r=r: nc.vector.tensor_tensor(
                        out=r[:, cb * 512:(cb + 1) * 512], in0=po[:], in1=r[:, cb * 512:(cb + 1) * 512], op=ALU.add),
                        outs=[r_t], ins=[po_t])
                sc.dma("sp", y[r0:r0 + 128, :], r[:], outs=[y_tt[k]], ins=[r_t])
            self.phase_end(sc, xt_t + hT_t + wt_t + rt_t + psT_t + psO_t)

    def phase_end(self, sc, tts):
        for e in ("pe", "act", "dve", "pool", "sp"):
            sc.wait_all(e, tts)

    def phase_ffn(self, sc, l, y, y_tt, g2, experts, router_ap, ident, ident_t, TBLK=1024):
        nc = self.nc
        ntb = TBLK // 128
        NF = D_FF // 128
        moe = router_ap is not None
        with ExitStack() as st:
            xt = self.sb(st, [128, D], F32, "xt"); xt_t = TT()
            xn = self.sb(st, [128, D], F32, "xn"); xn_t = TT()
            stat = [self.sb(st, [128, 4], F32, "stat") for _ in range(2)]
            stat_t = [TT() for _ in range(2)]
            g_fm = self.sb(st, [128, 16], F32, "gfm"); g_t = TT()
            hT = self.sb(st, [128, 16, TBLK], BF16, "hT"); hT_t = TT()
            actT = self.sb(st, [128, NF, TBLK], BF16, "actT"); act_t = TT()
            wp = [self.sb(st, [128, 16, 512], BF16, "wp") for _ in range(3)]
            wp_t = [TT() for _ in range(3)]
            sg = [self.sb(st, [128, 512], BF16, "sg") for _ in range(2)]
            sg_t = [TT() for _ in range(2)]
            rt = [self.sb(st, [128, 512], F32, "rt") for _ in range(3)]
            rt_t = [TT() for _ in range(3)]
            comb = self.sb(st, [128, ntb, NE], F32, "comb"); comb_t = TT()
            rsm = self.sb(st, [128, 8, NE], F32, "rsm"); rsm_t = TT()
            psT = [self.ps(st, [128, 4, 128], F32, "psT") for _ in range(2)]
            psT_t = [PT() for _ in range(2)]
            psG = [self.ps(st, [128, 512], F32, "psG") for _ in range(2)]
            psG_t = [PT() for _ in range(2)]
            psU = [self.ps(st, [128, 512], F32, "psU") for _ in range(2)]
            psU_t = [PT() for _ in range(2)]
            psY = [self.ps(st, [128, 512], F32, "psY") for _ in range(2)]
            psY_t = [PT() for _ in range(2)]
            self.load_fm_vec(sc, st, g_fm, g_t, g2[l], 16, ident, ident_t, psT[0], psT_t[0])
            if moe:
                rtr_f = self.sb(st, [128, 16, NE], F32, "rtrf"); rtr_ft = TT()
                hTf = [self.sb(st, [128, 4, 128], F32, "hTf") for _ in range(2)]
                hTf_t = [TT() for _ in range(2)]
                self.rtr_b = self.sb(st, [128, 16, NE], BF16, "rtrb"); self.rtr_bt = TT()
                self.rtr_lo = self.sb(st, [128, 16, NE], BF16, "rtrlo")
                self.hlo = [self.sb(st, [128, 4, 128], BF16, "hlo") for _ in range(2)]
                self.hlo_t = [TT() for _ in range(2)]
                for c in range(16):
                    sc.dma("sp", rtr_f[:, c, :], router_ap[c * 128:(c + 1) * 128, :], outs=[rtr_ft])
                sc.op("dve", lambda: nc.vector.tensor_copy(out=self.rtr_b[:], in_=rtr_f[:]), outs=[self.rtr_bt], ins=[rtr_ft])
                sc.op("dve", lambda: nc.vector.tensor_tensor(out=self.rtr_lo[:], in0=rtr_f[:], in1=self.rtr_b[:], op=ALU.subtract),
                      outs=[self.rtr_bt], ins=[rtr_ft, self.rtr_bt])
            wi = 0
            gi = 0
            yi = 0
            k = 0
            for tb in range(self.T // TBLK):
                t0 = tb * TBLK
                for j in range(ntb):
                    r0 = t0 + j * 128
                    self.tile_to_hT(sc, y[r0:r0 + 128, :], y_tt[r0 // 128], xt, xt_t, xn, xn_t, stat[k % 2], stat_t[k % 2],
                                    g_fm, g_t, psT, psT_t, hT, hT_t, j * 128, ident, ident_t, k,
                                    router=(rtr_f, rtr_ft, psY[0], psY_t[0], hTf, hTf_t) if moe else None)
                    if moe:
                        self.router_block(sc, comb, comb_t, rsm, rsm_t, psY[0], psY_t[0], j)
                    k += 1
                for e, (wg, wu, wd) in enumerate(experts):
                    for fb in range(D_FF // 512):
                        wgb, wgb_t = wp[wi % 3], wp_t[wi % 3]
                        wi += 1
                        sc.dma("pool", wgb[:], wg[:, fb * 512:(fb + 1) * 512].rearrange("(c p) n -> p c n", p=128), outs=[wgb_t])
                        wub, wub_t = wp[wi % 3], wp_t[wi % 3]
                        wi += 1
                        sc.dma("pool", wub[:], wu[:, fb * 512:(fb + 1) * 512].rearrange("(c p) n -> p c n", p=128), outs=[wub_t])
                        for f4 in range(4):
                            fc = fb * 4 + f4
                            for th in range(TBLK // 512):
                                pg, pg_t = psG[gi % 2], psG_t[gi % 2]
                                pu, pu_t = psU[gi % 2], psU_t[gi % 2]
                                s_, s_t = sg[gi % 2], sg_t[gi % 2]
                                gi += 1
                                for c in range(16):
                                    sc.op("pe", lambda c=c, pg=pg, wgb=wgb, f4=f4, th=th: nc.tensor.matmul(
                                        pg[:], lhsT=wgb[:, c, f4 * 128:(f4 + 1) * 128], rhs=hT[:, c, th * 512:(th + 1) * 512],
                                        start=(c == 0), stop=(c == 15)), outs=[pg_t], ins=[wgb_t, hT_t], mark=(c == 15))
                                for c in range(16):
                                    sc.op("pe", lambda c=c, pu=pu, wub=wub, f4=f4, th=th: nc.tensor.matmul(
                                        pu[:], lhsT=wub[:, c, f4 * 128:(f4 + 1) * 128], rhs=hT[:, c, th * 512:(th + 1) * 512],
                                        start=(c == 0), stop=(c == 15)), outs=[pu_t], ins=[wub_t, hT_t], mark=(c == 15))
                                sc.op("act", lambda pg=pg, s_=s_: nc.scalar.activation(out=s_[:], in_=pg[:], func=AF.Silu),
                                      outs=[s_t], ins=[pg_t])
                                sc.op("dve", lambda pu=pu, s_=s_, fc=fc, th=th: nc.vector.tensor_tensor(
                                    out=actT[:, fc, th * 512:(th + 1) * 512], in0=pu[:], in1=s_[:], op=ALU.mult),
                                    outs=[act_t], ins=[pu_t, s_t])
                    for cb in range(4):
                        pieces = []
                        for (f0, nf) in ((0, 16), (16, 16), (32, 12)):
                            wdb, wdb_t = wp[wi % 3], wp_t[wi % 3]
                            wi += 1
                            sc.dma("pool", wdb[:, 0:nf, :],
                                   wd[f0 * 128:(f0 + nf) * 128, cb * 512:(cb + 1) * 512].rearrange("(c p) n -> p c n", p=128),
                                   outs=[wdb_t])
                            pieces.append((wdb, wdb_t, f0, nf))
                        for j in range(ntb):
                            r0 = t0 + j * 128
                            py_, py_t = psY[yi % 2], psY_t[yi % 2]
                            r, r_t = rt[yi % 3], rt_t[yi % 3]
                            yi += 1
                            sc.dma("sp", r[:], y[r0:r0 + 128, cb * 512:(cb + 1) * 512], outs=[r_t], ins=[y_tt[r0 // 128]])
                            for (wdb, wdb_t, f0, nf) in pieces:
                                for c in range(nf):
                                    fc = f0 + c
                                    sc.op("pe", lambda c=c, fc=fc, py_=py_, wdb=wdb, j=j: nc.tensor.matmul(
                                        py_[:], lhsT=actT[:, fc, j * 128:(j + 1) * 128], rhs=wdb[:, c, :],
                                        start=(fc == 0), stop=(fc == NF - 1)), outs=[py_t], ins=[act_t, wdb_t],
                                        mark=(fc == NF - 1))
                            if moe:
                                sc.op("dve", lambda py_=py_, r=r, j=j, e=e: nc.vector.scalar_tensor_tensor(
                                    out=r[:], in0=py_[:], scalar=comb[:, j, e:e + 1], in1=r[:], op0=ALU.mult, op1=ALU.add),
                                    outs=[r_t], ins=[py_t, comb_t])
                            else:
                                sc.op("dve", lambda py_=py_, r=r: nc.vector.tensor_tensor(
                                    out=r[:], in0=py_[:], in1=r[:], op=ALU.add), outs=[r_t], ins=[py_t])
                            sc.dma("sp", y[r0:r0 + 128, cb * 512:(cb + 1) * 512], r[:], accs=[y_tt[r0 // 128]], ins=[r_t])
            self.phase_end(sc, [xt_t, xn_t, g_t, hT_t, act_t, comb_t, rsm_t] + stat_t + wp_t + sg_t + rt_t + psT_t + psG_t + psU_t + psY_t)

    def bcast_load(self, sc, dst, dst_t, vec_ap):
        sc.dma("sp", dst, vec_ap.partition_broadcast(128), outs=[dst_t])

    def phase_attn_prep(self, sc, l, p_ap, p_t, qkT, qkT_t, C, ident_b, identb_t):
        nc = self.nc
        I = self.I
        V = nc.vector
        with ExitStack() as st:
            xin = [self.sb(st, [128, 2560], F32, "xin") for _ in range(2)]
            xin_t = [TT() for _ in range(2)]
            tmp = self.sb(st, [128, 2560], F32, "tmp"); tmp_t = TT()
            xo = self.sb(st, [128, 2560], BF16, "xo"); xo_t = TT()
            st4 = self.sb(st, [128, 4, 28], F32, "st4"); st4_t = TT()
            gA = self.sb(st, [128, 2, 64], F32, "gA"); gA_t = TT()
            gB = self.sb(st, [128, 2, 128], F32, "gB"); gB_t = TT()
            cs = [self.sb(st, [128, 192], F32, "cs") for _ in range(2)]
            cs_t = [TT() for _ in range(2)]
            stage = [self.sb(st, [128, 20, 512], BF16, "stage") for _ in range(2)]
            stage_t = [TT() for _ in range(2)]
            psT = [self.ps(st, [128, 4, 128], BF16, "psTb") for _ in range(4)]
            psT_t = [PT() for _ in range(4)]
            self.bcast_load(sc, gA[:, 0, :], gA_t, I["da_q_norm"][l])
            self.bcast_load(sc, gA[:, 1, :], gA_t, I["da_k_norm"][l])
            self.bcast_load(sc, gB[:, 0, :], gB_t, I["dl_q_norm"][l])
            self.bcast_load(sc, gB[:, 1, :], gB_t, I["dl_k_norm"][l])
            pi = 0
            for k in range(self.NT):
                r0 = k * 128
                x, x_t = xin[k % 2], xin_t[k % 2]
                c_, c_t = cs[k % 2], cs_t[k % 2]
                sg, sg_t = stage[(k // 4) % 2], stage_t[(k // 4) % 2]
                pos0 = (k % 16) * 128
                sc.dma("sp", x[:, 0:1024], p_ap[r0:r0 + 128, 0:1024], outs=[x_t], ins=[p_t])
                sc.dma("sp", x[:, 1024:2560], p_ap[r0:r0 + 128, 1536:3072], accs=[x_t], ins=[p_t])
                sc.dma("sp", c_[:], C["rope"][pos0:pos0 + 128, :], outs=[c_t])
                for (c0, G, w, gt, gt_t, cso) in ((0, 16, 64, gA, gA_t, 0), (1024, 12, 128, gB, gB_t, 64)):
                    n = G * w
                    hw = w // 2
                    xv = x[:, c0:c0 + n].rearrange("p (g w) -> p g w", w=w)
                    tv = tmp[:, c0:c0 + n].rearrange("p (g w) -> p g w", w=w)
                    ov = xo[:, c0:c0 + n].rearrange("p (g w) -> p g w", w=w)
                    ss = st4[:, 0, 0:G]
                    rs = st4[:, 1, 0:G]
                    sc.op("dve", lambda: V.tensor_tensor(out=tv, in0=xv, in1=xv, op=ALU.mult), outs=[tmp_t], ins=[x_t])
                    sc.op("dve", lambda: V.reduce_sum(out=ss, in_=tv, axis=AX.X), outs=[st4_t], ins=[tmp_t])
                    sc.op("dve", lambda: V.tensor_scalar(out=rs, in0=ss, scalar1=1.0 / w, scalar2=EPS, op0=ALU.mult, op1=ALU.add),
                          outs=[st4_t], ins=[st4_t])
                    sc.op("act", lambda: nc.scalar.activation(out=ss, in_=rs, func=AF.Sqrt), outs=[st4_t], ins=[st4_t])
                    sc.op("dve", lambda: V.reciprocal(out=rs, in_=ss), outs=[st4_t], ins=[st4_t])
                    sc.op("dve", lambda: V.tensor_tensor(out=tv, in0=xv, in1=rs.unsqueeze(2).to_broadcast([128, G, w]), op=ALU.mult),
                          outs=[tmp_t], ins=[x_t, st4_t])
                    for qk in range(2):
                        g0 = qk * (G // 2)
                        gsl = tmp[:, c0 + g0 * w:c0 + (g0 + G // 2) * w].rearrange("p (g w) -> p g w", w=w)
                        gb = gt[:, qk, :].unsqueeze(1).to_broadcast([128, G // 2, w])
                        sc.op("dve", lambda gsl=gsl, gb=gb: V.tensor_tensor(out=gsl, in0=gsl, in1=gb, op=ALU.mult),
                              outs=[tmp_t], ins=[tmp_t, gt_t])
                    cosb = c_[:, cso:cso + hw].unsqueeze(1).to_broadcast([128, G, hw])
                    sinb = c_[:, cso + hw:cso + 2 * hw].unsqueeze(1).to_broadcast([128, G, hw])
                    x1, x2 = tv[:, :, 0:hw], tv[:, :, hw:w]
                    a1, a2 = xv[:, :, 0:hw], xv[:, :, hw:w]
                    sc.op("dve", lambda: V.tensor_tensor(out=a1, in0=x1, in1=cosb, op=ALU.mult), outs=[x_t], ins=[tmp_t, c_t])
                    sc.op("dve", lambda: V.tensor_tensor(out=a2, in0=x2, in1=sinb, op=ALU.mult), outs=[x_t], ins=[tmp_t, c_t])
                    sc.op("dve", lambda: V.tensor_tensor(out=ov[:, :, 0:hw], in0=a1, in1=a2, op=ALU.subtract), outs=[xo_t], ins=[x_t])
                    sc.op("dve", lambda: V.tensor_tensor(out=a1, in0=x2, in1=cosb, op=ALU.mult), outs=[x_t], ins=[tmp_t, c_t])
                    sc.op("dve", lambda: V.tensor_tensor(out=a2, in0=x1, in1=sinb, op=ALU.mult), outs=[x_t], ins=[tmp_t, c_t])
                    sc.op("dve", lambda: V.tensor_tensor(out=ov[:, :, hw:w], in0=a1, in1=a2, op=ALU.add), outs=[xo_t], ins=[x_t])
                for g5 in range(5):
                    pt, pt_t = psT[pi % 4], psT_t[pi % 4]
                    pi += 1
                    for j in range(4):
                        blk = g5 * 4 + j
                        sc.op("pe", lambda blk=blk, j=j, pt=pt: nc.tensor.transpose(pt[:, j, :], xo[:, blk * 128:(blk + 1) * 128], ident_b[:]),
                              outs=[pt_t], ins=[xo_t, identb_t], mark=(j == 3))
                    tcol = (k % 4) * 128
                    if g5 % 2 == 0:
                        sc.op("act", lambda pt=pt, g5=g5, sg=sg: nc.scalar.copy(out=sg[:, g5 * 4:g5 * 4 + 4, tcol:tcol + 128], in_=pt[:]),
                              outs=[sg_t], ins=[pt_t])
                    else:
                        sc.op("dve", lambda pt=pt, g5=g5, sg=sg: V.tensor_copy(out=sg[:, g5 * 4:g5 * 4 + 4, tcol:tcol + 128], in_=pt[:]),
                              outs=[sg_t], ins=[pt_t])
                if k % 4 == 3:
                    t0 = (k // 4) * 512
                    sc.dma("sp", qkT[:, :, t0:t0 + 512].rearrange("b p t -> p b t"), sg[:], accs=[qkT_t], ins=[sg_t])
            self.phase_end(sc, xin_t + [tmp_t, xo_t, st4_t, gA_t, gB_t] + cs_t + stage_t + psT_t)

    def phase_attn(self, sc, l, p_ap, p_t, qkT, qkT_t, mix_ap, mix_t, C):
        nc = self.nc
        I = self.I
        V = nc.vector
        lam_init = 0.8 - 0.6 * math.exp(-0.3 * l)
        with ExitStack() as st:
            qT = [self.sb(st, [128, S], BF16, "qT") for _ in range(2)]
            qT_t = [TT() for _ in range(2)]
            kT = [self.sb(st, [128, S], BF16, "kT") for _ in range(2)]
            kT_t = [TT() for _ in range(2)]
            vt = [self.sb(st, [128, 16, 130], BF16, "vt") for _ in range(2)]
            vt_t = [TT() for _ in range(2)]
            mA = self.sb(st, [128, 7, 128], BF16, "mA"); mA_t = TT()
            mB = self.sb(st, [128, 19, 128], BF16, "mB"); mB_t = TT()
            pe_ = [self.sb(st, [128, 512], BF16, "pexp") for _ in range(3)]
            pe_t = [TT() for _ in range(3)]
            o0 = self.sb(st, [128, 16, 128], F32, "o0"); o0_t = TT()
            ob = [self.sb(st, [128, 16, 128], F32, "ob") for _ in range(2)]
            ob_t = [TT() for _ in range(2)]
            sq = self.sb(st, [128, 16, 128], F32, "sq"); sq_t = TT()
            lamt = self.sb(st, [128, 4, 64], F32, "lamt"); lam_t = TT()
            lams = self.sb(st, [128, 8], F32, "lams"); lams_t = TT()
            gO = self.sb(st, [128, 128], F32, "gO"); gO_t = TT()
            rc = self.sb(st, [128, 8], F32, "rc"); rc_t = TT()
            nst = self.sb(st, [128, 3, 16], F32, "nst"); nst_t = TT()
            psS = [self.ps(st, [128, 512], F32, "psS") for _ in range(3)]
            psS_t = [PT() for _ in range(3)]
            psO = [self.ps(st, [128, 512], F32, "psO") for _ in range(4)]
            psO_t = [PT() for _ in range(4)]
            sc.dma("sp", mA[:], C["maskA"][:, :, :], outs=[mA_t])
            sc.dma("sp", mB[:], C["maskB"][:, :, :], outs=[mB_t])
            for i in range(2):
                sc.op("dve", lambda i=i: V.memset(vt[i][:, :, 128:130], 1.0), outs=[vt_t[i]])
            sc.dma("sp", lamt[:].rearrange("p a b -> p (a b)"), I["da_lambda"][l].rearrange("a b -> (a b)").partition_broadcast(128),
                   outs=[lam_t])
            self.bcast_load(sc, gO[:], gO_t, I["da_out_norm"][l])
            sc.op("dve", lambda: V.tensor_tensor(out=lamt[:, 0, :], in0=lamt[:, 0, :], in1=lamt[:, 1, :], op=ALU.mult), outs=[lam_t], ins=[lam_t])
            sc.op("dve", lambda: V.tensor_tensor(out=lamt[:, 2, :], in0=lamt[:, 2, :], in1=lamt[:, 3, :], op=ALU.mult), outs=[lam_t], ins=[lam_t])
            sc.op("dve", lambda: V.reduce_sum(out=lams[:, 0:1], in_=lamt[:, 0, :], axis=AX.X), outs=[lams_t], ins=[lam_t])
            sc.op("dve", lambda: V.reduce_sum(out=lams[:, 1:2], in_=lamt[:, 2, :], axis=AX.X), outs=[lams_t], ins=[lam_t])
            sc.op("act", lambda: nc.scalar.activation(out=lams[:, 2:4], in_=lams[:, 0:2], func=AF.Exp), outs=[lams_t], ins=[lams_t])
            sc.op("dve", lambda: V.tensor_tensor(out=lams[:, 4:5], in0=lams[:, 3:4], in1=lams[:, 2:3], op=ALU.subtract), outs=[lams_t], ins=[lams_t])
            sc.op("dve", lambda: V.tensor_scalar(out=lams[:, 4:5], in0=lams[:, 4:5], scalar1=-lam_init, scalar2=None, op0=ALU.add),
                  outs=[lams_t], ins=[lams_t])
            units = []
            for b in range(self.NB):
                for h in range(4):
                    units.append(("A", b, h))
                for h in range(6):
                    units.append(("B", b, h))
            si = 0
            ei = 0
            for ui, (kind, b, h) in enumerate(units):
                q, q_t = qT[ui % 2], qT_t[ui % 2]
                k_, k_t = kT[ui % 2], kT_t[ui % 2]
                v, v_t = vt[ui % 2], vt_t[ui % 2]
                if kind == "A":
                    qblk, kblk, vcol, ocol, nsub, kd, scale = h, 4 + h, 1024 + h * 128, h * 128, 2, 64, 64 ** -0.5
                    msk, msk_t = mA, mA_t
                else:
                    qblk, kblk, vcol, ocol, nsub, kd, scale = 8 + h, 14 + h, 3072 + h * 128, 512 + h * 128, 1, 128, 128 ** -0.5
                    msk, msk_t = mB, mB_t
                t0 = b * S
                sc.dma("sp", q[:], qkT[qblk, :, t0:t0 + S], outs=[q_t], ins=[qkT_t])
                sc.dma("sp", k_[:], qkT[kblk, :, t0:t0 + S], outs=[k_t], ins=[qkT_t])
                sc.dma("pool", v[:, :, 0:128], p_ap[t0:t0 + S, vcol:vcol + 128].rearrange("(n p) c -> p n c", p=128),
                       accs=[v_t], ins=[p_t])
                o_out, o_out_t = ob[ui % 2], ob_t[ui % 2]
                for c in range(nsub):
                    pb = c * kd if nsub == 2 else 0
                    for qb in range(4):
                        nkb = 4 * qb + 4
                        for kb in range(nkb):
                            ps_, ps_t = psS[si % 3], psS_t[si % 3]
                            si += 1
                            sc.op("pe", lambda ps_=ps_, kb=kb, qb=qb, pb=pb: nc.tensor.matmul(
                                ps_[:], lhsT=k_[pb:pb + kd, kb * 128:(kb + 1) * 128], rhs=q[pb:pb + kd, qb * 512:(qb + 1) * 512],
                                start=True, stop=True), outs=[ps_t], ins=[k_t, q_t])
                            e_, e_t = pe_[ei % 3], pe_t[ei % 3]
                            ei += 1
                            sc.op("act", lambda e_=e_, ps_=ps_: nc.scalar.activation(out=e_[:], in_=ps_[:], func=AF.Exp, scale=scale),
                                  outs=[e_t], ins=[ps_t])
                            d0 = 4 * qb - kb
                            if kind == "B" or d0 <= 0:
                                mi = d0 + 3
                                mv = msk[:, mi:mi + 4, :].rearrange("p a b -> p (a b)")
                                sc.op("dve", lambda e_=e_, mv=mv: V.tensor_tensor(out=e_[:], in0=e_[:], in1=mv, op=ALU.mult),
                                      outs=[e_t], ins=[e_t, msk_t])
                            for s_ in range(4):
                                if 4 * qb + s_ < kb:
                                    continue
                                last = (kb == 4 * qb + s_)
                                sc.op("pe", lambda s_=s_, e_=e_, kb=kb: nc.tensor.matmul(
                                    psO[s_][:, 0:129], lhsT=e_[:, s_ * 128:(s_ + 1) * 128], rhs=v[:, kb, 0:129],
                                    start=(kb == 0), stop=last), outs=[psO_t[s_]], ins=[e_t, v_t])
                        for s_ in range(4):
                            qi = qb * 4 + s_
                            sc.op("dve", lambda s_=s_: V.reciprocal(out=rc[:, s_:s_ + 1], in_=psO[s_][:, 128:129]),
                                  outs=[rc_t], ins=[psO_t[s_]])
                            if kind == "B":
                                sc.op("dve", lambda s_=s_, qi=qi: V.tensor_scalar(out=o_out[:, qi, :], in0=psO[s_][:, 0:128],
                                                                                scalar1=rc[:, s_:s_ + 1], scalar2=None, op0=ALU.mult),
                                      outs=[o_out_t], ins=[psO_t[s_], rc_t])
                            elif c == 0:
                                sc.op("dve", lambda s_=s_, qi=qi: V.tensor_scalar(out=o0[:, qi, :], in0=psO[s_][:, 0:128],
                                                                                scalar1=rc[:, s_:s_ + 1], scalar2=None, op0=ALU.mult),
                                      outs=[o0_t], ins=[psO_t[s_], rc_t])
                            else:
                                sc.op("dve", lambda s_=s_: V.tensor_tensor(out=rc[:, 4 + s_:5 + s_], in0=rc[:, s_:s_ + 1], in1=lams[:, 4:5], op=ALU.mult),
                                      outs=[rc_t], ins=[rc_t, lams_t])
                                sc.op("dve", lambda s_=s_, qi=qi: V.scalar_tensor_tensor(
                                    out=o_out[:, qi, :], in0=psO[s_][:, 0:128], scalar=rc[:, 4 + s_:5 + s_], in1=o0[:, qi, :],
                                    op0=ALU.mult, op1=ALU.add), outs=[o_out_t], ins=[psO_t[s_], rc_t, o0_t])
                if kind == "A":
                    sc.op("dve", lambda: V.tensor_tensor(out=sq[:], in0=o_out[:], in1=o_out[:], op=ALU.mult), outs=[sq_t], ins=[o_out_t])
                    sc.op("dve", lambda: V.reduce_sum(out=nst[:, 0, :], in_=sq[:], axis=AX.X), outs=[nst_t], ins=[sq_t])
                    sc.op("dve", lambda: V.tensor_scalar(out=nst[:, 1, :], in0=nst[:, 0, :], scalar1=1.0 / 128, scalar2=EPS,
                                                          op0=ALU.mult, op1=ALU.add), outs=[nst_t], ins=[nst_t])
                    sc.op("act", lambda: nc.scalar.activation(out=nst[:, 0, :], in_=nst[:, 1, :], func=AF.Sqrt), outs=[nst_t], ins=[nst_t])
                    sc.op("dve", lambda: V.reciprocal(out=nst[:, 1, :], in_=nst[:, 0, :]), outs=[nst_t], ins=[nst_t])
                    sc.op("dve", lambda: V.scalar_tensor_tensor(out=o_out[:], in0=o_out[:], scalar=1.0 - lam_init,
                                                                 in1=nst[:, 1, :].unsqueeze(2).to_broadcast([128, 16, 128]),
                                                                 op0=ALU.mult, op1=ALU.mult), outs=[o_out_t], ins=[o_out_t, nst_t])
                    sc.op("dve", lambda: V.tensor_tensor(out=o_out[:], in0=o_out[:], in1=gO[:].unsqueeze(1).to_broadcast([128, 16, 128]),
                                                          op=ALU.mult), outs=[o_out_t], ins=[o_out_t, gO_t])
                sc.dma("sp", mix_ap[t0:t0 + S, ocol:ocol + 128].rearrange("(n p) c -> p n c", p=128), o_out[:],
                       accs=[mix_t], ins=[o_out_t])
            self.phase_end(sc, qT_t + kT_t + vt_t + [mA_t, mB_t, o0_t, sq_t, lam_t, lams_t, gO_t, rc_t, nst_t] + pe_t + ob_t + psS_t + psO_t)

    def phase_rwkv(self, sc, l, p_ap, p_t, mix_ap, mix_t, C, ident, ident_t, ident_b, identb_t):
        nc = self.nc
        I = self.I
        V = nc.vector
        A = nc.scalar
        H = 12
        NEG = -math.exp(-0.5)
        with ExitStack() as st:
            def T_(shape, dt, name):
                return self.sb(st, shape, dt, name), TT()
            mu, mu_t = T_([128, RW_IN], F32, "mu")
            pv, pv_t = T_([128, 7, 768], F32, "pv")
            w2f, w2f_t = T_([64, 768], F32, "w2f")
            w2h, w2_t = T_([64, 2, 768], BF16, "w2h")
            a2b, a2_t = T_([64, 768], BF16, "a2b")
            g2b, g2_t = T_([128, 768], BF16, "g2b")
            triU, tri_t = T_([128, 128], BF16, "triU")
            ones, ones_t = T_([128, 128], BF16, "ones")
            mU, mU_t = T_([128, 256], BF16, "mU")
            mL, mL_t = T_([128, 128], BF16, "mL")
            xr, xr_t = T_([128, RW_IN], F32, "xr")
            pr, pr_t = T_([128, RW_IN], F32, "pr")
            lr, lr_t = T_([128, 256], F32, "lr")
            lrT, lrT_t = T_([128, 4, 128], BF16, "lrT")
            lw, lw_t = T_([128, 768], F32, "lw")
            lwb, lwb_t = T_([128, 2, 768], BF16, "lwb")
            asg, asg_t = T_([128, 768], F32, "asg")
            gg, gg_t = T_([128, 768], F32, "gg")
            kk, kk_t = T_([128, 768], F32, "kk")
            km, km_t = T_([128, 768], F32, "km")
            bb, bb_t = T_([128, 768], F32, "bb")
            cum, cum_t = T_([128, 768], F32, "cum")
            t1, t1_t = T_([128, 768], F32, "t1")
            t2, t2_t = T_([128, 768], F32, "t2")
            ex, ex_t = T_([128, 4, 768], F32, "ex")
            s12, s12_t = T_([128, 4, 12], F32, "s12")
            tb, tb_t = T_([128, 4, 768], BF16, "tb")
            hb, hb_t = T_([128, 3, 768], BF16, "hb")
            fm, fm_t = T_([64, H, 4, 128], BF16, "fm")
            gam, gam_t = T_([64, H], F32, "gam")
            Gb, Gb_t = T_([128, H, 256], BF16, "Gb")
            Gk, Gk_t = T_([128, H, 256], BF16, "Gk")
            Pm = [T_([128, H, 128], BF16, "Pm") for _ in range(2)]
            Qm = [T_([128, H, 128], BF16, "Qm") for _ in range(2)]
            Tm = [T_([128, H, 128], BF16, "Tm") for _ in range(2)]
            W0, W0_t = T_([128, H, 64], BF16, "W0")
            U, U_t = T_([128, H, 64], BF16, "U")
            Y, Y_t = T_([128, 768], F32, "Y")
            Sf, Sf_t = T_([64, H, 64], F32, "Sf")
            Sb, Sb_t = T_([64, H, 64], BF16, "Sb")
            ps = [self.ps(st, [128, 512], F32, "psR") for _ in range(6)]
            ps_t = [PT() for _ in range(6)]
            psb = [self.ps(st, [128, 8, 128], BF16, "psRb") for _ in range(2)]
            psb_t = [PT() for _ in range(2)]
            pi = [0]

            def bank():
                i = pi[0] % 6
                pi[0] += 1
                return ps[i], ps_t[i]

            def dve(fn, outs, ins):
                sc.op("dve", fn, outs=outs, ins=ins)

            def act(fn, outs, ins):
                sc.op("act", fn, outs=outs, ins=ins)

            def tt(out, a, b, op, outs, ins):
                dve(lambda: V.tensor_tensor(out=out, in0=a, in1=b, op=op), outs, ins)

            h3 = lambda ap: ap.rearrange("p (h d) -> p h d", d=64)
            self.bcast_load(sc, mu[:], mu_t, I["rw_mu"][l])
            for i, nm in enumerate(("rw_w0", "rw_a0", "rw_k_k", "rw_k_a")):
                self.bcast_load(sc, pv[:, i, :], pv_t, I[nm][l])
            self.bcast_load(sc, pv[:, 4, :], pv_t, I["rw_r_k"][l].rearrange("h d -> (h d)"))
            self.bcast_load(sc, pv[:, 5, :], pv_t, I["rw_ln_g"][l])
            self.bcast_load(sc, pv[:, 6, :], pv_t, I["rw_ln_b"][l])
            sc.dma("sp", w2f[:], I["rw_w2"][l], outs=[w2f_t])
            dve(lambda: V.tensor_copy(out=w2h[:, 0, :], in_=w2f[:]), [w2_t], [w2f_t])
            tt(w2h[:, 1, :], w2f[:], w2h[:, 0, :], ALU.subtract, [w2_t], [w2f_t, w2_t])
            sc.dma("pool", a2b[:], I["rw_a2"][l], outs=[a2_t])
            sc.dma("pool", g2b[:], I["rw_g2"][l], outs=[g2_t])
            sc.dma("sp", triU[:], C["triU"][:, :], outs=[tri_t])
            sc.dma("sp", ones[:], C["ones_b"][:, :], outs=[ones_t])
            sc.dma("sp", mU[:], C["maskU2"][:, :], outs=[mU_t])
            sc.dma("sp", mL[:], C["maskL"][:, :], outs=[mL_t])

            n_pre = -(-len(getattr(self, "pending_precast", [])) // self.NT)
            for k in range(self.NT):
                r0 = k * 128
                first = (k % 16 == 0)
                self.drain_precast(n_pre)
                sc.dma("sp", xr[:], p_ap[r0:r0 + 128, ATT_IN:D_IN], outs=[xr_t], ins=[p_t])
                if first:
                    dve(lambda: V.memset(pr[0:1, :], 0.0), [pr_t], [])
                    sc.dma("sp", pr[1:128, :], p_ap[r0:r0 + 127, ATT_IN:D_IN], accs=[pr_t], ins=[p_t])
                    dve(lambda: V.memset(Sf[:], 0.0), [Sf_t], [])
                    dve(lambda: V.memset(Sb[:], 0.0), [Sb_t], [])
                else:
                    sc.dma("sp", pr[:], p_ap[r0 - 1:r0 + 127, ATT_IN:D_IN], outs=[pr_t], ins=[p_t])
                tt(pr[:], pr[:], xr[:], ALU.subtract, [pr_t], [pr_t, xr_t])
                tt(pr[:], pr[:], mu[:], ALU.mult, [pr_t], [pr_t, mu_t])
                tt(xr[:], xr[:], pr[:], ALU.add, [xr_t], [xr_t, pr_t])
                xr_r, xr_k, xr_v = xr[:, 0:768], xr[:, 768:1536], xr[:, 1536:2304]
                act(lambda: A.activation(out=lr[:, 0:64], in_=xr[:, 2304:2368], func=AF.Tanh), [lr_t], [xr_t])
                act(lambda: A.copy(out=lr[:, 64:128], in_=xr[:, 2368:2432]), [lr_t], [xr_t])
                act(lambda: A.activation(out=lr[:, 128:256], in_=xr[:, 2432:2560], func=AF.Sigmoid), [lr_t], [xr_t])
                pz, pz_t = bank()
                pzv = pz[:].rearrange("p (a b) -> p a b", b=128)
                sc.op("pe", lambda: nc.tensor.transpose(pzv[0:64, 0, :], lr[:, 0:64], ident[:]), outs=[pz_t], ins=[lr_t, ident_t], mark=False)
                sc.op("pe", lambda: nc.tensor.transpose(pzv[0:64, 1, :], lr[:, 64:128], ident[:]), outs=[pz_t], ins=[lr_t, ident_t], mark=False)
                sc.op("pe", lambda: nc.tensor.transpose(pzv[:, 2, :], lr[:, 128:256], ident[:]), outs=[pz_t], ins=[lr_t, ident_t])
                dve(lambda: V.tensor_copy(out=lrT[0:64, 0, :], in_=pzv[0:64, 0, :]), [lrT_t], [pz_t])
                tt(lrT[0:64, 1, :], pzv[0:64, 0, :], lrT[0:64, 0, :], ALU.subtract, [lrT_t], [pz_t, lrT_t])
                dve(lambda: V.tensor_copy(out=lrT[0:64, 2, :], in_=pzv[0:64, 1, :]), [lrT_t], [pz_t])
                dve(lambda: V.tensor_copy(out=lrT[:, 3, :], in_=pzv[:, 2, :]), [lrT_t], [pz_t])
                for (c0, c1) in ((0, 512), (512, 768)):
                    n = c1 - c0
                    pb_, pb_t = bank()
                    for i, (li, wi) in enumerate(((0, 0), (1, 0), (0, 1))):
                        sc.op("pe", lambda li=li, wi=wi, i=i, pb_=pb_: nc.tensor.matmul(
                            pb_[:, 0:n], lhsT=lrT[0:64, li, :], rhs=w2h[:, wi, c0:c1], start=(i == 0), stop=(i == 2)),
                            outs=[pb_t], ins=[lrT_t, w2_t], mark=(i == 2))
                    tt(lw[:, c0:c1], pb_[:, 0:n], pv[:, 0, c0:c1], ALU.add, [lw_t], [pb_t, pv_t])
                    pb_, pb_t = bank()
                    sc.op("pe", lambda pb_=pb_: nc.tensor.matmul(pb_[:, 0:n], lhsT=lrT[0:64, 2, :], rhs=a2b[:, c0:c1], start=True, stop=True),
                          outs=[pb_t], ins=[lrT_t, a2_t])
                    tt(asg[:, c0:c1], pb_[:, 0:n], pv[:, 1, c0:c1], ALU.add, [asg_t], [pb_t, pv_t])
                    pb_, pb_t = bank()
                    sc.op("pe", lambda pb_=pb_: nc.tensor.matmul(pb_[:, 0:n], lhsT=lrT[:, 3, :], rhs=g2b[:, c0:c1], start=True, stop=True),
                          outs=[pb_t], ins=[lrT_t, g2_t])
                    act(lambda pb_=pb_: A.copy(out=gg[:, c0:c1], in_=pb_[:, 0:n]), [gg_t], [pb_t])
                act(lambda: A.activation(out=lw[:], in_=lw[:], func=AF.Sigmoid), [lw_t], [lw_t])
                act(lambda: A.activation(out=asg[:], in_=asg[:], func=AF.Sigmoid), [asg_t], [asg_t])
                dve(lambda: V.tensor_scalar(out=lw[:], in0=lw[:], scalar1=NEG, scalar2=None, op0=ALU.mult), [lw_t], [lw_t])
                tt(kk[:], xr_k, pv[:, 2, :], ALU.mult, [kk_t], [xr_t, pv_t])
                tt(t1[:], kk[:], kk[:], ALU.mult, [t1_t], [kk_t])
                dve(lambda: V.reduce_sum(out=s12[:, 0, :], in_=h3(t1[:]), axis=AX.X), [s12_t], [t1_t])
                act(lambda: A.activation(out=s12[:, 1, :], in_=s12[:, 0, :], func=AF.Sqrt), [s12_t], [s12_t])
                dve(lambda: V.tensor_scalar(out=s12[:, 1, :], in0=s12[:, 1, :], scalar1=1e-12, scalar2=None, op0=ALU.max), [s12_t], [s12_t])
                dve(lambda: V.reciprocal(out=s12[:, 0, :], in_=s12[:, 1, :]), [s12_t], [s12_t])
                tt(h3(kk[:]), h3(kk[:]), s12[:, 0, :].unsqueeze(2).to_broadcast([128, H, 64]), ALU.mult, [kk_t], [kk_t, s12_t])
                dve(lambda: V.scalar_tensor_tensor(out=t1[:], in0=asg[:], scalar=-1.0, in1=pv[:, 3, :], op0=ALU.add, op1=ALU.mult),
                    [t1_t], [asg_t, pv_t])
                dve(lambda: V.scalar_tensor_tensor(out=km[:], in0=t1[:], scalar=1.0, in1=xr_k, op0=ALU.add, op1=ALU.mult),
                    [km_t], [t1_t, xr_t])
                tt(bb[:], kk[:], asg[:], ALU.mult, [bb_t], [kk_t, asg_t])
                dve(lambda: V.tensor_copy(out=lwb[:, 0, :], in_=lw[:]), [lwb_t], [lw_t])
                tt(lwb[:, 1, :], lw[:], lwb[:, 0, :], ALU.subtract, [lwb_t], [lw_t, lwb_t])
                for (c0, c1) in ((0, 512), (512, 768)):
                    n = c1 - c0
                    pc, pc_t = bank()
                    for i in range(2):
                        sc.op("pe", lambda i=i, pc=pc: nc.tensor.matmul(pc[:, 0:n], lhsT=triU[:], rhs=lwb[:, i, c0:c1], start=(i == 0), stop=(i == 1)),
                              outs=[pc_t], ins=[tri_t, lwb_t], mark=(i == 1))
                    act(lambda pc=pc: A.copy(out=cum[:, c0:c1], in_=pc[:, 0:n]), [cum_t], [pc_t])
                    pc, pc_t = bank()
                    for i in range(2):
                        sc.op("pe", lambda i=i, pc=pc: nc.tensor.matmul(pc[:, 0:n], lhsT=ones[:], rhs=lwb[:, i, c0:c1], start=(i == 0), stop=(i == 1)),
                              outs=[pc_t], ins=[ones_t, lwb_t], mark=(i == 1))
                    tt(t2[:, c0:c1], pc[:, 0:n], cum[:, c0:c1], ALU.subtract, [t2_t], [pc_t, cum_t])
                tt(t1[:], cum[:], lw[:], ALU.subtract, [t1_t], [cum_t, lw_t])
                act(lambda: A.activation(out=ex[:, 0, :], in_=cum[:], func=AF.Exp), [ex_t], [cum_t])
                act(lambda: A.activation(out=ex[:, 1, :], in_=cum[:], func=AF.Exp, scale=-1.0), [ex_t], [cum_t])
                act(lambda: A.activation(out=ex[:, 2, :], in_=t1[:], func=AF.Exp), [ex_t], [t1_t])
                act(lambda: A.activation(out=ex[:, 3, :], in_=t2[:], func=AF.Exp), [ex_t], [t2_t])
                pg, pg_t = bank()
                for h in range(H):
                    for i in range(2):
                        sc.op("pe", lambda h=h, i=i, pg=pg: nc.tensor.matmul(pg[0:64, h:h + 1], lhsT=lwb[:, i, h * 64:(h + 1) * 64], rhs=ones[:, 0:1],
                                                                             start=(i == 0), stop=(i == 1)),
                              outs=[pg_t], ins=[lwb_t, ones_t], mark=(h == H - 1 and i == 1))
                act(lambda pg=pg: A.activation(out=gam[:], in_=pg[0:64, 0:H], func=AF.Exp), [gam_t], [pg_t])
                dve(lambda: V.scalar_tensor_tensor(out=tb[:, 0, :], in0=kk[:], scalar=-1.0, in1=ex[:, 2, :], op0=ALU.mult, op1=ALU.mult),
                    [tb_t], [kk_t, ex_t])
                tt(tb[:, 1, :], xr_r, ex[:, 0, :], ALU.mult, [tb_t], [xr_t, ex_t])
                tt(tb[:, 2, :], bb[:], ex[:, 1, :], ALU.mult, [tb_t], [bb_t, ex_t])
                tt(tb[:, 3, :], km[:], ex[:, 1, :], ALU.mult, [tb_t], [km_t, ex_t])
                dve(lambda: V.tensor_copy(out=hb[:, 0, :], in_=xr_v), [hb_t], [xr_t])
                tt(hb[:, 1, :], bb[:], ex[:, 3, :], ALU.mult, [hb_t], [bb_t, ex_t])
                tt(hb[:, 2, :], km[:], ex[:, 3, :], ALU.mult, [hb_t], [km_t, ex_t])
                for h in range(H):
                    pq, pq_t = psb[h % 2], psb_t[h % 2]
                    for a_ in range(4):
                        sc.op("pe", lambda h=h, a_=a_, pq=pq: nc.tensor.transpose(pq[0:64, a_, :], tb[:, a_, h * 64:(h + 1) * 64], ident_b[:]),
                              outs=[pq_t], ins=[tb_t, identb_t], mark=(a_ == 3))
                    if h % 2 == 0:
                        dve(lambda h=h, pq=pq: V.tensor_copy(out=fm[:, h, :, :], in_=pq[0:64, 0:4, :]), [fm_t], [pq_t])
                    else:
                        act(lambda h=h, pq=pq: A.copy(out=fm[:, h, :, :], in_=pq[0:64, 0:4, :]), [fm_t], [pq_t])
                for h in range(H):
                    pa, pa_t = bank()
                    sc.op("pe", lambda h=h, pa=pa: nc.tensor.matmul(pa[:, 0:256], lhsT=fm[:, h, 2, :], rhs=fm[:, h, 0:2, :].rearrange("p a t -> p (a t)"),
                                                                    start=True, stop=True), outs=[pa_t], ins=[fm_t])
                    tt(Gb[:, h, :], pa[:, 0:256], mU[:], ALU.mult, [Gb_t], [pa_t, mU_t])
                    pa2, pa2_t = bank()
                    sc.op("pe", lambda h=h, pa2=pa2: nc.tensor.matmul(pa2[:, 0:256], lhsT=fm[:, h, 3, :], rhs=fm[:, h, 0:2, :].rearrange("p a t -> p (a t)"),
                                                                      start=True, stop=True), outs=[pa2_t], ins=[fm_t])
                    tt(Gk[:, h, :], pa2[:, 0:256], mU[:], ALU.mult, [Gk_t], [pa2_t, mU_t])
                    pa3, pa3_t = bank()
                    sc.op("pe", lambda h=h, pa3=pa3: nc.tensor.matmul(pa3[:, 0:128], lhsT=fm[:, h, 0, :], rhs=fm[:, h, 2, :], start=True, stop=True),
                          outs=[pa3_t], ins=[fm_t])
                    tt(Qm[0][0][:, h, :], pa3[:, 0:128], mL[:], ALU.mult, [Qm[0][1]], [pa3_t, mL_t])
                dve(lambda: V.tensor_copy(out=Pm[0][0][:], in_=Gb[:, :, 0:128]), [Pm[0][1]], [Gb_t])
                tt(Tm[0][0][:], Gb[:, :, 0:128], ident_b[:].unsqueeze(1).to_broadcast([128, H, 128]), ALU.add, [Tm[0][1]], [Gb_t, identb_t])
                cp, cq, ct = 0, 0, 0
                for m in range(1, 7):
                    (P_, P_t), (Q_, Q_t), (Tc, Tc_t) = Pm[cp], Qm[cq], Tm[ct]
                    (Pn, Pn_t), (Qn, Qn_t), (Tn, Tn_t) = Pm[1 - cp], Qm[1 - cq], Tm[1 - ct]
                    for hg in range(3):
                        hs = range(hg * 4, hg * 4 + 4)
                        pqn, pqn_t = bank()
                        for j, h in enumerate(hs):
                            sc.op("pe", lambda h=h, j=j, pqn=pqn: nc.tensor.matmul(pqn[:, j * 128:(j + 1) * 128], lhsT=P_[:, h, :], rhs=Q_[:, h, :],
                                                                                 start=True, stop=True), outs=[pqn_t], ins=[P_t, Q_t], mark=(j == 3))
                        dve(lambda hg=hg, pqn=pqn: V.tensor_copy(out=Qn[:, hg * 4:hg * 4 + 4, :], in_=pqn[:].rearrange("p (a b) -> p a b", b=128)),
                            [Qn_t], [pqn_t])
                        if m < 6:
                            ppn, ppn_t = bank()
                            for j, h in enumerate(hs):
                                sc.op("pe", lambda h=h, j=j, ppn=ppn: nc.tensor.matmul(ppn[:, j * 128:(j + 1) * 128], lhsT=Q_[:, h, :], rhs=P_[:, h, :],
                                                                                     start=True, stop=True), outs=[ppn_t], ins=[P_t, Q_t], mark=(j == 3))
                            act(lambda hg=hg, ppn=ppn: A.copy(out=Pn[:, hg * 4:hg * 4 + 4, :], in_=ppn[:].rearrange("p (a b) -> p a b", b=128)),
                                [Pn_t], [ppn_t])
                        ptn, ptn_t = bank()
                        for j, h in enumerate(hs):
                            sc.op("pe", lambda h=h, j=j, ptn=ptn: nc.tensor.matmul(ptn[:, j * 128:(j + 1) * 128], lhsT=Qn[:, h, :], rhs=Tc[:, h, :],
                                                                                 start=True, stop=True), outs=[ptn_t], ins=[Qn_t, Tc_t], mark=(j == 3))
                        tt(Tn[:, hg * 4:hg * 4 + 4, :], ptn[:].rearrange("p (a b) -> p a b", b=128), Tc[:, hg * 4:hg * 4 + 4, :], ALU.add,
                           [Tn_t], [ptn_t, Tc_t])
                    cp, cq, ct = 1 - cp, 1 - cq, 1 - ct
                Ti, Ti_t = Tm[ct]
                for (h0, h1) in ((0, 8), (8, 12)):
                    pw, pw_t = bank()
                    for h in range(h0, h1):
                        o = pw[:, (h - h0) * 64:(h - h0 + 1) * 64]
                        sc.op("pe", lambda h=h, o=o: nc.tensor.matmul(o, lhsT=fm[:, h, 0, :], rhs=Sb[:, h, :], start=True, stop=False),
                              outs=[pw_t], ins=[fm_t, Sb_t], mark=False)
                        sc.op("pe", lambda h=h, o=o: nc.tensor.matmul(o, lhsT=Gk[:, h, 0:128], rhs=hb[:, 0, h * 64:(h + 1) * 64], start=False, stop=True),
                              outs=[pw_t], ins=[Gk_t, hb_t], mark=(h == h1 - 1))
                    dve(lambda pw=pw, h0=h0, h1=h1: V.tensor_copy(out=W0[:, h0:h1, :], in_=pw[:, 0:(h1 - h0) * 64].rearrange("p (a b) -> p a b", b=64)),
                        [W0_t], [pw_t])
                for (h0, h1) in ((0, 8), (8, 12)):
                    pu, pu_t = bank()
                    for h in range(h0, h1):
                        o = pu[:, (h - h0) * 64:(h - h0 + 1) * 64]
                        sc.op("pe", lambda h=h, o=o: nc.tensor.matmul(o, lhsT=Ti[:, h, :], rhs=W0[:, h, :], start=True, stop=True),
                              outs=[pu_t], ins=[Ti_t, W0_t], mark=(h == h1 - 1))
                    dve(lambda pu=pu, h0=h0, h1=h1: V.tensor_copy(out=U[:, h0:h1, :], in_=pu[:, 0:(h1 - h0) * 64].rearrange("p (a b) -> p a b", b=64)),
                        [U_t], [pu_t])
                for (h0, h1) in ((0, 8), (8, 12)):
                    py_, py_t = bank()
                    for h in range(h0, h1):
                        o = py_[:, (h - h0) * 64:(h - h0 + 1) * 64]
                        sc.op("pe", lambda h=h, o=o: nc.tensor.matmul(o, lhsT=fm[:, h, 1, :], rhs=Sb[:, h, :], start=True, stop=False),
                              outs=[py_t], ins=[fm_t, Sb_t], mark=False)
                        sc.op("pe", lambda h=h, o=o: nc.tensor.matmul(o, lhsT=Gb[:, h, 128:256], rhs=U[:, h, :], start=False, stop=False),
                              outs=[py_t], ins=[Gb_t, U_t], mark=False)
                        sc.op("pe", lambda h=h, o=o: nc.tensor.matmul(o, lhsT=Gk[:, h, 128:256], rhs=hb[:, 0, h * 64:(h + 1) * 64], start=False, stop=True),
                              outs=[py_t], ins=[Gk_t, hb_t], mark=(h == h1 - 1))
                    act(lambda py_=py_, h0=h0, h1=h1: A.copy(out=Y[:, h0 * 64:h1 * 64], in_=py_[:, 0:(h1 - h0) * 64]), [Y_t], [py_t])
                for (h0, h1) in ((0, 8), (8, 12)):
                    pn, pn_t = bank()
                    for h in range(h0, h1):
                        o = pn[0:64, (h - h0) * 64:(h - h0 + 1) * 64]
                        sc.op("pe", lambda h=h, o=o: nc.tensor.matmul(o, lhsT=hb[:, 1, h * 64:(h + 1) * 64], rhs=U[:, h, :], start=True, stop=False),
                              outs=[pn_t], ins=[hb_t, U_t], mark=False)
                        sc.op("pe", lambda h=h, o=o: nc.tensor.matmul(o, lhsT=hb[:, 2, h * 64:(h + 1) * 64], rhs=hb[:, 0, h * 64:(h + 1) * 64], start=False, stop=True),
                              outs=[pn_t], ins=[hb_t], mark=(h == h1 - 1))
                    tt(Sf[:, h0:h1, :], Sf[:, h0:h1, :], gam[:, h0:h1].unsqueeze(2).to_broadcast([64, h1 - h0, 64]), ALU.mult, [Sf_t], [Sf_t, gam_t])
                    tt(Sf[:, h0:h1, :], Sf[:, h0:h1, :], pn[0:64, 0:(h1 - h0) * 64].rearrange("p (a b) -> p a b", b=64), ALU.add, [Sf_t], [Sf_t, pn_t])
                dve(lambda: V.tensor_copy(out=Sb[:], in_=Sf[:]), [Sb_t], [Sf_t])
                Y3 = h3(Y[:])
                dve(lambda: V.reduce_sum(out=s12[:, 2, :], in_=Y3, axis=AX.X), [s12_t], [Y_t])
                dve(lambda: V.tensor_scalar(out=s12[:, 2, :], in0=s12[:, 2, :], scalar1=1.0 / 64, scalar2=None, op0=ALU.mult), [s12_t], [s12_t])
                tt(Y3, Y3, s12[:, 2, :].unsqueeze(2).to_broadcast([128, H, 64]), ALU.subtract, [Y_t], [Y_t, s12_t])
                tt(t1[:], Y[:], Y[:], ALU.mult, [t1_t], [Y_t])
                dve(lambda: V.reduce_sum(out=s12[:, 3, :], in_=h3(t1[:]), axis=AX.X), [s12_t], [t1_t])
                dve(lambda: V.tensor_scalar(out=s12[:, 3, :], in0=s12[:, 3, :], scalar1=1.0 / 64, scalar2=64e-5, op0=ALU.mult, op1=ALU.add),
                    [s12_t], [s12_t])
                act(lambda: A.activation(out=s12[:, 2, :], in_=s12[:, 3, :], func=AF.Sqrt), [s12_t], [s12_t])
                dve(lambda: V.reciprocal(out=s12[:, 3, :], in_=s12[:, 2, :]), [s12_t], [s12_t])
                tt(Y3, Y3, s12[:, 3, :].unsqueeze(2).to_broadcast([128, H, 64]), ALU.mult, [Y_t], [Y_t, s12_t])
                tt(Y[:], Y[:], pv[:, 5, :], ALU.mult, [Y_t], [Y_t, pv_t])
                tt(Y[:], Y[:], pv[:, 6, :], ALU.add, [Y_t], [Y_t, pv_t])
                tt(t1[:], xr_r, km[:], ALU.mult, [t1_t], [xr_t, km_t])
                tt(t1[:], t1[:], pv[:, 4, :], ALU.mult, [t1_t], [t1_t, pv_t])
                dve(lambda: V.reduce_sum(out=s12[:, 2, :], in_=h3(t1[:]), axis=AX.X), [s12_t], [t1_t])
                tt(h3(t1[:]), h3(xr_v), s12[:, 2, :].unsqueeze(2).to_broadcast([128, H, 64]), ALU.mult, [t1_t], [xr_t, s12_t])
                tt(Y[:], Y[:], t1[:], ALU.add, [Y_t], [Y_t, t1_t])
                tt(Y[:], Y[:], gg[:], ALU.mult, [Y_t], [Y_t, gg_t])
                sc.dma("sp", mix_ap[r0:r0 + 128, 1280:2048], Y[:], accs=[mix_t], ins=[Y_t])
            allt = [mu_t, pv_t, w2f_t, w2_t, a2_t, g2_t, tri_t, ones_t, mU_t, mL_t, xr_t, pr_t, lr_t, lrT_t, lw_t, lwb_t, asg_t, gg_t,
                    kk_t, km_t, bb_t, cum_t, t1_t, t2_t, ex_t, s12_t, tb_t, hb_t, fm_t, gam_t, Gb_t, Gk_t, W0_t, U_t, Y_t, Sf_t, Sb_t]
            allt += [x[1] for x in Pm + Qm + Tm] + ps_t + psb_t
            self.phase_end(sc, allt)

    def precast_moe(self, sc, wgt, wut, wdt):
        wgv, wuv, wdv = self.wcast
        todo = []
        for (src, dst, rows) in ((wgt, wgv, NE * 11 * 128), (wut, wuv, NE * 11 * 128), (wdt, wdv, NE * 16 * 128)):
            step = 256
            for r0 in range(0, rows, step):
                todo.append(lambda src=src, dst=dst, r0=r0, step=step: sc.dma("pool", dst[r0:r0 + step, :], src[r0:r0 + step, :],
                                                                              accs=[self.wcast_t]))
        return todo

    def drain_precast(self, n=None):
        todo = getattr(self, "pending_precast", [])
        k = len(todo) if n is None else min(n, len(todo))
        for _ in range(k):
            todo.pop(0)()

    def phase_moe(self, sc, l, xs, xs_tt, g2, wgt, wut, wdt, router_ap, C, ident, ident_t, ident_b, identb_t,
                  h2s, slot_tab):
        nc = self.nc
        V = nc.vector
        A = nc.scalar
        Tn, NTn = self.T, self.NT
        NBLK = (2 * Tn) // 512 + NE
        NSLOT = NBLK * 512
        NF = D_FF // 128
        U32 = mybir.dt.uint32
        IOA = bass.IndirectOffsetOnAxis
        with ExitStack() as st:
            def T_(shape, dt, name):
                return self.sb(st, shape, dt, name), TT()
            xt, xt_t = T_([128, D], F32, "xt")
            xn, xn_t = T_([128, D], F32, "xn")
            gb, gb_t = T_([128, D], F32, "gb")
            h2o = [T_([128, D], BF16, "h2o") for _ in range(1)]
            stat = [T_([128, 4], F32, "stat") for _ in range(2)]
            g_fm, g_t = T_([128, 16], F32, "gfm")
            hTa = [T_([128, 16, 128], BF16, "hTa") for _ in range(1)]
            hTf = [self.sb(st, [128, 4, 128], F32, "hTf") for _ in range(2)]
            hTf_t = [TT() for _ in range(2)]
            self.hlo = [self.sb(st, [128, 4, 128], BF16, "hlo") for _ in range(2)]
            self.hlo_t = [TT() for _ in range(2)]
            rtr_f, rtr_ft = T_([128, 16, NE], F32, "rtrf")
            self.rtr_b = self.sb(st, [128, 16, NE], BF16, "rtrb"); self.rtr_bt = TT()
            self.rtr_lo = self.sb(st, [128, 16, NE], BF16, "rtrlo")
            comb, comb_t = T_([128, 1, NE], F32, "comb")
            rsm, rsm_t = T_([128, 8, NE], F32, "rsm")
            sm, sm_t = T_([128, 8, NE], F32, "sm")
            Mb, Mb_t = T_([128, NE], BF16, "Mb")
            base, base_t = T_([128, NE], F32, "base")
            arr, arr_t = T_([128, 12, NTn], F32, "arr")
            pu, pu_t = T_([128, 2, NTn], U32, "pu")
            rows, rows_t = T_([128, 2, NTn, 2], F32, "rows")
            cst, cst_t = T_([128, 8 + NBLK + 11 + 16 + NTn + 16], F32, "cst")
            BE, BE_t = T_([128, NBLK], F32, "BE")
            triU, tri_t = T_([128, 128], BF16, "triU")
            ones, ones_t = T_([128, 128], BF16, "ones")
            slr, slr_t = T_([128, 4, 2], F32, "slr")
            ids, ids_t = T_([128, 4], U32, "ids")
            idxA, idxA_t = T_([128, 11], U32, "idxA")
            idxD, idxD_t = T_([128, 16], U32, "idxD")
            G = [T_([128, D], BF16, "G") for _ in range(2)]
            hT, hT_t = T_([128, 16, 512], BF16, "hT")
            actT, act_t = T_([128, NF, 512], BF16, "actT")
            wp = [T_([128, 16, 512], BF16, "wp") for _ in range(3)]
            sg = [T_([128, 512], BF16, "sg") for _ in range(2)]
            ot = [T_([128, 512], F32, "ot") for _ in range(3)]
            ids4, ids4_t = T_([128, 4, 4], U32, "ids4")
            psT = [self.ps(st, [128, 4, 128], F32, "psT") for _ in range(2)]
            psT_t = [PT() for _ in range(2)]
            psTb = self.ps(st, [128, 8, 128], BF16, "psTb"); psTb_t = PT()
            psG = [self.ps(st, [128, 512], F32, "psG") for _ in range(2)]
            psG_t = [PT() for _ in range(2)]
            psU = [self.ps(st, [128, 512], F32, "psU") for _ in range(2)]
            psU_t = [PT() for _ in range(2)]
            psY = self.ps(st, [128, 512], F32, "psY"); psY_t = PT()
            sinit_t, slot_t, h2_t = TT(), TT(), TT()
            ys_t = [TT(), TT()]
            o_i8, o_jj, o_cA, o_cD, o_tk = 0, 8, 8 + NBLK, 8 + NBLK + 11, 8 + NBLK + 27
            o_c4 = o_tk + NTn
            iota8 = cst[:, o_i8:o_i8 + 8]
            jj = cst[:, o_jj:o_jj + NBLK]
            R1, R2, E1, E2, W1, W2, S1, S2, TMP, P1, P2 = (arr[:, i, :] for i in range(11))

            def dve(fn, outs, ins):
                sc.op("dve", fn, outs=outs, ins=ins)

            sc.dma("sp", cst[:, 0:8], C["iota8"][:, :], outs=[cst_t])
            sc.dma("sp", cst[:, o_jj:o_jj + NBLK], C["jj"][:, 0:NBLK], accs=[cst_t])
            sc.dma("sp", cst[:, o_cA:o_cA + 27], C["constAD"][:, :], accs=[cst_t])
            sc.dma("sp", cst[:, o_tk:o_tk + NTn], C["tokid"][:, 0:NTn], accs=[cst_t])
            sc.dma("sp", cst[:, o_c4:o_c4 + 16], C["cb4"][:, :], accs=[cst_t])
            sc.dma("sp", triU[:], C["triU"][:, :], outs=[tri_t])
            sc.dma("sp", ones[:], C["ones_b"][:, :], outs=[ones_t])
            sc.dma("sp", slot_tab[0:NSLOT + 128, :], C["slot_init"][0:NSLOT + 128, :], outs=[sinit_t])
            self.bcast_load(sc, gb[:], gb_t, g2[l])
            self.load_fm_vec(sc, st, g_fm, g_t, g2[l], 16, ident, ident_t, psT[0], psT_t[0])
            for c in range(16):
                sc.dma("sp", rtr_f[:, c, :], router_ap[c * 128:(c + 1) * 128, :], accs=[rtr_ft])
            dve(lambda: V.tensor_copy(out=self.rtr_b[:], in_=rtr_f[:]), [self.rtr_bt], [rtr_ft])
            dve(lambda: V.tensor_tensor(out=self.rtr_lo[:], in0=rtr_f[:], in1=self.rtr_b[:], op=ALU.subtract), [self.rtr_bt], [rtr_ft, self.rtr_bt])
            dve(lambda: V.memset(base[:], 0.0), [base_t], [])
            for i in range(2):
                dve(lambda i=i: V.memset(G[i][0][:], 0.0), [G[i][1]], [])

            mask1, mask2 = rsm[:, 2, :], rsm[:, 4, :]
            w2c, w1c = rsm[:, 1, 3:4], rsm[:, 1, 4:5]
            for k in range(NTn):
                r0 = k * 128
                ha, ha_t = hTa[0]
                self.tile_to_hT(sc, xs[r0:r0 + 128, :], xs_tt[k], xt, xt_t, xn, xn_t, stat[k % 2][0], stat[k % 2][1],
                                g_fm, g_t, psT, psT_t, ha, ha_t, 0, ident, ident_t, k,
                                router=(rtr_f, rtr_ft, psY, psY_t, hTf, hTf_t))
                self.router_block(sc, comb, comb_t, rsm, rsm_t, psY, psY_t, 0)
                ho, ho_t = h2o[0]
                sc.op("pool", lambda ho=ho: nc.gpsimd.tensor_tensor(out=ho[:], in0=xn[:], in1=gb[:], op=ALU.mult),
                      outs=[ho_t], ins=[xn_t, gb_t])
                sc.dma("sp", h2s[r0:r0 + 128, :], ho[:], accs=[h2_t], ins=[ho_t])
                dve(lambda: V.tensor_tensor(out=Mb[:], in0=mask1, in1=mask2, op=ALU.add), [Mb_t], [rsm_t])
                sc.op("pe", lambda: nc.tensor.matmul(psY[:, 8:16], lhsT=triU[:], rhs=Mb[:], start=True, stop=True),
                      outs=[psY_t], ins=[tri_t, Mb_t])
                sc.op("pe", lambda: nc.tensor.matmul(psY[:, 16:24], lhsT=ones[:], rhs=Mb[:], start=True, stop=True),
                      outs=[psY_t], ins=[ones_t, Mb_t])
                rk = sm[:, 0, :]
                dve(lambda: V.scalar_tensor_tensor(out=rk, in0=psY[:, 8:16], scalar=-1.0, in1=base[:], op0=ALU.add, op1=ALU.add),
                    [sm_t], [psY_t, base_t])
                dve(lambda: V.tensor_tensor(out=base[:], in0=base[:], in1=psY[:, 16:24], op=ALU.add), [base_t], [base_t, psY_t])
                for (mk, src, dst) in ((mask1, rk, R1), (mask2, rk, R2), (mask1, iota8, E1), (mask2, iota8, E2)):
                    dve(lambda mk=mk, src=src: V.tensor_tensor(out=sm[:, 1, :], in0=mk, in1=src, op=ALU.mult), [sm_t], [rsm_t, sm_t, cst_t])
                    dve(lambda dst=dst, k=k: V.reduce_sum(out=dst[:, k:k + 1], in_=sm[:, 1, :], axis=AX.X), [arr_t], [sm_t])
                dve(lambda k=k: V.tensor_copy(out=W1[:, k:k + 1], in_=w1c), [arr_t], [rsm_t])
                dve(lambda k=k: V.tensor_copy(out=W2[:, k:k + 1], in_=w2c), [arr_t], [rsm_t])

            nb, st8 = sm[:, 2, :], sm[:, 3, :]
            dve(lambda: V.memset(sm[:, 2:4, :], 0.0), [sm_t], [])
            for j in range(Tn // 512):
                dve(lambda j=j: V.scalar_tensor_tensor(out=nb, in0=base[:], scalar=512.0 * j, in1=nb, op0=ALU.is_gt, op1=ALU.add),
                    [sm_t], [sm_t, base_t])
            for e in range(1, NE):
                dve(lambda e=e: V.tensor_tensor(out=st8[:, e:e + 1], in0=st8[:, e - 1:e], in1=nb[:, e - 1:e], op=ALU.add), [sm_t], [sm_t])
            dve(lambda: V.memset(BE[:], -1.0), [BE_t], [])
            dve(lambda: V.memset(arr[:, 6:8, :], 0.0), [arr_t], [])
            for e in range(NE):
                dve(lambda e=e: V.scalar_tensor_tensor(out=BE[:], in0=jj, scalar=st8[:, e:e + 1], in1=BE[:], op0=ALU.is_ge, op1=ALU.add),
                    [BE_t], [BE_t, sm_t, cst_t])
                for (Ex, Sx) in ((E1, S1), (E2, S2)):
                    dve(lambda Ex=Ex, e=e: V.tensor_scalar(out=TMP, in0=Ex, scalar1=float(e), scalar2=None, op0=ALU.is_equal), [arr_t], [arr_t])
                    dve(lambda Sx=Sx, e=e: V.scalar_tensor_tensor(out=Sx, in0=TMP, scalar=st8[:, e:e + 1], in1=Sx, op0=ALU.mult, op1=ALU.add),
                        [arr_t], [arr_t, sm_t])
            for (Sx, Rx, Px, i) in ((S1, R1, P1, 0), (S2, R2, P2, 1)):
                dve(lambda Sx=Sx, Rx=Rx, Px=Px: V.scalar_tensor_tensor(out=Px, in0=Sx, scalar=512.0, in1=Rx, op0=ALU.mult, op1=ALU.add),
                    [arr_t], [arr_t])
                dve(lambda Px=Px, i=i: V.tensor_copy(out=pu[:, i, :], in_=Px), [pu_t], [arr_t])
                dve(lambda i=i: V.tensor_copy(out=rows[:, i, :, 0], in_=cst[:, o_tk:o_tk + NTn]), [rows_t], [cst_t])
                dve(lambda i=i: V.tensor_copy(out=rows[:, i, :, 1], in_=arr[:, 4 + i, :]), [rows_t], [arr_t])
            for k in range(NTn):
                for i in range(2):
                    sc.idma(slot_tab[:, :], rows[:, i, k, :], accs=[slot_t], ins=[rows_t, pu_t, sinit_t],
                            out_offset=IOA(ap=pu[:, i, k:k + 1], axis=0), in_offset=None)

            if self.dbg2 is not None:
                dt_ = TT()
                sc.dma("sp", self.dbg2[:, 0:8], base[:], outs=[dt_], ins=[base_t])
                sc.dma("sp", self.dbg2[:, 8:8 + NBLK], BE[:], accs=[dt_], ins=[BE_t])
                sc.dma("sp", self.dbg2[:, 8 + NBLK:8 + NBLK + NTn], arr[:, 9, :], accs=[dt_], ins=[arr_t])
                sc.dma("sp", self.dbg2[:, 8 + NBLK + NTn:8 + NBLK + 2 * NTn], arr[:, 10, :], accs=[dt_], ins=[arr_t])
                sc.dma("sp", self.dbg3[:, :], slot_tab[0:NSLOT + 128, :], accs=[dt_], ins=[slot_t])
            wgv, wuv, wdv = self.wcast
            wc_t = self.wcast_t
            xs_v = xs.rearrange("r (b f) -> (r b) f", f=512)
            gi = 0
            wi = 0
            oi = 0
            for j in range(NBLK):
                sc.dma("sp", slr[:], slot_tab[j * 512:(j + 1) * 512, :].rearrange("(t p) c -> p t c", p=128), outs=[slr_t], ins=[slot_t])
                dve(lambda: V.tensor_copy(out=ids[:], in_=slr[:, :, 0]), [ids_t], [slr_t])
                dve(lambda: V.scalar_tensor_tensor(out=ids4[:], in0=slr[:, :, 0:1].to_broadcast([128, 4, 4]), scalar=4.0,
                                                   in1=cst[:, o_c4:o_c4 + 16].rearrange("p (a b) -> p a b", b=4),
                                                   op0=ALU.mult, op1=ALU.add), [ids4_t], [slr_t, cst_t])
                dve(lambda j=j: V.scalar_tensor_tensor(out=idxA[:], in0=BE[:, j:j + 1].to_broadcast([128, 11]), scalar=1408.0,
                                                       in1=cst[:, o_cA:o_cA + 11], op0=ALU.mult, op1=ALU.add), [idxA_t], [BE_t, cst_t])
                dve(lambda j=j: V.scalar_tensor_tensor(out=idxD[:], in0=BE[:, j:j + 1].to_broadcast([128, 16]), scalar=2048.0,
                                                       in1=cst[:, o_cD:o_cD + 16], op0=ALU.mult, op1=ALU.add), [idxD_t], [BE_t, cst_t])
                for t in range(4):
                    g_, g_t2 = G[t % 2]
                    sc.idma(g_[:], h2s[:, :], outs=[g_t2], ins=[ids_t, h2_t], out_offset=None, in_offset=IOA(ap=ids[:, t:t + 1], axis=0))
                    for half in range(2):
                        for c8 in range(8):
                            c = half * 8 + c8
                            sc.op("pe", lambda c=c, c8=c8, g_=g_: nc.tensor.transpose(psTb[:, c8, :], g_[:, c * 128:(c + 1) * 128], ident_b[:]),
                                  outs=[psTb_t], ins=[g_t2, identb_t], mark=(c8 == 7))
                        if half == 0:
                            dve(lambda t=t: V.tensor_copy(out=hT[:, 0:8, t * 128:(t + 1) * 128], in_=psTb[:]), [hT_t], [psTb_t])
                        else:
                            sc.op("act", lambda t=t: A.copy(out=hT[:, 8:16, t * 128:(t + 1) * 128], in_=psTb[:]), outs=[hT_t], ins=[psTb_t])
                for fb in range(11):
                    (wgb, wgb_t) = wp[wi % 3]; wi += 1
                    sc.idma(wgb[:].rearrange("p c f -> p (c f)"), wgv[:, :], outs=[wgb_t], ins=[idxA_t, wc_t], out_offset=None,
                            in_offset=IOA(ap=idxA[:, fb:fb + 1], axis=0))
                    (wub, wub_t) = wp[wi % 3]; wi += 1
                    sc.idma(wub[:].rearrange("p c f -> p (c f)"), wuv[:, :], outs=[wub_t], ins=[idxA_t, wc_t], out_offset=None,
                            in_offset=IOA(ap=idxA[:, fb:fb + 1], axis=0))
                    for f4 in range(4):
                        fc = fb * 4 + f4
                        pg, pg_t = psG[gi % 2], psG_t[gi % 2]
                        pu_, pu_t2 = psU[gi % 2], psU_t[gi % 2]
                        s_, s_t = sg[gi % 2]
                        gi += 1
                        for c in range(16):
                            sc.op("pe", lambda c=c, pg=pg, wgb=wgb, f4=f4: nc.tensor.matmul(
                                pg[:], lhsT=wgb[:, c, f4 * 128:(f4 + 1) * 128], rhs=hT[:, c, :], start=(c == 0), stop=(c == 15)),
                                outs=[pg_t], ins=[wgb_t, hT_t], mark=(c == 15))
                        for c in range(16):
                            sc.op("pe", lambda c=c, pu_=pu_, wub=wub, f4=f4: nc.tensor.matmul(
                                pu_[:], lhsT=wub[:, c, f4 * 128:(f4 + 1) * 128], rhs=hT[:, c, :], start=(c == 0), stop=(c == 15)),
                                outs=[pu_t2], ins=[wub_t, hT_t], mark=(c == 15))
                        sc.op("act", lambda pg=pg, s_=s_: A.activation(out=s_[:], in_=pg[:], func=AF.Silu), outs=[s_t], ins=[pg_t])
                        dve(lambda pu_=pu_, s_=s_, fc=fc: V.tensor_tensor(out=actT[:, fc, :], in0=pu_[:], in1=s_[:], op=ALU.mult),
                            [act_t], [pu_t2, s_t])
                for cb in range(4):
                    pieces = []
                    for pc in range(4):
                        if pc < 3:
                            wdb, wdb_t = wp[pc]
                        else:
                            wdb, wdb_t = hT, hT_t
                        sc.idma(wdb[:, 0:11, :].rearrange("p c f -> p (c f)"), wdv[:, :], outs=[wdb_t], ins=[idxD_t, wc_t], out_offset=None,
                                in_offset=IOA(ap=idxD[:, cb * 4 + pc:cb * 4 + pc + 1], axis=0))
                        pieces.append((wdb, wdb_t))
                    for t in range(4):
                        for pc, (wdb, wdb_t) in enumerate(pieces):
                            for c in range(11):
                                fc = pc * 11 + c
                                sc.op("pe", lambda c=c, fc=fc, wdb=wdb, t=t: nc.tensor.matmul(
                                    psY[:], lhsT=actT[:, fc, t * 128:(t + 1) * 128], rhs=wdb[:, c, :], start=(fc == 0), stop=(fc == NF - 1)),
                                    outs=[psY_t], ins=[act_t, wdb_t], mark=(fc == NF - 1))
                        o_, o_t = ot[oi % 3]
                        oi += 1
                        dve(lambda t=t, o_=o_: V.tensor_scalar(out=o_[:], in0=psY[:], scalar1=slr[:, t, 1:2],
                                                              scalar2=None, op0=ALU.mult), [o_t], [psY_t, slr_t])
                        sc.idma(xs_v, o_[:], accs=[ys_t[j % 2]], ins=[o_t, ids4_t, ys_t[(j + 1) % 2]],
                                out_offset=IOA(ap=ids4[:, t, cb:cb + 1], axis=0), in_offset=None, compute_op=ALU.add)
                wi = 0
            allt = [x[1] for x in h2o + stat + hTa + G + wp + sg + ot] + hTf_t + self.hlo_t + psT_t + psG_t + psU_t + ys_t
            allt += [xt_t, xn_t, gb_t, g_t, rtr_ft, self.rtr_bt, comb_t, rsm_t, sm_t, Mb_t, base_t, arr_t, pu_t, rows_t, cst_t, BE_t,
                     tri_t, ones_t, slr_t, ids_t, ids4_t, idxA_t, idxD_t, hT_t, act_t, psTb_t, psY_t, sinit_t, slot_t, h2_t]
            self.phase_end(sc, allt)

    def router_block(self, sc, comb, comb_t, rsm, rsm_t, pl, pl_t, j):
        nc = self.nc
        if True:
            lg, mask1, l2, mask2 = rsm[:, 0, :], rsm[:, 2, :], rsm[:, 3, :], rsm[:, 4, :]
            m1, m2, dd, w2, w1 = (rsm[:, 1, i:i + 1] for i in range(5))
            V = nc.vector
            sc.op("dve", lambda: V.tensor_copy(out=lg, in_=pl[:, 0:NE]), outs=[rsm_t], ins=[pl_t])
            sc.op("dve", lambda: V.reduce_max(out=m1, in_=lg, axis=AX.X), outs=[rsm_t], ins=[rsm_t])
            sc.op("dve", lambda: V.tensor_scalar(out=mask1, in0=lg, scalar1=m1, scalar2=None, op0=ALU.is_equal),
                  outs=[rsm_t], ins=[rsm_t])
            sc.op("dve", lambda: V.scalar_tensor_tensor(out=l2, in0=mask1, scalar=-1e30, in1=lg, op0=ALU.mult, op1=ALU.add),
                  outs=[rsm_t], ins=[rsm_t])
            sc.op("dve", lambda: V.reduce_max(out=m2, in_=l2, axis=AX.X), outs=[rsm_t], ins=[rsm_t])
            sc.op("dve", lambda: V.tensor_scalar(out=mask2, in0=l2, scalar1=m2, scalar2=None, op0=ALU.is_equal),
                  outs=[rsm_t], ins=[rsm_t])
            sc.op("dve", lambda: V.tensor_tensor(out=dd, in0=m2, in1=m1, op=ALU.subtract), outs=[rsm_t], ins=[rsm_t])
            sc.op("act", lambda: nc.scalar.activation(out=w2, in_=dd, func=AF.Sigmoid), outs=[rsm_t], ins=[rsm_t])
            sc.op("dve", lambda: V.tensor_scalar(out=w1, in0=w2, scalar1=-1.0, scalar2=1.0, op0=ALU.mult, op1=ALU.add),
                  outs=[rsm_t], ins=[rsm_t])
            sc.op("dve", lambda j=j: V.tensor_scalar(out=comb[:, j, :], in0=mask1, scalar1=w1, scalar2=None, op0=ALU.mult),
                  outs=[comb_t], ins=[rsm_t])
            sc.op("dve", lambda j=j: V.scalar_tensor_tensor(out=comb[:, j, :], in0=mask2, scalar=w2, in1=comb[:, j, :],
                                                            op0=ALU.mult, op1=ALU.add), outs=[comb_t], ins=[rsm_t, comb_t])

    def build(self):
        cfg = self.cfg
        nc = self.nc
        T_ = self.T
        layers = cfg.get("layers", list(range(DEPTH)))
        phases = cfg.get("phases", None)
        has = lambda ph: phases is None or ph in phases
        I = {}
        I["x"] = self.din("x", [T_, D])
        for nm, shp in self.input_shapes(cfg).items():
            I[nm] = self.din(nm, shp, BF16 if nm in BF16_INPUTS else F32)
        y = self.dout("y", [T_, D])
        p_ap = self.dscr("p_scr", [T_, D_IN])
        mix_ap = self.dscr("mix_scr", [T_, D])
        qkT = self.dscr("qkT_scr", [20, 128, T_], BF16)
        xs = self.dscr("xs_scr", [T_ + 128, D])
        h2s = self.dscr("h2_scr", [T_ + 128, D], BF16)
        slot_tab = self.dscr("slot_scr", [NBLK_MAX * 512 + 128, 2])
        self.wcast = (self.dscr("wgb_scr", [NE * 11 * 128, 8192], BF16), self.dscr("wub_scr", [NE * 11 * 128, 8192], BF16),
                      self.dscr("wdb_scr", [NE * 16 * 128, 5632], BF16))
        self.wcast_t = TT()
        dbg = None
        if cfg.get("dbg"):
            dbg = self.dout("dbg", cfg["dbg_shape"])
        self.dbg2 = self.dout("dbg2", cfg["dbg2_shape"]) if cfg.get("dbg2_shape") else None
        self.dbg3 = self.dout("dbg3", cfg["dbg3_shape"]) if cfg.get("dbg3_shape") else None
        self.I = I
        with ExitStack() as st:
            sc = Sched(nc, st)
            self.sc = sc
            ident = self.sb(st, [128, 128], F32, "identf"); ident_t = TT()
            sc.dma("sp", ident[:], I["ident_f"][:, :], outs=[ident_t])
            p_t, mix_t, qkT_t = TT(), TT(), TT()
            ident_b = self.sb(st, [128, 128], BF16, "identb"); identb_t = TT()
            sc.dma("sp", ident_b[:], I["ident_b"][:, :], outs=[identb_t])
            C = I
            y_tt = [TT() for _ in range(self.NT)]
            for i in range(self.NT):
                sc.dma("sp", xs[i * 128:(i + 1) * 128, :], I["x"][i * 128:(i + 1) * 128, :], outs=[y_tt[i]])
            zt = self.sb(st, [128, D], BF16, "zt"); zt_t = TT()
            sc.op("dve", lambda: nc.vector.memset(zt[:], 0.0), outs=[zt_t])
            sc.dma("sp", h2s[T_:T_ + 128, :], zt[:], ins=[zt_t])
            y_out = y
            y = xs
            for l in layers:
                if has("inproj"):
                    self.phase_inproj(sc, l, y, y_tt, I["w_in"], I["norm1_g"], p_ap, p_t, ident, ident_t)
                if has("attn_prep"):
                    self.phase_attn_prep(sc, l, p_ap, p_t, qkT, qkT_t, C, ident_b, identb_t)
                if has("attn"):
                    self.phase_attn(sc, l, p_ap, p_t, qkT, qkT_t, mix_ap, mix_t, C)
                if has("rwkv"):
                    if l % 2 == 1 and has("ffn") and cfg.get("sparse_moe", True):
                        i_ = l // 2
                        self.pending_precast = self.precast_moe(sc, I["moe_wg_t"][i_], I["moe_wu_t"][i_], I["moe_wd_t"][i_])
                        self.precast_layer = l
                    self.phase_rwkv(sc, l, p_ap, p_t, mix_ap, mix_t, C, ident, ident_t, ident_b, identb_t)
                if has("outproj"):
                    src = mix_ap if not cfg.get("outproj_from_x") else I["x"]
                    self.phase_outproj(sc, l, src, mix_t, I["w_out"], y, y_tt, ident, ident_t)
                if has("ffn"):
                    i = l // 2
                    if l % 2 == 0:
                        ex = [(I["ffn_w_gate"][i], I["ffn_w_up"][i], I["ffn_w_down"][i])]
                        self.phase_ffn(sc, l, y, y_tt, I["norm2_g"], ex, None, ident, ident_t)
                    else:
                        ne = cfg.get("n_experts", NE)
                        if cfg.get("sparse_moe", True):
                            if not getattr(self, "precast_layer", None) == l:
                                self.pending_precast = self.precast_moe(sc, I["moe_wg_t"][i], I["moe_wu_t"][i], I["moe_wd_t"][i])
                            self.drain_precast()
                            self.phase_moe(sc, l, y, y_tt, I["norm2_g"], I["moe_wg_t"][i], I["moe_wu_t"][i], I["moe_wd_t"][i],
                                           I["moe_router"][i], C, ident, ident_t, ident_b, identb_t, h2s, slot_tab)
                        else:
                            ex = [(I["moe_w_gate"][i][e], I["moe_w_up"][i][e], I["moe_w_down"][i][e]) for e in range(ne)]
                            self.phase_ffn(sc, l, y, y_tt, I["norm2_g"], ex, I["moe_router"][i], ident, ident_t)
            if dbg is not None:
                self.emit_dbg(sc, dbg, locals())
            for i in range(self.NT):
                sc.dma("sp", y_out[i * 128:(i + 1) * 128, :], xs[i * 128:(i + 1) * 128, :], outs=[TT()], ins=[y_tt[i]])
            e_sp = sc.E["sp"]
            n_ = len(e_sp["dma_sems"])
            for slot in range(n_):
                cnt = (e_sp["dma_i"] - 1 - slot) // n_ + 1 if e_sp["dma_i"] > slot else 0
                if cnt > 0:
                    sc._wait(e_sp, ("Dsp%d" % slot, e_sp["dma_sems"][slot], 16 * cnt, "dma"))
        return nc

    @staticmethod
    def input_shapes(cfg):
        nl = cfg.get("n_layer_weights", DEPTH)
        nd = cfg.get("n_dense", 2)
        nm = cfg.get("n_moe", 2)
        nei = cfg.get("n_experts", NE)
        shp = {
            "norm1_g": [DEPTH, D], "w_in": [nl, D, D_IN], "w_out": [nl, D, D], "norm2_g": [DEPTH, D],
            "ffn_w_gate": [nd, D, D_FF], "ffn_w_up": [nd, D, D_FF], "ffn_w_down": [nd, D_FF, D],
            "moe_router": [2, D, NE], "moe_w_gate": [nm, nei, D, D_FF], "moe_w_up": [nm, nei, D, D_FF],
            "moe_w_down": [nm, nei, D_FF, D], "ident_f": [128, 128],
            "da_q_norm": [DEPTH, 64], "da_k_norm": [DEPTH, 64], "da_lambda": [DEPTH, 4, 64], "da_out_norm": [DEPTH, 128],
            "dl_q_norm": [DEPTH, 128], "dl_k_norm": [DEPTH, 128],
            "ident_b": [128, 128], "rope": [S, 192], "maskA": [128, 7, 128], "maskB": [128, 19, 128],
            "triU": [128, 128], "ones_b": [128, 128], "maskU2": [128, 256], "maskL": [128, 128],
            "rw_mu": [DEPTH, RW_IN], "rw_w0": [DEPTH, 768], "rw_w2": [DEPTH, 64, 768], "rw_a0": [DEPTH, 768],
            "rw_a2": [DEPTH, 64, 768], "rw_g2": [DEPTH, 128, 768], "rw_k_k": [DEPTH, 768], "rw_k_a": [DEPTH, 768],
            "rw_r_k": [DEPTH, 12, 64], "rw_ln_g": [DEPTH, 768], "rw_ln_b": [DEPTH, 768],
            "iota8": [128, 8], "jj": [128, NBLK_MAX], "constAD": [128, 27], "tokid": [128, NT],
            "slot_init": [NBLK_MAX * 512 + 128, 2], "cb4": [128, 16],
            "moe_wg_t": [nm, NE * 11 * 128, 16 * 512], "moe_wu_t": [nm, NE * 11 * 128, 16 * 512],
            "moe_wd_t": [nm, NE * 16 * 128, 11 * 512],
        }
        if cfg.get("sparse_moe", True):
            for nm_ in ("moe_w_gate", "moe_w_up", "moe_w_down"):
                shp[nm_] = [1, 2, 2]
        else:
            for nm_ in ("moe_wg_t", "moe_wu_t", "moe_wd_t"):
                shp[nm_] = [1, 2, 2]
        if True:
            pass
        for nm_ in cfg.get("unused", ()):
            shp[nm_] = [1, 2, 2]
        return shp

    def emit_dbg(self, sc, dbg, env):
        kind = self.cfg["dbg"]
        d_t = TT()
        rows = self.cfg["dbg_rows"]
        if kind == "p":
            for i, r in enumerate(rows):
                sc.dma("sp", dbg[i * 128:(i + 1) * 128, :], env["p_ap"][r * 128:(r + 1) * 128, :], outs=[d_t], ins=[env["p_t"]])
        if kind == "mix":
            for i, r in enumerate(rows):
                sc.dma("sp", dbg[i * 128:(i + 1) * 128, :], env["mix_ap"][r * 128:(r + 1) * 128, :], outs=[d_t], ins=[env["mix_t"]])
        if kind == "y":
            for i, r in enumerate(rows):
                sc.dma("sp", dbg[i * 128:(i + 1) * 128, :], env["y"][r * 128:(r + 1) * 128, :], outs=[d_t], ins=[env["y_tt"][r]])
        sc.wait_all("sp", [d_t])


_WEIGHT_NAMES = ("norm1_g", "w_in", "da_q_norm", "da_k_norm", "da_lambda", "da_out_norm", "dl_q_norm", "dl_k_norm",
                 "rw_mu", "rw_w0", "rw_w2", "rw_a0", "rw_a2", "rw_g2", "rw_k_k", "rw_k_a", "rw_r_k", "rw_ln_g", "rw_ln_b",
                 "w_out", "norm2_g", "ffn_w_gate", "ffn_w_up", "ffn_w_down", "moe_router", "moe_w_gate", "moe_w_up",
                 "moe_w_down")


def retile_moe(wg, wu, wd):
    n, E = wg.shape[0], wg.shape[1]
    def gu(w):
        return np.ascontiguousarray(w.reshape(n, E, 16, 128, 11, 512).transpose(0, 1, 4, 3, 2, 5)).reshape(n, E * 11 * 128, 16 * 512)
    wdt = np.ascontiguousarray(wd.reshape(n, E, 4, 11, 128, 4, 512).transpose(0, 1, 5, 2, 4, 3, 6)).reshape(n, E * 16 * 128, 11 * 512)
    return gu(wg), gu(wu), wdt


def kernel(**inputs):
    x = np.ascontiguousarray(np.asarray(inputs["x"], dtype=np.float32))
    bsz = x.shape[0]
    assert bsz == NB * NCORES and x.shape[1] == S and x.shape[2] == D
    cfg = {}
    nc = Builder(cfg).build()
    shared = dict(host_constants())
    dummy = np.zeros((1, 2, 2), np.float32)
    for nm in _WEIGHT_NAMES:
        if nm in ("moe_w_gate", "moe_w_up", "moe_w_down"):
            shared[nm] = dummy
        else:
            shared[nm] = np.ascontiguousarray(np.asarray(inputs[nm], dtype=np.float32))
    wgt, wut, wdt = retile_moe(np.asarray(inputs["moe_w_gate"], dtype=np.float32), np.asarray(inputs["moe_w_up"], dtype=np.float32),
                               np.asarray(inputs["moe_w_down"], dtype=np.float32))
    shared["moe_wg_t"], shared["moe_wu_t"], shared["moe_wd_t"] = wgt, wut, wdt
    in_maps = []
    for c in range(NCORES):
        m = dict(shared)
        m["x"] = x[c * NB:(c + 1) * NB].reshape(T, D)
        in_maps.append(m)
    res = run_bass_kernel_spmd(nc, in_maps, core_ids=list(range(NCORES)))
    out = np.stack([np.asarray(r["y"]).reshape(NB, S, D) for r in res.results], axis=0)
    return out.reshape(bsz, S, D).astype(np.float32)
```

```python
import math
import numpy as np
import ml_dtypes
import concourse.bass as bass
import concourse.mybir as mybir
from concourse.bass_utils import run_bass_kernel_spmd

F32 = mybir.dt.float32
BF16 = mybir.dt.bfloat16
AF = mybir.ActivationFunctionType
ALU = mybir.AluOpType
AX = mybir.AxisListType

D = 2048
S = 2048
NB = 2
T = NB * S
NT = T // 128
DEPTH = 4
D_IN = 6400
ATT_IN = 3840
RW_IN = 2560
D_FF = 5632
NE = 8
EPS = 1e-6
NCORES = 8
NBLK_MAX = (2 * T) // 512 + NE


class TT:
    __slots__ = ("w", "r", "name", "excl")

    def __init__(self, name="", excl=False):
        self.w = {}
        self.r = {}
        self.name = name
        self.excl = excl


def PT():
    return TT(excl=True)


class Sched:
    def __init__(self, nc, stack, ndma_sp=40, ndma_pool=24, ndma_act=8):
        self.nc = nc
        self.E = {}
        for name, obj in (("pe", nc.tensor), ("act", nc.scalar), ("dve", nc.vector),
                          ("pool", nc.gpsimd), ("sp", nc.sync)):
            sem = stack.enter_context(nc.semaphore("e_" + name))
            self.E[name] = {"obj": obj, "sem": sem, "count": 0, "waited": {}, "dma_i": 0, "dma_sems": []}
        for name, n in (("sp", ndma_sp), ("pool", ndma_pool), ("act", ndma_act)):
            self.E[name]["dma_sems"] = [stack.enter_context(nc.semaphore("d_%s%d" % (name, i))) for i in range(n)]

    def _wait(self, e, tok):
        key, sem, val, _ = tok
        if e["waited"].get(key, 0) >= val:
            return
        e["obj"].wait_ge(sem, val)
        e["waited"][key] = val

    def _deps(self, eng, outs, ins, same_engine, accs=()):
        e = self.E[eng]
        toks = []
        for t in ins:
            toks.extend(t.w.values())
            if t.excl:
                toks.extend(t.r.values())
        for t in outs:
            toks.extend(t.w.values())
            toks.extend(t.r.values())
        for t in accs:
            toks.extend(t.r.values())
        for tok in toks:
            if tok[3] == eng and not same_engine:
                continue
            self._wait(e, tok)

    def op(self, eng, fn, outs=(), ins=(), same_engine=True, mark=True):
        e = self.E[eng]
        if eng == "pe":
            same_engine = False
        self._deps(eng, outs, ins, same_engine)
        ins_ = fn()
        if mark:
            e["count"] += 1
            ins_.then_inc(e["sem"], 1)
            val = e["count"]
        else:
            val = e["count"] + 1
        tok = ("E" + eng, e["sem"], val, eng)
        for t in outs:
            t.w = {tok[0]: tok}
            t.r = {}
        for t in ins:
            t.r[tok[0]] = tok
        return ins_

    def dma(self, q, out, in_, outs=(), ins=(), accs=(), **kw):
        e = self.E[q]
        self._deps(q, outs, ins, True, accs)
        n = len(e["dma_sems"])
        slot, gen = e["dma_i"] % n, e["dma_i"] // n
        e["dma_i"] += 1
        sem = e["dma_sems"][slot]
        key = "D%s%d" % (q, slot)
        if gen > 0:
            self._wait(e, (key, sem, 16 * gen, "dma"))
        e["obj"].dma_start(out=out, in_=in_, **kw).then_inc(sem, 16)
        tok = (key, sem, 16 * (gen + 1), "dma")
        for t in outs:
            t.w = {key: tok}
            t.r = {}
        for t in accs:
            t.w[key] = tok
            t.r = {}
        for t in ins:
            t.r[key] = tok
        return tok

    def idma(self, out, in_, outs=(), ins=(), accs=(), **kw):
        q = "pool"
        e = self.E[q]
        self._deps(q, outs, ins, True, accs)
        n = len(e["dma_sems"])
        slot, gen = e["dma_i"] % n, e["dma_i"] // n
        e["dma_i"] += 1
        sem = e["dma_sems"][slot]
        key = "D%s%d" % (q, slot)
        if gen > 0:
            self._wait(e, (key, sem, 16 * gen, "dma"))
        e["obj"].indirect_dma_start(out=out, in_=in_, **kw).then_inc(sem, 16)
        tok = (key, sem, 16 * (gen + 1), "dma")
        for t in outs:
            t.w = {key: tok}
            t.r = {}
        for t in accs:
            t.w[key] = tok
            t.r = {}
        for t in ins:
            t.r[key] = tok
        return tok

    def wait_all(self, eng, tts):
        e = self.E[eng]
        for t in tts:
            for tok in list(t.w.values()) + list(t.r.values()):
                self._wait(e, tok)


from contextlib import ExitStack


BF16_INPUTS = ("ident_b", "maskA", "maskB", "triU", "ones_b", "maskU2", "maskL")


def host_constants(Tn=T):
    bf = ml_dtypes.bfloat16
    C = {"ident_f": np.eye(128, dtype=np.float32), "ident_b": np.eye(128, dtype=np.float32).astype(bf)}
    pos = np.arange(S, dtype=np.float64)[:, None]
    tabs = []
    for half in (32, 64):
        inv = 10000.0 ** (-np.arange(half, dtype=np.float64) / half)
        ang = (pos.astype(np.float32) * inv.astype(np.float32)[None, :]).astype(np.float32)
        tabs += [np.cos(ang), np.sin(ang)]
    C["rope"] = np.concatenate(tabs, axis=1).astype(np.float32)
    ki = np.arange(128)[:, None]
    qi = np.arange(128)[None, :]
    mA = np.zeros((128, 7, 128), np.float32)
    mB = np.zeros((128, 19, 128), np.float32)
    for j in range(19):
        delta = (j - 3) * 128 + qi - ki
        cnt = (delta >= 0) * ((delta <= 128).astype(np.int64) + ((delta % 4 == 0) & (delta <= 512)) + ((delta % 16 == 0) & (delta <= 2048)))
        mB[:, j, :] = cnt
        if j < 7:
            mA[:, j, :] = (delta >= 0)
    C["maskA"] = mA.astype(bf)
    C["maskB"] = mB.astype(bf)
    C["triU"] = (ki <= qi).astype(np.float32).astype(bf)
    C["ones_b"] = np.ones((128, 128), np.float32).astype(bf)
    C["maskU2"] = np.concatenate([(ki < qi), (ki <= qi)], axis=1).astype(np.float32).astype(bf)
    C["maskL"] = (ki > qi).astype(np.float32).astype(bf)
    pcol = np.arange(128, dtype=np.float32)[:, None]
    C["iota8"] = np.tile(np.arange(8, dtype=np.float32)[None, :], (128, 1))
    C["jj"] = np.tile(np.arange(NBLK_MAX, dtype=np.float32)[None, :], (128, 1))
    C["constAD"] = np.concatenate([np.arange(11, dtype=np.float32)[None, :] * 128 + pcol,
                                   np.arange(16, dtype=np.float32)[None, :] * 128 + pcol], axis=1).astype(np.float32)
    C["tokid"] = (np.arange(NT, dtype=np.float32)[None, :] * 128 + pcol).astype(np.float32)
    si = np.zeros((NBLK_MAX * 512 + 128, 2), np.float32)
    si[:, 0] = Tn + (np.arange(si.shape[0]) % 128)
    C["slot_init"] = si
    C["cb4"] = np.tile(np.arange(4, dtype=np.float32)[None, :], (128, 4))
    return C


class Builder:
    def __init__(self, cfg):
        self.cfg = cfg
        self.nc = bass.Bass("TRN2", target_bir_lowering=False)
        self.uid = 0
        self.NB = cfg.get("NB", NB)
        self.T = self.NB * S
        self.NT = self.T // 128

    def sb(self, st, shape, dt, name=None):
        self.uid += 1
        return st.enter_context(self.nc.sbuf_tensor("%s_%d" % (name or "sb", self.uid), list(shape), dt))

    def ps(self, st, shape, dt, name=None):
        self.uid += 1
        return st.enter_context(self.nc.psum_tensor("%s_%d" % (name or "ps", self.uid), list(shape), dt))

    def din(self, name, shape, dt=F32):
        return self.nc.dram_tensor(name, list(shape), dt, kind="ExternalInput").ap()

    def dout(self, name, shape, dt=F32):
        return self.nc.dram_tensor(name, list(shape), dt, kind="ExternalOutput").ap()

    def dscr(self, name, shape, dt=F32):
        return self.nc.dram_tensor(name, list(shape), dt, kind="Internal").ap()

    def load_fm_vec(self, sc, st, dst, dst_t, vec_ap, n, ident, ident_t, pst, pst_t):
        nc = self.nc
        tmp = self.sb(st, [n, 128], F32, "fmtmp"); tmp_t = TT()
        sc.dma("sp", tmp[:], vec_ap.rearrange("(c p) -> c p", p=128), outs=[tmp_t])
        flat = pst[:].rearrange("p a b -> p (a b)") if len(pst.shape) == 3 else pst[:]
        sc.op("pe", lambda: nc.tensor.transpose(flat[:, 0:n], tmp[:], ident[0:n, 0:n]), outs=[pst_t], ins=[tmp_t, ident_t])
        sc.op("dve", lambda: nc.vector.tensor_copy(out=dst[:, 0:n], in_=flat[:, 0:n]), outs=[dst_t], ins=[pst_t])

    def rms_tile_to_hT(self, sc, x_src_ap, xtile, xt_t, xn, xn_t, junk, junk_t, stat, stat_t, g_fm, g_t,
                       psT, psT_t, hT, hT_t, col0, ident, ident_t, x_dram_t, k):
        nc = self.nc
        sc.dma("sp", xtile[:], x_src_ap, outs=[xt_t], ins=[x_dram_t])
        sc.op("dve", lambda: nc.vector.memset(stat[:, 0:1], 0.0), outs=[stat_t])
        sc.op("act", lambda: nc.scalar.activation(out=junk[:], in_=xtile[:], func=AF.Square, accum_out=stat[:, 0:1]),
              outs=[junk_t, stat_t], ins=[xt_t])
        sc.op("dve", lambda: nc.vector.tensor_scalar(out=stat[:, 1:2], in0=stat[:, 0:1], scalar1=1.0 / D, scalar2=EPS,
                                                      op0=ALU.mult, op1=ALU.add), outs=[stat_t], ins=[stat_t])
        sc.op("act", lambda: nc.scalar.activation(out=stat[:, 3:4], in_=stat[:, 1:2], func=AF.Sqrt),
              outs=[stat_t], ins=[stat_t])
        sc.op("dve", lambda: nc.vector.reciprocal(out=stat[:, 2:3], in_=stat[:, 3:4]), outs=[stat_t], ins=[stat_t])
        sc.op("act", lambda: nc.scalar.activation(out=xn[:], in_=xtile[:], func=AF.Copy, scale=stat[:, 2:3]),
              outs=[xn_t], ins=[xt_t, stat_t])
        for g4 in range(4):
            pt, pt_t = psT[(k * 4 + g4) % len(psT)], psT_t[(k * 4 + g4) % len(psT)]
            for j in range(4):
                c = g4 * 4 + j
                sc.op("pe", lambda c=c, j=j, pt=pt: nc.tensor.transpose(pt[:, j, :], xn[:, c * 128:(c + 1) * 128], ident[:]),
                      outs=[pt_t], ins=[xn_t, ident_t], mark=(j == 3))
            gb = g_fm[:, g4 * 4:g4 * 4 + 4].unsqueeze(2).to_broadcast([128, 4, 128])
            sc.op("dve", lambda pt=pt, gb=gb, g4=g4: nc.vector.tensor_tensor(
                out=hT[:, g4 * 4:g4 * 4 + 4, col0:col0 + 128], in0=pt[:], in1=gb, op=ALU.mult),
                outs=[hT_t], ins=[pt_t, g_t])

    def phase_inproj(self, sc, l, x_ap, x_t, w_in, g1, p_ap, p_t, ident, ident_t, TBLK=1024):
        nc = self.nc
        ntb = TBLK // 128
        with ExitStack() as st:
            xt = [self.sb(st, [128, D], F32, "xt") for _ in range(2)]
            xt_t = [TT() for _ in range(2)]
            xn = [self.sb(st, [128, D], F32, "xn") for _ in range(2)]
            xn_t = [TT() for _ in range(2)]
            junk = self.sb(st, [128, D], BF16, "junk"); junk_t = TT()
            stat = [self.sb(st, [128, 4], F32, "stat") for _ in range(2)]
            stat_t = [TT() for _ in range(2)]
            g_fm = self.sb(st, [128, 16], F32, "gfm"); g_t = TT()
            hT = self.sb(st, [128, 16, TBLK], BF16, "hT"); hT_t = TT()
            wt = [self.sb(st, [128, 16, 512], BF16, "wt") for _ in range(2)]
            wt_t = [TT() for _ in range(2)]
            ot = [self.sb(st, [128, 512], F32, "ot") for _ in range(3)]
            ot_t = [TT() for _ in range(3)]
            psT = [self.ps(st, [128, 4, 128], F32, "psT") for _ in range(3)]
            psT_t = [PT() for _ in range(3)]
            psO = [self.ps(st, [128, 512], F32, "psO") for _ in range(4)]
            psO_t = [PT() for _ in range(4)]
            self.load_fm_vec(sc, st, g_fm, g_t, g1[l], 16, ident, ident_t, psT[0], psT_t[0])
            k = 0
            oi = 0
            wi = 0
            for tb in range(self.T // TBLK):
                for j in range(ntb):
                    r0 = tb * TBLK + j * 128
                    self.rms_tile_to_hT(sc, x_ap[r0:r0 + 128, :], xt[k % 2], xt_t[k % 2], xn[k % 2], xn_t[k % 2],
                                        junk, junk_t, stat[k % 2], stat_t[k % 2], g_fm, g_t, psT, psT_t,
                                        hT, hT_t, j * 128, ident, ident_t, x_t[r0 // 128], k)
                    k += 1
                for cb in range((D_IN + 511) // 512):
                    c0 = cb * 512
                    ncol = min(512, D_IN - c0)
                    w, w_t = wt[wi % 2], wt_t[wi % 2]
                    wi += 1
                    sc.dma("pool", w[:, :, 0:ncol], w_in[l][:, c0:c0 + ncol].rearrange("(c p) n -> p c n", p=128),
                           outs=[w_t])
                    for j in range(ntb):
                        po, po_t = psO[oi % 4], psO_t[oi % 4]
                        o, o_t = ot[oi % 3], ot_t[oi % 3]
                        for c in range(16):
                            sc.op("pe", lambda c=c, j=j, po=po, w=w: nc.tensor.matmul(
                                po[:, 0:ncol], lhsT=hT[:, c, j * 128:(j + 1) * 128], rhs=w[:, c, 0:ncol],
                                start=(c == 0), stop=(c == 15)), outs=[po_t], ins=[hT_t, w_t], mark=(c == 15))
                        if oi % 2 == 0:
                            sc.op("act", lambda po=po, o=o: nc.scalar.copy(out=o[:, 0:ncol], in_=po[:, 0:ncol]),
                                  outs=[o_t], ins=[po_t])
                        else:
                            sc.op("dve", lambda po=po, o=o: nc.vector.tensor_copy(out=o[:, 0:ncol], in_=po[:, 0:ncol]),
                                  outs=[o_t], ins=[po_t])
                        r0 = tb * TBLK + j * 128
                        sc.dma("sp", p_ap[r0:r0 + 128, c0:c0 + ncol], o[:, 0:ncol], accs=[p_t], ins=[o_t])
                        oi += 1
            sc.wait_all("sp", [p_t])
            for e in ("pe", "act", "dve", "pool"):
                sc.wait_all(e, xt_t + xn_t + [junk_t, g_t, hT_t] + stat_t + wt_t + ot_t + psT_t + psO_t)

    def tile_to_hT(self, sc, src_ap, src_t, xtile, xt_t, xn, xn_t, stat, stat_t, g_fm, g_t,
                   psT, psT_t, hT, hT_t, col0, ident, ident_t, k, norm=True, router=None):
        nc = self.nc
        sc.dma("sp", xtile[:], src_ap, outs=[xt_t], ins=[src_t])
        src = xtile
        src_tt = xt_t
        if norm:
            sc.op("dve", lambda: nc.vector.memset(stat[:, 0:1], 0.0), outs=[stat_t])
            sc.op("act", lambda: nc.scalar.activation(out=xn[:], in_=xtile[:], func=AF.Square, accum_out=stat[:, 0:1]),
                  outs=[xn_t, stat_t], ins=[xt_t])
            sc.op("dve", lambda: nc.vector.tensor_scalar(out=stat[:, 1:2], in0=stat[:, 0:1], scalar1=1.0 / D, scalar2=EPS,
                                                          op0=ALU.mult, op1=ALU.add), outs=[stat_t], ins=[stat_t])
            sc.op("act", lambda: nc.scalar.activation(out=stat[:, 3:4], in_=stat[:, 1:2], func=AF.Sqrt),
                  outs=[stat_t], ins=[stat_t])
            sc.op("dve", lambda: nc.vector.reciprocal(out=stat[:, 2:3], in_=stat[:, 3:4]), outs=[stat_t], ins=[stat_t])
            sc.op("act", lambda: nc.scalar.activation(out=xn[:], in_=xtile[:], func=AF.Copy, scale=stat[:, 2:3]),
                  outs=[xn_t], ins=[xt_t, stat_t])
            src = xn
            src_tt = xn_t
        for g4 in range(4):
            pt, pt_t = psT[(k * 4 + g4) % len(psT)], psT_t[(k * 4 + g4) % len(psT)]
            for j in range(4):
                c = g4 * 4 + j
                sc.op("pe", lambda c=c, j=j, pt=pt: nc.tensor.transpose(pt[:, j, :], src[:, c * 128:(c + 1) * 128], ident[:]),
                      outs=[pt_t], ins=[src_tt, ident_t], mark=(j == 3))
            if g_fm is not None:
                gb = g_fm[:, g4 * 4:g4 * 4 + 4].unsqueeze(2).to_broadcast([128, 4, 128])
                sc.op("dve", lambda pt=pt, gb=gb, g4=g4: nc.vector.tensor_tensor(
                    out=hT[:, g4 * 4:g4 * 4 + 4, col0:col0 + 128], in0=pt[:], in1=gb, op=ALU.mult),
                    outs=[hT_t], ins=[pt_t, g_t])
                if router is not None:
                    rtr_f, rtr_ft, pl, pl_t, hTf, hTf_t = router
                    hf, hf_t = hTf[(k * 4 + g4) % 2], hTf_t[(k * 4 + g4) % 2]
                    if self.cfg.get("router_fp32", True):
                        hlo, hlo_t = self.hlo[(k * 4 + g4) % 2], self.hlo_t[(k * 4 + g4) % 2]
                        sc.op("dve", lambda pt=pt, gb=gb, hf=hf: nc.vector.tensor_tensor(out=hf[:], in0=pt[:], in1=gb, op=ALU.mult),
                              outs=[hf_t], ins=[pt_t, g_t])
                        sc.op("dve", lambda hf=hf, hlo=hlo, g4=g4: nc.vector.tensor_tensor(
                            out=hlo[:], in0=hf[:], in1=hT[:, g4 * 4:g4 * 4 + 4, col0:col0 + 128], op=ALU.subtract),
                            outs=[hlo_t], ins=[hf_t, hT_t])
                        for j in range(4):
                            c = g4 * 4 + j
                            sc.op("pe", lambda c=c: nc.tensor.matmul(
                                pl[:, 0:NE], lhsT=hT[:, c, col0:col0 + 128], rhs=self.rtr_b[:, c, :], start=(c == 0), stop=False),
                                outs=[pl_t], ins=[hT_t, self.rtr_bt], mark=False)
                            sc.op("pe", lambda c=c, j=j, hlo=hlo: nc.tensor.matmul(
                                pl[:, 0:NE], lhsT=hlo[:, j, :], rhs=self.rtr_b[:, c, :], start=False, stop=False),
                                outs=[pl_t], ins=[hlo_t, self.rtr_bt], mark=False)
                            sc.op("pe", lambda c=c: nc.tensor.matmul(
                                pl[:, 0:NE], lhsT=hT[:, c, col0:col0 + 128], rhs=self.rtr_lo[:, c, :], start=False, stop=(c == 15)),
                                outs=[pl_t], ins=[hT_t, self.rtr_bt], mark=(j == 3))
                    else:
                        for j in range(4):
                            c = g4 * 4 + j
                            sc.op("pe", lambda c=c, j=j: nc.tensor.matmul(
                                pl[:, 0:NE], lhsT=hT[:, c, col0:col0 + 128], rhs=self.rtr_b[:, c, :], start=(c == 0), stop=(c == 15)),
                                outs=[pl_t], ins=[hT_t, self.rtr_bt], mark=(j == 3))
            else:
                sc.op("dve", lambda pt=pt, g4=g4: nc.vector.tensor_copy(
                    out=hT[:, g4 * 4:g4 * 4 + 4, col0:col0 + 128], in_=pt[:]), outs=[hT_t], ins=[pt_t])

    def phase_outproj(self, sc, l, mix_ap, mix_t, w_out, y, y_tt, ident, ident_t):
        nc = self.nc
        with ExitStack() as st:
            xt = [self.sb(st, [128, D], F32, "xt") for _ in range(2)]
            xt_t = [TT() for _ in range(2)]
            hT = [self.sb(st, [128, 16, 128], BF16, "hT") for _ in range(2)]
            hT_t = [TT() for _ in range(2)]
            wt = self.sb(st, [128, 16, D], BF16, "wo")
            wt_t = [TT() for _ in range(4)]
            rt = [self.sb(st, [128, D], F32, "rt") for _ in range(2)]
            rt_t = [TT() for _ in range(2)]
            psT = [self.ps(st, [128, 4, 128], F32, "psT") for _ in range(3)]
            psT_t = [PT() for _ in range(3)]
            psO = [self.ps(st, [128, 512], F32, "psO") for _ in range(4)]
            psO_t = [PT() for _ in range(4)]
            for cb in range(4):
                sc.dma("pool", wt[:, :, cb * 512:(cb + 1) * 512],
                       w_out[l][:, cb * 512:(cb + 1) * 512].rearrange("(c p) n -> p c n", p=128), outs=[wt_t[cb]])
            oi = 0
            for k in range(self.NT):
                r0 = k * 128
                h, h_t = hT[k % 2], hT_t[k % 2]
                self.tile_to_hT(sc, mix_ap[r0:r0 + 128, :], mix_t, xt[k % 2], xt_t[k % 2], None, None, None, None,
                                None, None, psT, psT_t, h, h_t, 0, ident, ident_t, k, norm=False)
                r, r_t = rt[k % 2], rt_t[k % 2]
                sc.dma("sp", r[:], y[r0:r0 + 128, :], outs=[r_t], ins=[y_tt[k]])
                for cb in range(4):
                    po, po_t = psO[oi % 4], psO_t[oi % 4]
                    oi += 1
                    for c in range(16):
                        sc.op("pe", lambda c=c, po=po, cb=cb, h=h: nc.tensor.matmul(
                            po[:], lhsT=h[:, c, :], rhs=wt[:, c, cb * 512:(cb + 1) * 512],
                            start=(c == 0), stop=(c == 15)), outs=[po_t], ins=[h_t, wt_t[cb]], mark=(c == 15))
                    sc.op("dve", lambda po=po, cb=cb, r=r: nc.vector.tensor_tensor(
                        out=r[:, cb * 512:(cb + 1) * 512], in0=po[:], in1=r[:, cb * 512:(cb + 1) * 512], op=ALU.add),
                        outs=[r_t], ins=[po_t])
                sc.dma("sp", y[r0:r0 + 128, :], r[:], outs=[y_tt[k]], ins=[r_t])
            self.phase_end(sc, xt_t + hT_t + wt_t + rt_t + psT_t + psO_t)

    def phase_end(self, sc, tts):
        for e in ("pe", "act", "dve", "pool", "sp"):
            sc.wait_all(e, tts)

    def phase_ffn(self, sc, l, y, y_tt, g2, experts, router_ap, ident, ident_t, TBLK=1024):
        nc = self.nc
        ntb = TBLK // 128
        NF = D_FF // 128
        moe = router_ap is not None
        with ExitStack() as st:
            xt = self.sb(st, [128, D], F32, "xt"); xt_t = TT()
            xn = self.sb(st, [128, D], F32, "xn"); xn_t = TT()
            stat = [self.sb(st, [128, 4], F32, "stat") for _ in range(2)]
            stat_t = [TT() for _ in range(2)]
            g_fm = self.sb(st, [128, 16], F32, "gfm"); g_t = TT()
            hT = self.sb(st, [128, 16, TBLK], BF16, "hT"); hT_t = TT()
            actT = self.sb(st, [128, NF, TBLK], BF16, "actT"); act_t = TT()
            wp = [self.sb(st, [128, 16, 512], BF16, "wp") for _ in range(3)]
            wp_t = [TT() for _ in range(3)]
            sg = [self.sb(st, [128, 512], BF16, "sg") for _ in range(2)]
            sg_t = [TT() for _ in range(2)]
            rt = [self.sb(st, [128, 512], F32, "rt") for _ in range(3)]
            rt_t = [TT() for _ in range(3)]
            comb = self.sb(st, [128, ntb, NE], F32, "comb"); comb_t = TT()
            rsm = self.sb(st, [128, 8, NE], F32, "rsm"); rsm_t = TT()
            psT = [self.ps(st, [128, 4, 128], F32, "psT") for _ in range(2)]
            psT_t = [PT() for _ in range(2)]
            psG = [self.ps(st, [128, 512], F32, "psG") for _ in range(2)]
            psG_t = [PT() for _ in range(2)]
            psU = [self.ps(st, [128, 512], F32, "psU") for _ in range(2)]
            psU_t = [PT() for _ in range(2)]
            psY = [self.ps(st, [128, 512], F32, "psY") for _ in range(2)]
            psY_t = [PT() for _ in range(2)]
            self.load_fm_vec(sc, st, g_fm, g_t, g2[l], 16, ident, ident_t, psT[0], psT_t[0])
            if moe:
                rtr_f = self.sb(st, [128, 16, NE], F32, "rtrf"); rtr_ft = TT()
                hTf = [self.sb(st, [128, 4, 128], F32, "hTf") for _ in range(2)]
                hTf_t = [TT() for _ in range(2)]
                self.rtr_b = self.sb(st, [128, 16, NE], BF16, "rtrb"); self.rtr_bt = TT()
                self.rtr_lo = self.sb(st, [128, 16, NE], BF16, "rtrlo")
                self.hlo = [self.sb(st, [128, 4, 128], BF16, "hlo") for _ in range(2)]
                self.hlo_t = [TT() for _ in range(2)]
                for c in range(16):
                    sc.dma("sp", rtr_f[:, c, :], router_ap[c * 128:(c + 1) * 128, :], outs=[rtr_ft])
                sc.op("dve", lambda: nc.vector.tensor_copy(out=self.rtr_b[:], in_=rtr_f[:]), outs=[self.rtr_bt], ins=[rtr_ft])
                sc.op("dve", lambda: nc.vector.tensor_tensor(out=self.rtr_lo[:], in0=rtr_f[:], in1=self.rtr_b[:], op=ALU.subtract),
                      outs=[self.rtr_bt], ins=[rtr_ft, self.rtr_bt])
            wi = 0
            gi = 0
            yi = 0
            k = 0
            for tb in range(self.T // TBLK):
                t0 = tb * TBLK
                for j in range(ntb):
                    r0 = t0 + j * 128
                    self.tile_to_hT(sc, y[r0:r0 + 128, :], y_tt[r0 // 128], xt, xt_t, xn, xn_t, stat[k % 2], stat_t[k % 2],
                                    g_fm, g_t, psT, psT_t, hT, hT_t, j * 128, ident, ident_t, k,
                                    router=(rtr_f, rtr_ft, psY[0], psY_t[0], hTf, hTf_t) if moe else None)
                    if moe:
                        self.router_block(sc, comb, comb_t, rsm, rsm_t, psY[0], psY_t[0], j)
                    k += 1
                for e, (wg, wu, wd) in enumerate(experts):
                    for fb in range(D_FF // 512):
                        wgb, wgb_t = wp[wi % 3], wp_t[wi % 3]
                        wi += 1
                        sc.dma("pool", wgb[:], wg[:, fb * 512:(fb + 1) * 512].rearrange("(c p) n -> p c n", p=128), outs=[wgb_t])
                        wub, wub_t = wp[wi % 3], wp_t[wi % 3]
                        wi += 1
                        sc.dma("pool", wub[:], wu[:, fb * 512:(fb + 1) * 512].rearrange("(c p) n -> p c n", p=128), outs=[wub_t])
                        for f4 in range(4):
                            fc = fb * 4 + f4
                            for th in range(TBLK // 512):
                                pg, pg_t = psG[gi % 2], psG_t[gi % 2]
                                pu, pu_t = psU[gi % 2], psU_t[gi % 2]
                                s_, s_t = sg[gi % 2], sg_t[gi % 2]
                                gi += 1
                                for c in range(16):
                                    sc.op("pe", lambda c=c, pg=pg, wgb=wgb, f4=f4, th=th: nc.tensor.matmul(
                                        pg[:], lhsT=wgb[:, c, f4 * 128:(f4 + 1) * 128], rhs=hT[:, c, th * 512:(th + 1) * 512],
                                        start=(c == 0), stop=(c == 15)), outs=[pg_t], ins=[wgb_t, hT_t], mark=(c == 15))
                                for c in range(16):
                                    sc.op("pe", lambda c=c, pu=pu, wub=wub, f4=f4, th=th: nc.tensor.matmul(
                                        pu[:], lhsT=wub[:, c, f4 * 128:(f4 + 1) * 128], rhs=hT[:, c, th * 512:(th + 1) * 512],
                                        start=(c == 0), stop=(c == 15)), outs=[pu_t], ins=[wub_t, hT_t], mark=(c == 15))
                                sc.op("act", lambda pg=pg, s_=s_: nc.scalar.activation(out=s_[:], in_=pg[:], func=AF.Silu),
                                      outs=[s_t], ins=[pg_t])
                                sc.op("dve", lambda pu=pu, s_=s_, fc=fc, th=th: nc.vector.tensor_tensor(
                                    out=actT[:, fc, th * 512:(th + 1) * 512], in0=pu[:], in1=s_[:], op=ALU.mult),
                                    outs=[act_t], ins=[pu_t, s_t])
                    for cb in range(4):
                        pieces = []
                        for (f0, nf) in ((0, 16), (16, 16), (32, 12)):
                            wdb, wdb_t = wp[wi % 3], wp_t[wi % 3]
                            wi += 1
                            sc.dma("pool", wdb[:, 0:nf, :],
                                   wd[f0 * 128:(f0 + nf) * 128, cb * 512:(cb + 1) * 512].rearrange("(c p) n -> p c n", p=128),
                                   outs=[wdb_t])
                            pieces.append((wdb, wdb_t, f0, nf))
                        for j in range(ntb):
                            r0 = t0 + j * 128
                            py_, py_t = psY[yi % 2], psY_t[yi % 2]
                            r, r_t = rt[yi % 3], rt_t[yi % 3]
                            yi += 1
                            sc.dma("sp", r[:], y[r0:r0 + 128, cb * 512:(cb + 1) * 512], outs=[r_t], ins=[y_tt[r0 // 128]])
                            for (wdb, wdb_t, f0, nf) in pieces:
                                for c in range(nf):
                                    fc = f0 + c
                                    sc.op("pe", lambda c=c, fc=fc, py_=py_, wdb=wdb, j=j: nc.tensor.matmul(
                                        py_[:], lhsT=actT[:, fc, j * 128:(j + 1) * 128], rhs=wdb[:, c, :],
                                        start=(fc == 0), stop=(fc == NF - 1)), outs=[py_t], ins=[act_t, wdb_t],
                                        mark=(fc == NF - 1))
                            if moe:
                                sc.op("dve", lambda py_=py_, r=r, j=j, e=e: nc.vector.scalar_tensor_tensor(
                                    out=r[:], in0=py_[:], scalar=comb[:, j, e:e + 1], in1=r[:], op0=ALU.mult, op1=ALU.add),
                                    outs=[r_t], ins=[py_t, comb_t])
                            else:
                                sc.op("dve", lambda py_=py_, r=r: nc.vector.tensor_tensor(
                                    out=r[:], in0=py_[:], in1=r[:], op=ALU.add), outs=[r_t], ins=[py_t])
                            sc.dma("sp", y[r0:r0 + 128, cb * 512:(cb + 1) * 512], r[:], accs=[y_tt[r0 // 128]], ins=[r_t])
            self.phase_end(sc, [xt_t, xn_t, g_t, hT_t, act_t, comb_t, rsm_t] + stat_t + wp_t + sg_t + rt_t + psT_t + psG_t + psU_t + psY_t)

    def bcast_load(self, sc, dst, dst_t, vec_ap):
        sc.dma("sp", dst, vec_ap.partition_broadcast(128), outs=[dst_t])

    def phase_attn_prep(self, sc, l, p_ap, p_t, qkT, qkT_t, C, ident_b, identb_t):
        nc = self.nc
        I = self.I
        V = nc.vector
        with ExitStack() as st:
            xin = [self.sb(st, [128, 2560], F32, "xin") for _ in range(2)]
            xin_t = [TT() for _ in range(2)]
            tmp = self.sb(st, [128, 2560], F32, "tmp"); tmp_t = TT()
            xo = self.sb(st, [128, 2560], BF16, "xo"); xo_t = TT()
            st4 = self.sb(st, [128, 4, 28], F32, "st4"); st4_t = TT()
            gA = self.sb(st, [128, 2, 64], F32, "gA"); gA_t = TT()
            gB = self.sb(st, [128, 2, 128], F32, "gB"); gB_t = TT()
            cs = [self.sb(st, [128, 192], F32, "cs") for _ in range(2)]
            cs_t = [TT() for _ in range(2)]
            stage = [self.sb(st, [128, 20, 512], BF16, "stage") for _ in range(2)]
            stage_t = [TT() for _ in range(2)]
            psT = [self.ps(st, [128, 4, 128], BF16, "psTb") for _ in range(4)]
            psT_t = [PT() for _ in range(4)]
            self.bcast_load(sc, gA[:, 0, :], gA_t, I["da_q_norm"][l])
            self.bcast_load(sc, gA[:, 1, :], gA_t, I["da_k_norm"][l])
            self.bcast_load(sc, gB[:, 0, :], gB_t, I["dl_q_norm"][l])
            self.bcast_load(sc, gB[:, 1, :], gB_t, I["dl_k_norm"][l])
            pi = 0
            for k in range(self.NT):
                r0 = k * 128
                x, x_t = xin[k % 2], xin_t[k % 2]
                c_, c_t = cs[k % 2], cs_t[k % 2]
                sg, sg_t = stage[(k // 4) % 2], stage_t[(k // 4) % 2]
                pos0 = (k % 16) * 128
                sc.dma("sp", x[:, 0:1024], p_ap[r0:r0 + 128, 0:1024], outs=[x_t], ins=[p_t])
                sc.dma("sp", x[:, 1024:2560], p_ap[r0:r0 + 128, 1536:3072], accs=[x_t], ins=[p_t])
                sc.dma("sp", c_[:], C["rope"][pos0:pos0 + 128, :], outs=[c_t])
                for (c0, G, w, gt, gt_t, cso) in ((0, 16, 64, gA, gA_t, 0), (1024, 12, 128, gB, gB_t, 64)):
                    n = G * w
                    hw = w // 2
                    xv = x[:, c0:c0 + n].rearrange("p (g w) -> p g w", w=w)
                    tv = tmp[:, c0:c0 + n].rearrange("p (g w) -> p g w", w=w)
                    ov = xo[:, c0:c0 + n].rearrange("p (g w) -> p g w", w=w)
                    ss = st4[:, 0, 0:G]
                    rs = st4[:, 1, 0:G]
                    sc.op("dve", lambda: V.tensor_tensor(out=tv, in0=xv, in1=xv, op=ALU.mult), outs=[tmp_t], ins=[x_t])
                    sc.op("dve", lambda: V.reduce_sum(out=ss, in_=tv, axis=AX.X), outs=[st4_t], ins=[tmp_t])
                    sc.op("dve", lambda: V.tensor_scalar(out=rs, in0=ss, scalar1=1.0 / w, scalar2=EPS, op0=ALU.mult, op1=ALU.add),
                          outs=[st4_t], ins=[st4_t])
                    sc.op("act", lambda: nc.scalar.activation(out=ss, in_=rs, func=AF.Sqrt), outs=[st4_t], ins=[st4_t])
                    sc.op("dve", lambda: V.reciprocal(out=rs, in_=ss), outs=[st4_t], ins=[st4_t])
                    sc.op("dve", lambda: V.tensor_tensor(out=tv, in0=xv, in1=rs.unsqueeze(2).to_broadcast([128, G, w]), op=ALU.mult),
                          outs=[tmp_t], ins=[x_t, st4_t])
                    for qk in range(2):
                        g0 = qk * (G // 2)
                        gsl = tmp[:, c0 + g0 * w:c0 + (g0 + G // 2) * w].rearrange("p (g w) -> p g w", w=w)
                        gb = gt[:, qk, :].unsqueeze(1).to_broadcast([128, G // 2, w])
                        sc.op("dve", lambda gsl=gsl, gb=gb: V.tensor_tensor(out=gsl, in0=gsl, in1=gb, op=ALU.mult),
                              outs=[tmp_t], ins=[tmp_t, gt_t])
                    cosb = c_[:, cso:cso + hw].unsqueeze(1).to_broadcast([128, G, hw])
                    sinb = c_[:, cso + hw:cso + 2 * hw].unsqueeze(1).to_broadcast([128, G, hw])
                    x1, x2 = tv[:, :, 0:hw], tv[:, :, hw:w]
                    a1, a2 = xv[:, :, 0:hw], xv[:, :, hw:w]
                    sc.op("dve", lambda: V.tensor_tensor(out=a1, in0=x1, in1=cosb, op=ALU.mult), outs=[x_t], ins=[tmp_t, c_t])
                    sc.op("dve", lambda: V.tensor_tensor(out=a2, in0=x2, in1=sinb, op=ALU.mult), outs=[x_t], ins=[tmp_t, c_t])
                    sc.op("dve", lambda: V.tensor_tensor(out=ov[:, :, 0:hw], in0=a1, in1=a2, op=ALU.subtract), outs=[xo_t], ins=[x_t])
                    sc.op("dve", lambda: V.tensor_tensor(out=a1, in0=x2, in1=cosb, op=ALU.mult), outs=[x_t], ins=[tmp_t, c_t])
                    sc.op("dve", lambda: V.tensor_tensor(out=a2, in0=x1, in1=sinb, op=ALU.mult), outs=[x_t], ins=[tmp_t, c_t])
                    sc.op("dve", lambda: V.tensor_tensor(out=ov[:, :, hw:w], in0=a1, in1=a2, op=ALU.add), outs=[xo_t], ins=[x_t])
                for g5 in range(5):
                    pt, pt_t = psT[pi % 4], psT_t[pi % 4]
                    pi += 1
                    for j in range(4):
                        blk = g5 * 4 + j
                        sc.op("pe", lambda blk=blk, j=j, pt=pt: nc.tensor.transpose(pt[:, j, :], xo[:, blk * 128:(blk + 1) * 128], ident_b[:]),
                              outs=[pt_t], ins=[xo_t, identb_t], mark=(j == 3))
                    tcol = (k % 4) * 128
                    if g5 % 2 == 0:
                        sc.op("act", lambda pt=pt, g5=g5, sg=sg: nc.scalar.copy(out=sg[:, g5 * 4:g5 * 4 + 4, tcol:tcol + 128], in_=pt[:]),
                              outs=[sg_t], ins=[pt_t])
                    else:
                        sc.op("dve", lambda pt=pt, g5=g5, sg=sg: V.tensor_copy(out=sg[:, g5 * 4:g5 * 4 + 4, tcol:tcol + 128], in_=pt[:]),
                              outs=[sg_t], ins=[pt_t])
                if k % 4 == 3:
                    t0 = (k // 4) * 512
                    sc.dma("sp", qkT[:, :, t0:t0 + 512].rearrange("b p t -> p b t"), sg[:], accs=[qkT_t], ins=[sg_t])
            self.phase_end(sc, xin_t + [tmp_t, xo_t, st4_t, gA_t, gB_t] + cs_t + stage_t + psT_t)

    def phase_attn(self, sc, l, p_ap, p_t, qkT, qkT_t, mix_ap, mix_t, C):
        nc = self.nc
        I = self.I
        V = nc.vector
        lam_init = 0.8 - 0.6 * math.exp(-0.3 * l)
        with ExitStack() as st:
            qT = [self.sb(st, [128, S], BF16, "qT") for _ in range(2)]
            qT_t = [TT() for _ in range(2)]
            kT = [self.sb(st, [128, S], BF16, "kT") for _ in range(2)]
            kT_t = [TT() for _ in range(2)]
            vt = [self.sb(st, [128, 16, 130], BF16, "vt") for _ in range(2)]
            vt_t = [TT() for _ in range(2)]
            mA = self.sb(st, [128, 7, 128], BF16, "mA"); mA_t = TT()
            mB = self.sb(st, [128, 19, 128], BF16, "mB"); mB_t = TT()
            pe_ = [self.sb(st, [128, 512], BF16, "pexp") for _ in range(4)]
            pe_t = [TT() for _ in range(4)]
            si = 0
            ei = 0
            o0 = self.sb(st, [128, 16, 128], F32, "o0"); o0_t = TT()
            ob = [self.sb(st, [128, 16, 128], F32, "ob") for _ in range(2)]
            ob_t = [TT() for _ in range(2)]
            sq = self.sb(st, [128, 16, 128], F32, "sq"); sq_t = TT()
            lamt = self.sb(st, [128, 4, 64], F32, "lamt"); lam_t = TT()
            lams = self.sb(st, [128, 8], F32, "lams"); lams_t = TT()
            gO = self.sb(st, [128, 128], F32, "gO"); gO_t = TT()
            rc = self.sb(st, [128, 8], F32, "rc"); rc_t = TT()
            nst = self.sb(st, [128, 3, 16], F32, "nst"); nst_t = TT()
            psS = [self.ps(st, [128, 512], F32, "psS") for _ in range(4)]
            psS_t = [PT() for _ in range(4)]
            psO = [self.ps(st, [128, 512], F32, "psO") for _ in range(4)]
            psO_t = [PT() for _ in range(4)]
            sc.dma("sp", mA[:], C["maskA"][:, :, :], outs=[mA_t])
            sc.dma("sp", mB[:], C["maskB"][:, :, :], outs=[mB_t])
            for i in range(2):
                sc.op("dve", lambda i=i: V.memset(vt[i][:, :, 128:130], 1.0), outs=[vt_t[i]])
            sc.dma("sp", lamt[:].rearrange("p a b -> p (a b)"), I["da_lambda"][l].rearrange("a b -> (a b)").partition_broadcast(128),
                   outs=[lam_t])
            self.bcast_load(sc, gO[:], gO_t, I["da_out_norm"][l])
            sc.op("dve", lambda: V.tensor_tensor(out=lamt[:, 0, :], in0=lamt[:, 0, :], in1=lamt[:, 1, :], op=ALU.mult), outs=[lam_t], ins=[lam_t])
            sc.op("dve", lambda: V.tensor_tensor(out=lamt[:, 2, :], in0=lamt[:, 2, :], in1=lamt[:, 3, :], op=ALU.mult), outs=[lam_t], ins=[lam_t])
            sc.op("dve", lambda: V.reduce_sum(out=lams[:, 0:1], in_=lamt[:, 0, :], axis=AX.X), outs=[lams_t], ins=[lam_t])
            sc.op("dve", lambda: V.reduce_sum(out=lams[:, 1:2], in_=lamt[:, 2, :], axis=AX.X), outs=[lams_t], ins=[lam_t])
            sc.op("act", lambda: nc.scalar.activation(out=lams[:, 2:4], in_=lams[:, 0:2], func=AF.Exp), outs=[lams_t], ins=[lams_t])
            sc.op("dve", lambda: V.tensor_tensor(out=lams[:, 4:5], in0=lams[:, 3:4], in1=lams[:, 2:3], op=ALU.subtract), outs=[lams_t], ins=[lams_t])
            sc.op("dve", lambda: V.tensor_scalar(out=lams[:, 4:5], in0=lams[:, 4:5], scalar1=-lam_init, scalar2=None, op0=ALU.add),
                  outs=[lams_t], ins=[lams_t])
            units = []
            for b in range(self.NB):
                for h in range(4):
                    units.append(("A", b, h))
                for h in range(6):
                    units.append(("B", b, h))
            si = 0
            ei = 0
            for ui, (kind, b, h) in enumerate(units):
                q, q_t = qT[ui % 2], qT_t[ui % 2]
                k_, k_t = kT[ui % 2], kT_t[ui % 2]
                v, v_t = vt[ui % 2], vt_t[ui % 2]
                if kind == "A":
                    qblk, kblk, vcol, ocol, nsub, kd, scale = h, 4 + h, 1024 + h * 128, h * 128, 2, 64, 64 ** -0.5
                    msk, msk_t = mA, mA_t
                else:
                    qblk, kblk, vcol, ocol, nsub, kd, scale = 8 + h, 14 + h, 3072 + h * 128, 512 + h * 128, 1, 128, 128 ** -0.5
                    msk, msk_t = mB, mB_t
                t0 = b * S
                sc.dma("sp", q[:], qkT[qblk, :, t0:t0 + S], outs=[q_t], ins=[qkT_t])
                sc.dma("sp", k_[:], qkT[kblk, :, t0:t0 + S], outs=[k_t], ins=[qkT_t])
                sc.dma("pool", v[:, :, 0:128], p_ap[t0:t0 + S, vcol:vcol + 128].rearrange("(n p) c -> p n c", p=128),
                       accs=[v_t], ins=[p_t])
                o_out, o_out_t = ob[ui % 2], ob_t[ui % 2]
                for c in range(nsub):
                    pb = c * kd if nsub == 2 else 0
                    pairs = [(qb, kb) for qb in range(4) for kb in range(4 * qb + 4)]

                    def emit_score(qb, kb):
                        nonlocal si, ei
                        ps_, ps_t = psS[si % 4], psS_t[si % 4]
                        si += 1
                        sc.op("pe", lambda: nc.tensor.matmul(
                            ps_[:], lhsT=k_[pb:pb + kd, kb * 128:(kb + 1) * 128], rhs=q[pb:pb + kd, qb * 512:(qb + 1) * 512],
                            start=True, stop=True), outs=[ps_t], ins=[k_t, q_t])
                        e_, e_t = pe_[ei % 4], pe_t[ei % 4]
                        ei += 1
                        sc.op("act", lambda: nc.scalar.activation(out=e_[:], in_=ps_[:], func=AF.Exp, scale=scale),
                              outs=[e_t], ins=[ps_t])
                        d0 = 4 * qb - kb
                        if kind == "B" or d0 <= 0:
                            mi = d0 + 3
                            mv = msk[:, mi:mi + 4, :].rearrange("p a b -> p (a b)")
                            sc.op("pool", lambda: nc.gpsimd.tensor_tensor(out=e_[:], in0=e_[:], in1=mv, op=ALU.mult),
                                  outs=[e_t], ins=[e_t, msk_t])
                        return e_, e_t

                    queue = [emit_score(*pairs[0]), emit_score(*pairs[1])]
                    for pi_, (qb, kb) in enumerate(pairs):
                        e_, e_t = queue.pop(0)
                        if pi_ + 2 < len(pairs):
                            queue.append(emit_score(*pairs[pi_ + 2]))
                        for s_ in range(4):
                            if 4 * qb + s_ < kb:
                                continue
                            last = (kb == 4 * qb + s_)
                            sc.op("pe", lambda s_=s_, e_=e_, kb=kb: nc.tensor.matmul(
                                psO[s_][:, 0:129], lhsT=e_[:, s_ * 128:(s_ + 1) * 128], rhs=v[:, kb, 0:129],
                                start=(kb == 0), stop=last), outs=[psO_t[s_]], ins=[e_t, v_t])
                        if kb != 4 * qb + 3:
                            continue
                        for s_ in range(4):
                            qi = qb * 4 + s_
                            sc.op("dve", lambda s_=s_: V.reciprocal(out=rc[:, s_:s_ + 1], in_=psO[s_][:, 128:129]),
                                  outs=[rc_t], ins=[psO_t[s_]])
                            if kind == "B":
                                sc.op("dve", lambda s_=s_, qi=qi: V.tensor_scalar(out=o_out[:, qi, :], in0=psO[s_][:, 0:128],
                                                                                scalar1=rc[:, s_:s_ + 1], scalar2=None, op0=ALU.mult),
                                      outs=[o_out_t], ins=[psO_t[s_], rc_t])
                            elif c == 0:
                                sc.op("dve", lambda s_=s_, qi=qi: V.tensor_scalar(out=o0[:, qi, :], in0=psO[s_][:, 0:128],
                                                                                scalar1=rc[:, s_:s_ + 1], scalar2=None, op0=ALU.mult),
                                      outs=[o0_t], ins=[psO_t[s_], rc_t])
                            else:
                                sc.op("dve", lambda s_=s_: V.tensor_tensor(out=rc[:, 4 + s_:5 + s_], in0=rc[:, s_:s_ + 1], in1=lams[:, 4:5], op=ALU.mult),
                                      outs=[rc_t], ins=[rc_t, lams_t])
                                sc.op("dve", lambda s_=s_, qi=qi: V.scalar_tensor_tensor(
                                    out=o_out[:, qi, :], in0=psO[s_][:, 0:128], scalar=rc[:, 4 + s_:5 + s_], in1=o0[:, qi, :],
                                    op0=ALU.mult, op1=ALU.add), outs=[o_out_t], ins=[psO_t[s_], rc_t, o0_t])
                if kind == "A":
                    sc.op("dve", lambda: V.tensor_tensor(out=sq[:], in0=o_out[:], in1=o_out[:], op=ALU.mult), outs=[sq_t], ins=[o_out_t])
                    sc.op("dve", lambda: V.reduce_sum(out=nst[:, 0, :], in_=sq[:], axis=AX.X), outs=[nst_t], ins=[sq_t])
                    sc.op("dve", lambda: V.tensor_scalar(out=nst[:, 1, :], in0=nst[:, 0, :], scalar1=1.0 / 128, scalar2=EPS,
                                                          op0=ALU.mult, op1=ALU.add), outs=[nst_t], ins=[nst_t])
                    sc.op("act", lambda: nc.scalar.activation(out=nst[:, 0, :], in_=nst[:, 1, :], func=AF.Sqrt), outs=[nst_t], ins=[nst_t])
                    sc.op("dve", lambda: V.reciprocal(out=nst[:, 1, :], in_=nst[:, 0, :]), outs=[nst_t], ins=[nst_t])
                    sc.op("dve", lambda: V.scalar_tensor_tensor(out=o_out[:], in0=o_out[:], scalar=1.0 - lam_init,
                                                                 in1=nst[:, 1, :].unsqueeze(2).to_broadcast([128, 16, 128]),
                                                                 op0=ALU.mult, op1=ALU.mult), outs=[o_out_t], ins=[o_out_t, nst_t])
                    sc.op("dve", lambda: V.tensor_tensor(out=o_out[:], in0=o_out[:], in1=gO[:].unsqueeze(1).to_broadcast([128, 16, 128]),
                                                          op=ALU.mult), outs=[o_out_t], ins=[o_out_t, gO_t])
                sc.dma("sp", mix_ap[t0:t0 + S, ocol:ocol + 128].rearrange("(n p) c -> p n c", p=128), o_out[:],
                       accs=[mix_t], ins=[o_out_t])
            self.phase_end(sc, qT_t + kT_t + vt_t + [mA_t, mB_t, o0_t, sq_t, lam_t, lams_t, gO_t, rc_t, nst_t] + pe_t + ob_t + psS_t + psO_t)

    def phase_rwkv(self, sc, l, p_ap, p_t, mix_ap, mix_t, C, ident, ident_t, ident_b, identb_t):
        nc = self.nc
        I = self.I
        V = nc.vector
        A = nc.scalar
        H = 12
        NEG = -math.exp(-0.5)
        with ExitStack() as st:
            def T_(shape, dt, name):
                return self.sb(st, shape, dt, name), TT()
            mu, mu_t = T_([128, RW_IN], F32, "mu")
            pv, pv_t = T_([128, 7, 768], F32, "pv")
            w2f, w2f_t = T_([64, 768], F32, "w2f")
            w2h, w2_t = T_([64, 2, 768], BF16, "w2h")
            a2b, a2_t = T_([64, 768], BF16, "a2b")
            g2b, g2_t = T_([128, 768], BF16, "g2b")
            triU, tri_t = T_([128, 128], BF16, "triU")
            ones, ones_t = T_([128, 128], BF16, "ones")
            mU, mU_t = T_([128, 256], BF16, "mU")
            mL, mL_t = T_([128, 128], BF16, "mL")
            xr, xr_t = T_([128, RW_IN], F32, "xr")
            pr, pr_t = T_([128, RW_IN], F32, "pr")
            lr, lr_t = T_([128, 256], F32, "lr")
            lrT, lrT_t = T_([128, 4, 128], BF16, "lrT")
            lw, lw_t = T_([128, 768], F32, "lw")
            lwb, lwb_t = T_([128, 2, 768], BF16, "lwb")
            asg, asg_t = T_([128, 768], F32, "asg")
            gg, gg_t = T_([128, 768], F32, "gg")
            kk, kk_t = T_([128, 768], F32, "kk")
            km, km_t = T_([128, 768], F32, "km")
            bb, bb_t = T_([128, 768], F32, "bb")
            cum, cum_t = T_([128, 768], F32, "cum")
            t1, t1_t = T_([128, 768], F32, "t1")
            t2, t2_t = T_([128, 768], F32, "t2")
            ex, ex_t = T_([128, 4, 768], F32, "ex")
            s12, s12_t = T_([128, 4, 12], F32, "s12")
            tb, tb_t = T_([128, 4, 768], BF16, "tb")
            hb, hb_t = T_([128, 3, 768], BF16, "hb")
            fm, fm_t = T_([64, H, 4, 128], BF16, "fm")
            gam, gam_t = T_([64, H], F32, "gam")
            Gb, Gb_t = T_([128, H, 256], BF16, "Gb")
            Gk, Gk_t = T_([128, H, 256], BF16, "Gk")
            Pm = [T_([128, H, 128], BF16, "Pm") for _ in range(2)]
            Qm = [T_([128, H, 128], BF16, "Qm") for _ in range(2)]
            Tm = [T_([128, H, 128], BF16, "Tm") for _ in range(2)]
            W0, W0_t = T_([128, H, 64], BF16, "W0")
            U, U_t = T_([128, H, 64], BF16, "U")
            Y, Y_t = T_([128, 768], F32, "Y")
            Sf, Sf_t = T_([64, H, 64], F32, "Sf")
            Sb, Sb_t = T_([64, H, 64], BF16, "Sb")
            ps = [self.ps(st, [128, 512], F32, "psR") for _ in range(6)]
            ps_t = [PT() for _ in range(6)]
            psb = [self.ps(st, [128, 8, 128], BF16, "psRb") for _ in range(2)]
            psb_t = [PT() for _ in range(2)]
            pi = [0]

            def bank():
                i = pi[0] % 6
                pi[0] += 1
                return ps[i], ps_t[i]

            def dve(fn, outs, ins):
                sc.op("dve", fn, outs=outs, ins=ins)

            def act(fn, outs, ins):
                sc.op("act", fn, outs=outs, ins=ins)

            def tt(out, a, b, op, outs, ins):
                dve(lambda: V.tensor_tensor(out=out, in0=a, in1=b, op=op), outs, ins)

            h3 = lambda ap: ap.rearrange("p (h d) -> p h d", d=64)
            self.bcast_load(sc, mu[:], mu_t, I["rw_mu"][l])
            for i, nm in enumerate(("rw_w0", "rw_a0", "rw_k_k", "rw_k_a")):
                self.bcast_load(sc, pv[:, i, :], pv_t, I[nm][l])
            self.bcast_load(sc, pv[:, 4, :], pv_t, I["rw_r_k"][l].rearrange("h d -> (h d)"))
            self.bcast_load(sc, pv[:, 5, :], pv_t, I["rw_ln_g"][l])
            self.bcast_load(sc, pv[:, 6, :], pv_t, I["rw_ln_b"][l])
            sc.dma("sp", w2f[:], I["rw_w2"][l], outs=[w2f_t])
            dve(lambda: V.tensor_copy(out=w2h[:, 0, :], in_=w2f[:]), [w2_t], [w2f_t])
            tt(w2h[:, 1, :], w2f[:], w2h[:, 0, :], ALU.subtract, [w2_t], [w2f_t, w2_t])
            sc.dma("pool", a2b[:], I["rw_a2"][l], outs=[a2_t])
            sc.dma("pool", g2b[:], I["rw_g2"][l], outs=[g2_t])
            sc.dma("sp", triU[:], C["triU"][:, :], outs=[tri_t])
            sc.dma("sp", ones[:], C["ones_b"][:, :], outs=[ones_t])
            sc.dma("sp", mU[:], C["maskU2"][:, :], outs=[mU_t])
            sc.dma("sp", mL[:], C["maskL"][:, :], outs=[mL_t])

            n_pre = -(-len(getattr(self, "pending_precast", [])) // self.NT)
            for k in range(self.NT):
                r0 = k * 128
                first = (k % 16 == 0)
                self.drain_precast(n_pre)
                sc.dma("sp", xr[:], p_ap[r0:r0 + 128, ATT_IN:D_IN], outs=[xr_t], ins=[p_t])
                if first:
                    dve(lambda: V.memset(pr[0:1, :], 0.0), [pr_t], [])
                    sc.dma("sp", pr[1:128, :], p_ap[r0:r0 + 127, ATT_IN:D_IN], accs=[pr_t], ins=[p_t])
                    dve(lambda: V.memset(Sf[:], 0.0), [Sf_t], [])
                    dve(lambda: V.memset(Sb[:], 0.0), [Sb_t], [])
                else:
                    sc.dma("sp", pr[:], p_ap[r0 - 1:r0 + 127, ATT_IN:D_IN], outs=[pr_t], ins=[p_t])
                tt(pr[:], pr[:], xr[:], ALU.subtract, [pr_t], [pr_t, xr_t])
                tt(pr[:], pr[:], mu[:], ALU.mult, [pr_t], [pr_t, mu_t])
                tt(xr[:], xr[:], pr[:], ALU.add, [xr_t], [xr_t, pr_t])
                xr_r, xr_k, xr_v = xr[:, 0:768], xr[:, 768:1536], xr[:, 1536:2304]
                act(lambda: A.activation(out=lr[:, 0:64], in_=xr[:, 2304:2368], func=AF.Tanh), [lr_t], [xr_t])
                act(lambda: A.copy(out=lr[:, 64:128], in_=xr[:, 2368:2432]), [lr_t], [xr_t])
                act(lambda: A.activation(out=lr[:, 128:256], in_=xr[:, 2432:2560], func=AF.Sigmoid), [lr_t], [xr_t])
                pz, pz_t = bank()
                pzv = pz[:].rearrange("p (a b) -> p a b", b=128)
                sc.op("pe", lambda: nc.tensor.transpose(pzv[0:64, 0, :], lr[:, 0:64], ident[:]), outs=[pz_t], ins=[lr_t, ident_t], mark=False)
                sc.op("pe", lambda: nc.tensor.transpose(pzv[0:64, 1, :], lr[:, 64:128], ident[:]), outs=[pz_t], ins=[lr_t, ident_t], mark=False)
                sc.op("pe", lambda: nc.tensor.transpose(pzv[:, 2, :], lr[:, 128:256], ident[:]), outs=[pz_t], ins=[lr_t, ident_t])
                dve(lambda: V.tensor_copy(out=lrT[0:64, 0, :], in_=pzv[0:64, 0, :]), [lrT_t], [pz_t])
                tt(lrT[0:64, 1, :], pzv[0:64, 0, :], lrT[0:64, 0, :], ALU.subtract, [lrT_t], [pz_t, lrT_t])
                dve(lambda: V.tensor_copy(out=lrT[0:64, 2, :], in_=pzv[0:64, 1, :]), [lrT_t], [pz_t])
                dve(lambda: V.tensor_copy(out=lrT[:, 3, :], in_=pzv[:, 2, :]), [lrT_t], [pz_t])
                for (c0, c1) in ((0, 512), (512, 768)):
                    n = c1 - c0
                    pb_, pb_t = bank()
                    for i, (li, wi) in enumerate(((0, 0), (1, 0), (0, 1))):
                        sc.op("pe", lambda li=li, wi=wi, i=i, pb_=pb_: nc.tensor.matmul(
                            pb_[:, 0:n], lhsT=lrT[0:64, li, :], rhs=w2h[:, wi, c0:c1], start=(i == 0), stop=(i == 2)),
                            outs=[pb_t], ins=[lrT_t, w2_t], mark=(i == 2))
                    tt(lw[:, c0:c1], pb_[:, 0:n], pv[:, 0, c0:c1], ALU.add, [lw_t], [pb_t, pv_t])
                    pb_, pb_t = bank()
                    sc.op("pe", lambda pb_=pb_: nc.tensor.matmul(pb_[:, 0:n], lhsT=lrT[0:64, 2, :], rhs=a2b[:, c0:c1], start=True, stop=True),
                          outs=[pb_t], ins=[lrT_t, a2_t])
                    tt(asg[:, c0:c1], pb_[:, 0:n], pv[:, 1, c0:c1], ALU.add, [asg_t], [pb_t, pv_t])
                    pb_, pb_t = bank()
                    sc.op("pe", lambda pb_=pb_: nc.tensor.matmul(pb_[:, 0:n], lhsT=lrT[:, 3, :], rhs=g2b[:, c0:c1], start=True, stop=True),
                          outs=[pb_t], ins=[lrT_t, g2_t])
                    act(lambda pb_=pb_: A.copy(out=gg[:, c0:c1], in_=pb_[:, 0:n]), [gg_t], [pb_t])
                act(lambda: A.activation(out=lw[:], in_=lw[:], func=AF.Sigmoid), [lw_t], [lw_t])
                act(lambda: A.activation(out=asg[:], in_=asg[:], func=AF.Sigmoid), [asg_t], [asg_t])
                dve(lambda: V.tensor_scalar(out=lw[:], in0=lw[:], scalar1=NEG, scalar2=None, op0=ALU.mult), [lw_t], [lw_t])
                tt(kk[:], xr_k, pv[:, 2, :], ALU.mult, [kk_t], [xr_t, pv_t])
                tt(t1[:], kk[:], kk[:], ALU.mult, [t1_t], [kk_t])
                dve(lambda: V.reduce_sum(out=s12[:, 0, :], in_=h3(t1[:]), axis=AX.X), [s12_t], [t1_t])
                act(lambda: A.activation(out=s12[:, 1, :], in_=s12[:, 0, :], func=AF.Sqrt), [s12_t], [s12_t])
                dve(lambda: V.tensor_scalar(out=s12[:, 1, :], in0=s12[:, 1, :], scalar1=1e-12, scalar2=None, op0=ALU.max), [s12_t], [s12_t])
                dve(lambda: V.reciprocal(out=s12[:, 0, :], in_=s12[:, 1, :]), [s12_t], [s12_t])
                tt(h3(kk[:]), h3(kk[:]), s12[:, 0, :].unsqueeze(2).to_broadcast([128, H, 64]), ALU.mult, [kk_t], [kk_t, s12_t])
                dve(lambda: V.scalar_tensor_tensor(out=t1[:], in0=asg[:], scalar=-1.0, in1=pv[:, 3, :], op0=ALU.add, op1=ALU.mult),
                    [t1_t], [asg_t, pv_t])
                dve(lambda: V.scalar_tensor_tensor(out=km[:], in0=t1[:], scalar=1.0, in1=xr_k, op0=ALU.add, op1=ALU.mult),
                    [km_t], [t1_t, xr_t])
                tt(bb[:], kk[:], asg[:], ALU.mult, [bb_t], [kk_t, asg_t])
                dve(lambda: V.tensor_copy(out=lwb[:, 0, :], in_=lw[:]), [lwb_t], [lw_t])
                tt(lwb[:, 1, :], lw[:], lwb[:, 0, :], ALU.subtract, [lwb_t], [lw_t, lwb_t])
                for (c0, c1) in ((0, 512), (512, 768)):
                    n = c1 - c0
                    pc, pc_t = bank()
                    for i in range(2):
                        sc.op("pe", lambda i=i, pc=pc: nc.tensor.matmul(pc[:, 0:n], lhsT=triU[:], rhs=lwb[:, i, c0:c1], start=(i == 0), stop=(i == 1)),
                              outs=[pc_t], ins=[tri_t, lwb_t], mark=(i == 1))
                    act(lambda pc=pc: A.copy(out=cum[:, c0:c1], in_=pc[:, 0:n]), [cum_t], [pc_t])
                    pc, pc_t = bank()
                    for i in range(2):
                        sc.op("pe", lambda i=i, pc=pc: nc.tensor.matmul(pc[:, 0:n], lhsT=ones[:], rhs=lwb[:, i, c0:c1], start=(i == 0), stop=(i == 1)),
                              outs=[pc_t], ins=[ones_t, lwb_t], mark=(i == 1))
                    tt(t2[:, c0:c1], pc[:, 0:n], cum[:, c0:c1], ALU.subtract, [t2_t], [pc_t, cum_t])
                tt(t1[:], cum[:], lw[:], ALU.subtract, [t1_t], [cum_t, lw_t])
                act(lambda: A.activation(out=ex[:, 0, :], in_=cum[:], func=AF.Exp), [ex_t], [cum_t])
                act(lambda: A.activation(out=ex[:, 1, :], in_=cum[:], func=AF.Exp, scale=-1.0), [ex_t], [cum_t])
                act(lambda: A.activation(out=ex[:, 2, :], in_=t1[:], func=AF.Exp), [ex_t], [t1_t])
                act(lambda: A.activation(out=ex[:, 3, :], in_=t2[:], func=AF.Exp), [ex_t], [t2_t])
                pg, pg_t = bank()
                for h in range(H):
                    for i in range(2):
                        sc.op("pe", lambda h=h, i=i, pg=pg: nc.tensor.matmul(pg[0:64, h:h + 1], lhsT=lwb[:, i, h * 64:(h + 1) * 64], rhs=ones[:, 0:1],
                                                                             start=(i == 0), stop=(i == 1)),
                              outs=[pg_t], ins=[lwb_t, ones_t], mark=(h == H - 1 and i == 1))
                act(lambda pg=pg: A.activation(out=gam[:], in_=pg[0:64, 0:H], func=AF.Exp), [gam_t], [pg_t])
                dve(lambda: V.scalar_tensor_tensor(out=tb[:, 0, :], in0=kk[:], scalar=-1.0, in1=ex[:, 2, :], op0=ALU.mult, op1=ALU.mult),
                    [tb_t], [kk_t, ex_t])
                tt(tb[:, 1, :], xr_r, ex[:, 0, :], ALU.mult, [tb_t], [xr_t, ex_t])
                tt(tb[:, 2, :], bb[:], ex[:, 1, :], ALU.mult, [tb_t], [bb_t, ex_t])
                tt(tb[:, 3, :], km[:], ex[:, 1, :], ALU.mult, [tb_t], [km_t, ex_t])
                dve(lambda: V.tensor_copy(out=hb[:, 0, :], in_=xr_v), [hb_t], [xr_t])
                tt(hb[:, 1, :], bb[:], ex[:, 3, :], ALU.mult, [hb_t], [bb_t, ex_t])
                tt(hb[:, 2, :], km[:], ex[:, 3, :], ALU.mult, [hb_t], [km_t, ex_t])
                for h in range(H):
                    pq, pq_t = psb[h % 2], psb_t[h % 2]
                    for a_ in range(4):
                        sc.op("pe", lambda h=h, a_=a_, pq=pq: nc.tensor.transpose(pq[0:64, a_, :], tb[:, a_, h * 64:(h + 1) * 64], ident_b[:]),
                              outs=[pq_t], ins=[tb_t, identb_t], mark=(a_ == 3))
                    if h % 2 == 0:
                        dve(lambda h=h, pq=pq: V.tensor_copy(out=fm[:, h, :, :], in_=pq[0:64, 0:4, :]), [fm_t], [pq_t])
                    else:
                        act(lambda h=h, pq=pq: A.copy(out=fm[:, h, :, :], in_=pq[0:64, 0:4, :]), [fm_t], [pq_t])
                for h in range(H):
                    pa, pa_t = bank()
                    sc.op("pe", lambda h=h, pa=pa: nc.tensor.matmul(pa[:, 0:256], lhsT=fm[:, h, 2, :], rhs=fm[:, h, 0:2, :].rearrange("p a t -> p (a t)"),
                                                                    start=True, stop=True), outs=[pa_t], ins=[fm_t])
                    tt(Gb[:, h, :], pa[:, 0:256], mU[:], ALU.mult, [Gb_t], [pa_t, mU_t])
                    pa2, pa2_t = bank()
                    sc.op("pe", lambda h=h, pa2=pa2: nc.tensor.matmul(pa2[:, 0:256], lhsT=fm[:, h, 3, :], rhs=fm[:, h, 0:2, :].rearrange("p a t -> p (a t)"),
                                                                      start=True, stop=True), outs=[pa2_t], ins=[fm_t])
                    tt(Gk[:, h, :], pa2[:, 0:256], mU[:], ALU.mult, [Gk_t], [pa2_t, mU_t])
                    pa3, pa3_t = bank()
                    sc.op("pe", lambda h=h, pa3=pa3: nc.tensor.matmul(pa3[:, 0:128], lhsT=fm[:, h, 0, :], rhs=fm[:, h, 2, :], start=True, stop=True),
                          outs=[pa3_t], ins=[fm_t])
                    tt(Qm[0][0][:, h, :], pa3[:, 0:128], mL[:], ALU.mult, [Qm[0][1]], [pa3_t, mL_t])
                dve(lambda: V.tensor_copy(out=Pm[0][0][:], in_=Gb[:, :, 0:128]), [Pm[0][1]], [Gb_t])
                tt(Tm[0][0][:], Gb[:, :, 0:128], ident_b[:].unsqueeze(1).to_broadcast([128, H, 128]), ALU.add, [Tm[0][1]], [Gb_t, identb_t])
                cp, cq, ct = 0, 0, 0
                for m in range(1, 7):
                    (P_, P_t), (Q_, Q_t), (Tc, Tc_t) = Pm[cp], Qm[cq], Tm[ct]
                    (Pn, Pn_t), (Qn, Qn_t), (Tn, Tn_t) = Pm[1 - cp], Qm[1 - cq], Tm[1 - ct]
                    for hg in range(3):
                        hs = range(hg * 4, hg * 4 + 4)
                        pqn, pqn_t = bank()
                        for j, h in enumerate(hs):
                            sc.op("pe", lambda h=h, j=j, pqn=pqn: nc.tensor.matmul(pqn[:, j * 128:(j + 1) * 128], lhsT=P_[:, h, :], rhs=Q_[:, h, :],
                                                                                 start=True, stop=True), outs=[pqn_t], ins=[P_t, Q_t], mark=(j == 3))
                        dve(lambda hg=hg, pqn=pqn: V.tensor_copy(out=Qn[:, hg * 4:hg * 4 + 4, :], in_=pqn[:].rearrange("p (a b) -> p a b", b=128)),
                            [Qn_t], [pqn_t])
                        if m < 6:
                            ppn, ppn_t = bank()
                            for j, h in enumerate(hs):
                                sc.op("pe", lambda h=h, j=j, ppn=ppn: nc.tensor.matmul(ppn[:, j * 128:(j + 1) * 128], lhsT=Q_[:, h, :], rhs=P_[:, h, :],
                                                                                     start=True, stop=True), outs=[ppn_t], ins=[P_t, Q_t], mark=(j == 3))
                            act(lambda hg=hg, ppn=ppn: A.copy(out=Pn[:, hg * 4:hg * 4 + 4, :], in_=ppn[:].rearrange("p (a b) -> p a b", b=128)),
                                [Pn_t], [ppn_t])
                        ptn, ptn_t = bank()
                        for j, h in enumerate(hs):
                            sc.op("pe", lambda h=h, j=j, ptn=ptn: nc.tensor.matmul(ptn[:, j * 128:(j + 1) * 128], lhsT=Qn[:, h, :], rhs=Tc[:, h, :],
                                                                                 start=True, stop=True), outs=[ptn_t], ins=[Qn_t, Tc_t], mark=(j == 3))
                        tt(Tn[:, hg * 4:hg * 4 + 4, :], ptn[:].rearrange("p (a b) -> p a b", b=128), Tc[:, hg * 4:hg * 4 + 4, :], ALU.add,
                           [Tn_t], [ptn_t, Tc_t])
                    cp, cq, ct = 1 - cp, 1 - cq, 1 - ct
                Ti, Ti_t = Tm[ct]
                for (h0, h1) in ((0, 8), (8, 12)):
                    pw, pw_t = bank()
                    for h in range(h0, h1):
                        o = pw[:, (h - h0) * 64:(h - h0 + 1) * 64]
                        sc.op("pe", lambda h=h, o=o: nc.tensor.matmul(o, lhsT=fm[:, h, 0, :], rhs=Sb[:, h, :], start=True, stop=False),
                              outs=[pw_t], ins=[fm_t, Sb_t], mark=False)
                        sc.op("pe", lambda h=h, o=o: nc.tensor.matmul(o, lhsT=Gk[:, h, 0:128], rhs=hb[:, 0, h * 64:(h + 1) * 64], start=False, stop=True),
                              outs=[pw_t], ins=[Gk_t, hb_t], mark=(h == h1 - 1))
                    dve(lambda pw=pw, h0=h0, h1=h1: V.tensor_copy(out=W0[:, h0:h1, :], in_=pw[:, 0:(h1 - h0) * 64].rearrange("p (a b) -> p a b", b=64)),
                        [W0_t], [pw_t])
                for (h0, h1) in ((0, 8), (8, 12)):
                    pu, pu_t = bank()
                    for h in range(h0, h1):
                        o = pu[:, (h - h0) * 64:(h - h0 + 1) * 64]
                        sc.op("pe", lambda h=h, o=o: nc.tensor.matmul(o, lhsT=Ti[:, h, :], rhs=W0[:, h, :], start=True, stop=True),
                              outs=[pu_t], ins=[Ti_t, W0_t], mark=(h == h1 - 1))
                    dve(lambda pu=pu, h0=h0, h1=h1: V.tensor_copy(out=U[:, h0:h1, :], in_=pu[:, 0:(h1 - h0) * 64].rearrange("p (a b) -> p a b", b=64)),
                        [U_t], [pu_t])
                for (h0, h1) in ((0, 8), (8, 12)):
                    py_, py_t = bank()
                    for h in range(h0, h1):
                        o = py_[:, (h - h0) * 64:(h - h0 + 1) * 64]
                        sc.op("pe", lambda h=h, o=o: nc.tensor.matmul(o, lhsT=fm[:, h, 1, :], rhs=Sb[:, h, :], start=True, stop=False),
                              outs=[py_t], ins=[fm_t, Sb_t], mark=False)
                        sc.op("pe", lambda h=h, o=o: nc.tensor.matmul(o, lhsT=Gb[:, h, 128:256], rhs=U[:, h, :], start=False, stop=False),
                              outs=[py_t], ins=[Gb_t, U_t], mark=False)
                        sc.op("pe", lambda h=h, o=o: nc.tensor.matmul(o, lhsT=Gk[:, h, 128:256], rhs=hb[:, 0, h * 64:(h + 1) * 64], start=False, stop=True),
                              outs=[py_t], ins=[Gk_t, hb_t], mark=(h == h1 - 1))
                    act(lambda py_=py_, h0=h0, h1=h1: A.copy(out=Y[:, h0 * 64:h1 * 64], in_=py_[:, 0:(h1 - h0) * 64]), [Y_t], [py_t])
                for (h0, h1) in ((0, 8), (8, 12)):
                    pn, pn_t = bank()
                    for h in range(h0, h1):
                        o = pn[0:64, (h - h0) * 64:(h - h0 + 1) * 64]
                        sc.op("pe", lambda h=h, o=o: nc.tensor.matmul(o, lhsT=hb[:, 1, h * 64:(h + 1) * 64], rhs=U[:, h, :], start=True, stop=False),
                              outs=[pn_t], ins=[hb_t, U_t], mark=False)
                        sc.op("pe", lambda h=h, o=o: nc.tensor.matmul(o, lhsT=hb[:, 2, h * 64:(h + 1) * 64], rhs=hb[:, 0, h * 64:(h + 1) * 64], start=False, stop=True),
                              outs=[pn_t], ins=[hb_t], mark=(h == h1 - 1))
                    tt(Sf[:, h0:h1, :], Sf[:, h0:h1, :], gam[:, h0:h1].unsqueeze(2).to_broadcast([64, h1 - h0, 64]), ALU.mult, [Sf_t], [Sf_t, gam_t])
                    tt(Sf[:, h0:h1, :], Sf[:, h0:h1, :], pn[0:64, 0:(h1 - h0) * 64].rearrange("p (a b) -> p a b", b=64), ALU.add, [Sf_t], [Sf_t, pn_t])
                dve(lambda: V.tensor_copy(out=Sb[:], in_=Sf[:]), [Sb_t], [Sf_t])
                Y3 = h3(Y[:])
                dve(lambda: V.reduce_sum(out=s12[:, 2, :], in_=Y3, axis=AX.X), [s12_t], [Y_t])
                dve(lambda: V.tensor_scalar(out=s12[:, 2, :], in0=s12[:, 2, :], scalar1=1.0 / 64, scalar2=None, op0=ALU.mult), [s12_t], [s12_t])
                tt(Y3, Y3, s12[:, 2, :].unsqueeze(2).to_broadcast([128, H, 64]), ALU.subtract, [Y_t], [Y_t, s12_t])
                tt(t1[:], Y[:], Y[:], ALU.mult, [t1_t], [Y_t])
                dve(lambda: V.reduce_sum(out=s12[:, 3, :], in_=h3(t1[:]), axis=AX.X), [s12_t], [t1_t])
                dve(lambda: V.tensor_scalar(out=s12[:, 3, :], in0=s12[:, 3, :], scalar1=1.0 / 64, scalar2=64e-5, op0=ALU.mult, op1=ALU.add),
                    [s12_t], [s12_t])
                act(lambda: A.activation(out=s12[:, 2, :], in_=s12[:, 3, :], func=AF.Sqrt), [s12_t], [s12_t])
                dve(lambda: V.reciprocal(out=s12[:, 3, :], in_=s12[:, 2, :]), [s12_t], [s12_t])
                tt(Y3, Y3, s12[:, 3, :].unsqueeze(2).to_broadcast([128, H, 64]), ALU.mult, [Y_t], [Y_t, s12_t])
                tt(Y[:], Y[:], pv[:, 5, :], ALU.mult, [Y_t], [Y_t, pv_t])
                tt(Y[:], Y[:], pv[:, 6, :], ALU.add, [Y_t], [Y_t, pv_t])
                tt(t1[:], xr_r, km[:], ALU.mult, [t1_t], [xr_t, km_t])
                tt(t1[:], t1[:], pv[:, 4, :], ALU.mult, [t1_t], [t1_t, pv_t])
                dve(lambda: V.reduce_sum(out=s12[:, 2, :], in_=h3(t1[:]), axis=AX.X), [s12_t], [t1_t])
                tt(h3(t1[:]), h3(xr_v), s12[:, 2, :].unsqueeze(2).to_broadcast([128, H, 64]), ALU.mult, [t1_t], [xr_t, s12_t])
                tt(Y[:], Y[:], t1[:], ALU.add, [Y_t], [Y_t, t1_t])
                tt(Y[:], Y[:], gg[:], ALU.mult, [Y_t], [Y_t, gg_t])
                sc.dma("sp", mix_ap[r0:r0 + 128, 1280:2048], Y[:], accs=[mix_t], ins=[Y_t])
            allt = [mu_t, pv_t, w2f_t, w2_t, a2_t, g2_t, tri_t, ones_t, mU_t, mL_t, xr_t, pr_t, lr_t, lrT_t, lw_t, lwb_t, asg_t, gg_t,
                    kk_t, km_t, bb_t, cum_t, t1_t, t2_t, ex_t, s12_t, tb_t, hb_t, fm_t, gam_t, Gb_t, Gk_t, W0_t, U_t, Y_t, Sf_t, Sb_t]
            allt += [x[1] for x in Pm + Qm + Tm] + ps_t + psb_t
            self.phase_end(sc, allt)

    def precast_moe(self, sc, wgt, wut, wdt):
        wgv, wuv, wdv = self.wcast
        todo = []
        for (src, dst, rows) in ((wgt, wgv, NE * 11 * 128), (wut, wuv, NE * 11 * 128), (wdt, wdv, NE * 16 * 128)):
            step = 256
            for r0 in range(0, rows, step):
                todo.append(lambda src=src, dst=dst, r0=r0, step=step: sc.dma("pool", dst[r0:r0 + step, :], src[r0:r0 + step, :],
                                                                              accs=[self.wcast_t]))
        return todo

    def drain_precast(self, n=None):
        todo = getattr(self, "pending_precast", [])
        k = len(todo) if n is None else min(n, len(todo))
        for _ in range(k):
            todo.pop(0)()

    def phase_moe(self, sc, l, xs, xs_tt, g2, wgt, wut, wdt, router_ap, C, ident, ident_t, ident_b, identb_t,
                  h2s, slot_tab):
        nc = self.nc
        V = nc.vector
        A = nc.scalar
        Tn, NTn = self.T, self.NT
        NBLK = (2 * Tn) // 512 + NE
        NSLOT = NBLK * 512
        NF = D_FF // 128
        U32 = mybir.dt.uint32
        IOA = bass.IndirectOffsetOnAxis
        with ExitStack() as st:
            def T_(shape, dt, name):
                return self.sb(st, shape, dt, name), TT()
            comb, comb_t = T_([128, 1, NE], F32, "comb")
            rsm, rsm_t = T_([128, 8, NE], F32, "rsm")
            sm, sm_t = T_([128, 8, NE], F32, "sm")
            Mb, Mb_t = T_([128, NE], BF16, "Mb")
            base, base_t = T_([128, NE], F32, "base")
            arr, arr_t = T_([128, 12, NTn], F32, "arr")
            pu, pu_t = T_([128, 2, NTn], U32, "pu")
            rows, rows_t = T_([128, 2, NTn, 2], F32, "rows")
            cst, cst_t = T_([128, 8 + NBLK + 11 + 16 + NTn + 16], F32, "cst")
            BE, BE_t = T_([128, NBLK], F32, "BE")
            triU, tri_t = T_([128, 128], BF16, "triU")
            ones, ones_t = T_([128, 128], BF16, "ones")
            psT = [self.ps(st, [128, 4, 128], F32, "psT") for _ in range(2)]
            psT_t = [PT() for _ in range(2)]
            psTb = self.ps(st, [128, 8, 128], BF16, "psTb"); psTb_t = PT()
            psG = [self.ps(st, [128, 512], F32, "psG") for _ in range(2)]
            psG_t = [PT() for _ in range(2)]
            psU = [self.ps(st, [128, 512], F32, "psU") for _ in range(2)]
            psU_t = [PT() for _ in range(2)]
            psY = self.ps(st, [128, 512], F32, "psY"); psY_t = PT()
            sinit_t, slot_t, h2_t = TT(), TT(), TT()
            ys_t = [TT(), TT()]
            o_i8, o_jj, o_cA, o_cD, o_tk = 0, 8, 8 + NBLK, 8 + NBLK + 11, 8 + NBLK + 27
            o_c4 = o_tk + NTn
            iota8 = cst[:, o_i8:o_i8 + 8]
            jj = cst[:, o_jj:o_jj + NBLK]
            R1, R2, E1, E2, W1, W2, S1, S2, TMP, P1, P2 = (arr[:, i, :] for i in range(11))

            def dve(fn, outs, ins):
                sc.op("dve", fn, outs=outs, ins=ins)

            stA = ExitStack()
            st_save = st

            def TA(shape, dt, name):
                return self.sb(stA, shape, dt, name), TT()
            xt, xt_t = TA([128, D], F32, "xt")
            xn, xn_t = TA([128, D], F32, "xn")
            gb, gb_t = TA([128, D], F32, "gb")
            h2o = [TA([128, D], BF16, "h2o") for _ in range(1)]
            stat = [TA([128, 4], F32, "stat") for _ in range(2)]
            g_fm, g_t = TA([128, 16], F32, "gfm")
            hTa = [TA([128, 16, 128], BF16, "hTa") for _ in range(1)]
            hTf = [self.sb(stA, [128, 4, 128], F32, "hTf") for _ in range(2)]
            hTf_t = [TT() for _ in range(2)]
            self.hlo = [self.sb(stA, [128, 4, 128], BF16, "hlo") for _ in range(2)]
            self.hlo_t = [TT() for _ in range(2)]
            rtr_f, rtr_ft = TA([128, 16, NE], F32, "rtrf")
            self.rtr_b = self.sb(stA, [128, 16, NE], BF16, "rtrb"); self.rtr_bt = TT()
            self.rtr_lo = self.sb(stA, [128, 16, NE], BF16, "rtrlo")
            fmtmp_holder = stA
            sc.dma("sp", cst[:, 0:8], C["iota8"][:, :], outs=[cst_t])
            sc.dma("sp", cst[:, o_jj:o_jj + NBLK], C["jj"][:, 0:NBLK], accs=[cst_t])
            sc.dma("sp", cst[:, o_cA:o_cA + 27], C["constAD"][:, :], accs=[cst_t])
            sc.dma("sp", cst[:, o_tk:o_tk + NTn], C["tokid"][:, 0:NTn], accs=[cst_t])
            sc.dma("sp", cst[:, o_c4:o_c4 + 16], C["cb4"][:, :], accs=[cst_t])
            sc.dma("sp", triU[:], C["triU"][:, :], outs=[tri_t])
            sc.dma("sp", ones[:], C["ones_b"][:, :], outs=[ones_t])
            sc.dma("sp", slot_tab[0:NSLOT + 128, :], C["slot_init"][0:NSLOT + 128, :], outs=[sinit_t])
            self.bcast_load(sc, gb[:], gb_t, g2[l])
            self.load_fm_vec(sc, stA, g_fm, g_t, g2[l], 16, ident, ident_t, psT[0], psT_t[0])
            for c in range(16):
                sc.dma("sp", rtr_f[:, c, :], router_ap[c * 128:(c + 1) * 128, :], accs=[rtr_ft])
            dve(lambda: V.tensor_copy(out=self.rtr_b[:], in_=rtr_f[:]), [self.rtr_bt], [rtr_ft])
            dve(lambda: V.tensor_tensor(out=self.rtr_lo[:], in0=rtr_f[:], in1=self.rtr_b[:], op=ALU.subtract), [self.rtr_bt], [rtr_ft, self.rtr_bt])
            dve(lambda: V.memset(base[:], 0.0), [base_t], [])

            mask1, mask2 = rsm[:, 2, :], rsm[:, 4, :]
            w2c, w1c = rsm[:, 1, 3:4], rsm[:, 1, 4:5]
            for k in range(NTn):
                r0 = k * 128
                ha, ha_t = hTa[0]
                self.tile_to_hT(sc, xs[r0:r0 + 128, :], xs_tt[k], xt, xt_t, xn, xn_t, stat[k % 2][0], stat[k % 2][1],
                                g_fm, g_t, psT, psT_t, ha, ha_t, 0, ident, ident_t, k,
                                router=(rtr_f, rtr_ft, psY, psY_t, hTf, hTf_t))
                self.router_block(sc, comb, comb_t, rsm, rsm_t, psY, psY_t, 0)
                ho, ho_t = h2o[0]
                sc.op("pool", lambda ho=ho: nc.gpsimd.tensor_tensor(out=ho[:], in0=xn[:], in1=gb[:], op=ALU.mult),
                      outs=[ho_t], ins=[xn_t, gb_t])
                sc.dma("sp", h2s[r0:r0 + 128, :], ho[:], accs=[h2_t], ins=[ho_t])
                dve(lambda: V.tensor_tensor(out=Mb[:], in0=mask1, in1=mask2, op=ALU.add), [Mb_t], [rsm_t])
                sc.op("pe", lambda: nc.tensor.matmul(psY[:, 8:16], lhsT=triU[:], rhs=Mb[:], start=True, stop=True),
                      outs=[psY_t], ins=[tri_t, Mb_t])
                sc.op("pe", lambda: nc.tensor.matmul(psY[:, 16:24], lhsT=ones[:], rhs=Mb[:], start=True, stop=True),
                      outs=[psY_t], ins=[ones_t, Mb_t])
                rk = sm[:, 0, :]
                dve(lambda: V.scalar_tensor_tensor(out=rk, in0=psY[:, 8:16], scalar=-1.0, in1=base[:], op0=ALU.add, op1=ALU.add),
                    [sm_t], [psY_t, base_t])
                dve(lambda: V.tensor_tensor(out=base[:], in0=base[:], in1=psY[:, 16:24], op=ALU.add), [base_t], [base_t, psY_t])
                for (mk, src, dst) in ((mask1, rk, R1), (mask2, rk, R2), (mask1, iota8, E1), (mask2, iota8, E2)):
                    dve(lambda mk=mk, src=src: V.tensor_tensor(out=sm[:, 1, :], in0=mk, in1=src, op=ALU.mult), [sm_t], [rsm_t, sm_t, cst_t])
                    dve(lambda dst=dst, k=k: V.reduce_sum(out=dst[:, k:k + 1], in_=sm[:, 1, :], axis=AX.X), [arr_t], [sm_t])
                dve(lambda k=k: V.tensor_copy(out=W1[:, k:k + 1], in_=w1c), [arr_t], [rsm_t])
                dve(lambda k=k: V.tensor_copy(out=W2[:, k:k + 1], in_=w2c), [arr_t], [rsm_t])

            nb, st8 = sm[:, 2, :], sm[:, 3, :]
            dve(lambda: V.memset(sm[:, 2:4, :], 0.0), [sm_t], [])
            for j in range(Tn // 512):
                dve(lambda j=j: V.scalar_tensor_tensor(out=nb, in0=base[:], scalar=512.0 * j, in1=nb, op0=ALU.is_gt, op1=ALU.add),
                    [sm_t], [sm_t, base_t])
            for e in range(1, NE):
                dve(lambda e=e: V.tensor_tensor(out=st8[:, e:e + 1], in0=st8[:, e - 1:e], in1=nb[:, e - 1:e], op=ALU.add), [sm_t], [sm_t])
            dve(lambda: V.memset(BE[:], -1.0), [BE_t], [])
            dve(lambda: V.memset(arr[:, 6:8, :], 0.0), [arr_t], [])
            for e in range(NE):
                dve(lambda e=e: V.scalar_tensor_tensor(out=BE[:], in0=jj, scalar=st8[:, e:e + 1], in1=BE[:], op0=ALU.is_ge, op1=ALU.add),
                    [BE_t], [BE_t, sm_t, cst_t])
                for (Ex, Sx) in ((E1, S1), (E2, S2)):
                    dve(lambda Ex=Ex, e=e: V.tensor_scalar(out=TMP, in0=Ex, scalar1=float(e), scalar2=None, op0=ALU.is_equal), [arr_t], [arr_t])
                    dve(lambda Sx=Sx, e=e: V.scalar_tensor_tensor(out=Sx, in0=TMP, scalar=st8[:, e:e + 1], in1=Sx, op0=ALU.mult, op1=ALU.add),
                        [arr_t], [arr_t, sm_t])
            for (Sx, Rx, Px, i) in ((S1, R1, P1, 0), (S2, R2, P2, 1)):
                dve(lambda Sx=Sx, Rx=Rx, Px=Px: V.scalar_tensor_tensor(out=Px, in0=Sx, scalar=512.0, in1=Rx, op0=ALU.mult, op1=ALU.add),
                    [arr_t], [arr_t])
                dve(lambda Px=Px, i=i: V.tensor_copy(out=pu[:, i, :], in_=Px), [pu_t], [arr_t])
                dve(lambda i=i: V.tensor_copy(out=rows[:, i, :, 0], in_=cst[:, o_tk:o_tk + NTn]), [rows_t], [cst_t])
                dve(lambda i=i: V.tensor_copy(out=rows[:, i, :, 1], in_=arr[:, 4 + i, :]), [rows_t], [arr_t])
            for k in range(NTn):
                for i in range(2):
                    sc.idma(slot_tab[:, :], rows[:, i, k, :], accs=[slot_t], ins=[rows_t, pu_t, sinit_t],
                            out_offset=IOA(ap=pu[:, i, k:k + 1], axis=0), in_offset=None)

            if self.dbg2 is not None:
                dt_ = TT()
                sc.dma("sp", self.dbg2[:, 0:8], base[:], outs=[dt_], ins=[base_t])
                sc.dma("sp", self.dbg2[:, 8:8 + NBLK], BE[:], accs=[dt_], ins=[BE_t])
                sc.dma("sp", self.dbg2[:, 8 + NBLK:8 + NBLK + NTn], arr[:, 9, :], accs=[dt_], ins=[arr_t])
                sc.dma("sp", self.dbg2[:, 8 + NBLK + NTn:8 + NBLK + 2 * NTn], arr[:, 10, :], accs=[dt_], ins=[arr_t])
                sc.dma("sp", self.dbg3[:, :], slot_tab[0:NSLOT + 128, :], accs=[dt_], ins=[slot_t])
            self.phase_end(sc, [x[1] for x in h2o + stat + hTa] + hTf_t + self.hlo_t + [xt_t, xn_t, gb_t, g_t, rtr_ft, self.rtr_bt])
            stA.close()
            stB = ExitStack()

            def TB(shape, dt, name):
                return self.sb(stB, shape, dt, name), TT()
            slr2 = [TB([128, 4, 2], F32, "slr") for _ in range(2)]
            ids2 = [TB([128, 4], U32, "ids") for _ in range(2)]
            ids42 = [TB([128, 4, 4], U32, "ids4") for _ in range(2)]
            idxA2 = [TB([128, 11], U32, "idxA") for _ in range(2)]
            idxD2 = [TB([128, 16], U32, "idxD") for _ in range(2)]
            G = [TB([128, D], BF16, "G") for _ in range(2)]
            hT2 = [TB([128, 16, 512], BF16, "hT") for _ in range(2)]
            actT, act_t = TB([128, NF, 512], BF16, "actT")
            wp = [TB([128, 16, 512], BF16, "wp") for _ in range(3)]
            wp4, wp4_t = TB([128, 11, 512], BF16, "wp4")
            sg = [TB([128, 512], BF16, "sg") for _ in range(2)]
            ot = [TB([128, 512], F32, "ot") for _ in range(3)]
            for i in range(2):
                dve(lambda i=i: V.memset(G[i][0][:], 0.0), [G[i][1]], [])
            wgv, wuv, wdv = self.wcast
            wc_t = self.wcast_t
            xs_v = xs.rearrange("r (b f) -> (r b) f", f=512)
            gi = 0
            wi = 0
            oi = 0
            gq = [0]

            def prep_block(j):
                slr, slr_t = slr2[j % 2]
                ids, ids_t = ids2[j % 2]
                ids4, ids4_t = ids42[j % 2]
                idxA, idxA_t = idxA2[j % 2]
                idxD, idxD_t = idxD2[j % 2]
                hT, hT_t = hT2[j % 2]
                sc.dma("sp", slr[:], slot_tab[j * 512:(j + 1) * 512, :].rearrange("(t p) c -> p t c", p=128), outs=[slr_t], ins=[slot_t])
                dve(lambda: V.tensor_copy(out=ids[:], in_=slr[:, :, 0]), [ids_t], [slr_t])
                dve(lambda: V.scalar_tensor_tensor(out=ids4[:], in0=slr[:, :, 0:1].to_broadcast([128, 4, 4]), scalar=4.0,
                                                   in1=cst[:, o_c4:o_c4 + 16].rearrange("p (a b) -> p a b", b=4),
                                                   op0=ALU.mult, op1=ALU.add), [ids4_t], [slr_t, cst_t])
                dve(lambda: V.scalar_tensor_tensor(out=idxA[:], in0=BE[:, j:j + 1].to_broadcast([128, 11]), scalar=1408.0,
                                                   in1=cst[:, o_cA:o_cA + 11], op0=ALU.mult, op1=ALU.add), [idxA_t], [BE_t, cst_t])
                dve(lambda: V.scalar_tensor_tensor(out=idxD[:], in0=BE[:, j:j + 1].to_broadcast([128, 16]), scalar=2048.0,
                                                   in1=cst[:, o_cD:o_cD + 16], op0=ALU.mult, op1=ALU.add), [idxD_t], [BE_t, cst_t])
                for t in range(4):
                    g_, g_t2 = G[gq[0] % 2]
                    gq[0] += 1
                    sc.idma(g_[:], h2s[:, :], outs=[g_t2], ins=[ids_t, h2_t], out_offset=None, in_offset=IOA(ap=ids[:, t:t + 1], axis=0))
                    for half in range(2):
                        for c8 in range(8):
                            c = half * 8 + c8
                            sc.op("pe", lambda c=c, c8=c8, g_=g_: nc.tensor.transpose(psTb[:, c8, :], g_[:, c * 128:(c + 1) * 128], ident_b[:]),
                                  outs=[psTb_t], ins=[g_t2, identb_t], mark=(c8 == 7))
                        if half == 0:
                            dve(lambda t=t: V.tensor_copy(out=hT[:, 0:8, t * 128:(t + 1) * 128], in_=psTb[:]), [hT_t], [psTb_t])
                        else:
                            sc.op("act", lambda t=t: A.copy(out=hT[:, 8:16, t * 128:(t + 1) * 128], in_=psTb[:]), outs=[hT_t], ins=[psTb_t])

            prep_block(0)
            for j in range(NBLK):
                slr, slr_t = slr2[j % 2]
                ids4, ids4_t = ids42[j % 2]
                idxA, idxA_t = idxA2[j % 2]
                idxD, idxD_t = idxD2[j % 2]
                hT, hT_t = hT2[j % 2]
                for fb in range(11):
                    (wgb, wgb_t) = wp[wi % 3]; wi += 1
                    sc.idma(wgb[:].rearrange("p c f -> p (c f)"), wgv[:, :], outs=[wgb_t], ins=[idxA_t, wc_t], out_offset=None,
                            in_offset=IOA(ap=idxA[:, fb:fb + 1], axis=0))
                    (wub, wub_t) = wp[wi % 3]; wi += 1
                    sc.idma(wub[:].rearrange("p c f -> p (c f)"), wuv[:, :], outs=[wub_t], ins=[idxA_t, wc_t], out_offset=None,
                            in_offset=IOA(ap=idxA[:, fb:fb + 1], axis=0))
                    for f4 in range(4):
                        fc = fb * 4 + f4
                        pg, pg_t = psG[gi % 2], psG_t[gi % 2]
                        pu_, pu_t2 = psU[gi % 2], psU_t[gi % 2]
                        s_, s_t = sg[gi % 2]
                        gi += 1
                        for c in range(16):
                            sc.op("pe", lambda c=c, pg=pg, wgb=wgb, f4=f4: nc.tensor.matmul(
                                pg[:], lhsT=wgb[:, c, f4 * 128:(f4 + 1) * 128], rhs=hT[:, c, :], start=(c == 0), stop=(c == 15)),
                                outs=[pg_t], ins=[wgb_t, hT_t], mark=(c == 15))
                        for c in range(16):
                            sc.op("pe", lambda c=c, pu_=pu_, wub=wub, f4=f4: nc.tensor.matmul(
                                pu_[:], lhsT=wub[:, c, f4 * 128:(f4 + 1) * 128], rhs=hT[:, c, :], start=(c == 0), stop=(c == 15)),
                                outs=[pu_t2], ins=[wub_t, hT_t], mark=(c == 15))
                        sc.op("act", lambda pg=pg, s_=s_: A.activation(out=s_[:], in_=pg[:], func=AF.Silu), outs=[s_t], ins=[pg_t])
                        dve(lambda pu_=pu_, s_=s_, fc=fc: V.tensor_tensor(out=actT[:, fc, :], in0=pu_[:], in1=s_[:], op=ALU.mult),
                            [act_t], [pu_t2, s_t])
                if j + 1 < NBLK:
                    prep_block(j + 1)
                for cb in range(4):
                    pieces = []
                    for pc in range(4):
                        if pc < 3:
                            wdb, wdb_t = wp[pc]
                        else:
                            wdb, wdb_t = wp4, wp4_t
                        sc.idma(wdb[:, 0:11, :].rearrange("p c f -> p (c f)"), wdv[:, :], outs=[wdb_t], ins=[idxD_t, wc_t], out_offset=None,
                                in_offset=IOA(ap=idxD[:, cb * 4 + pc:cb * 4 + pc + 1], axis=0))
                        pieces.append((wdb, wdb_t))
                    for t in range(4):
                        for pc, (wdb, wdb_t) in enumerate(pieces):
                            for c in range(11):
                                fc = pc * 11 + c
                                sc.op("pe", lambda c=c, fc=fc, wdb=wdb, t=t: nc.tensor.matmul(
                                    psY[:], lhsT=actT[:, fc, t * 128:(t + 1) * 128], rhs=wdb[:, c, :], start=(fc == 0), stop=(fc == NF - 1)),
                                    outs=[psY_t], ins=[act_t, wdb_t], mark=(fc == NF - 1))
                        o_, o_t = ot[oi % 3]
                        oi += 1
                        dve(lambda t=t, o_=o_: V.tensor_scalar(out=o_[:], in0=psY[:], scalar1=slr[:, t, 1:2],
                                                              scalar2=None, op0=ALU.mult), [o_t], [psY_t, slr_t])
                        sc.idma(xs_v, o_[:], accs=[ys_t[j % 2]], ins=[o_t, ids4_t, ys_t[(j + 1) % 2]],
                                out_offset=IOA(ap=ids4[:, t, cb:cb + 1], axis=0), in_offset=None, compute_op=ALU.add)
                wi = 0
            allt = [x[1] for x in slr2 + ids2 + ids42 + idxA2 + idxD2 + G + hT2 + wp + sg + ot] + [act_t, wp4_t]
            allt += psT_t + psG_t + psU_t + ys_t
            allt += [comb_t, rsm_t, sm_t, Mb_t, base_t, arr_t, pu_t, rows_t, cst_t, BE_t, tri_t, ones_t, psTb_t, psY_t, sinit_t, slot_t, h2_t]
            self.phase_end(sc, allt)
            stB.close()

    def router_block(self, sc, comb, comb_t, rsm, rsm_t, pl, pl_t, j):
        nc = self.nc
        if True:
            lg, mask1, l2, mask2 = rsm[:, 0, :], rsm[:, 2, :], rsm[:, 3, :], rsm[:, 4, :]
            m1, m2, dd, w2, w1 = (rsm[:, 1, i:i + 1] for i in range(5))
            V = nc.vector
            sc.op("dve", lambda: V.tensor_copy(out=lg, in_=pl[:, 0:NE]), outs=[rsm_t], ins=[pl_t])
            sc.op("dve", lambda: V.reduce_max(out=m1, in_=lg, axis=AX.X), outs=[rsm_t], ins=[rsm_t])
            sc.op("dve", lambda: V.tensor_scalar(out=mask1, in0=lg, scalar1=m1, scalar2=None, op0=ALU.is_equal),
                  outs=[rsm_t], ins=[rsm_t])
            sc.op("dve", lambda: V.scalar_tensor_tensor(out=l2, in0=mask1, scalar=-1e30, in1=lg, op0=ALU.mult, op1=ALU.add),
                  outs=[rsm_t], ins=[rsm_t])
            sc.op("dve", lambda: V.reduce_max(out=m2, in_=l2, axis=AX.X), outs=[rsm_t], ins=[rsm_t])
            sc.op("dve", lambda: V.tensor_scalar(out=mask2, in0=l2, scalar1=m2, scalar2=None, op0=ALU.is_equal),
                  outs=[rsm_t], ins=[rsm_t])
            sc.op("dve", lambda: V.tensor_tensor(out=dd, in0=m2, in1=m1, op=ALU.subtract), outs=[rsm_t], ins=[rsm_t])
            sc.op("act", lambda: nc.scalar.activation(out=w2, in_=dd, func=AF.Sigmoid), outs=[rsm_t], ins=[rsm_t])
            sc.op("dve", lambda: V.tensor_scalar(out=w1, in0=w2, scalar1=-1.0, scalar2=1.0, op0=ALU.mult, op1=ALU.add),
                  outs=[rsm_t], ins=[rsm_t])
            sc.op("dve", lambda j=j: V.tensor_scalar(out=comb[:, j, :], in0=mask1, scalar1=w1, scalar2=None, op0=ALU.mult),
                  outs=[comb_t], ins=[rsm_t])
            sc.op("dve", lambda j=j: V.scalar_tensor_tensor(out=comb[:, j, :], in0=mask2, scalar=w2, in1=comb[:, j, :],
                                                            op0=ALU.mult, op1=ALU.add), outs=[comb_t], ins=[rsm_t, comb_t])

    def build(self):
        cfg = self.cfg
        nc = self.nc
        T_ = self.T
        layers = cfg.get("layers", list(range(DEPTH)))
        phases = cfg.get("phases", None)
        has = lambda ph: phases is None or ph in phases
        I = {}
        I["x"] = self.din("x", [T_, D])
        for nm, shp in self.input_shapes(cfg).items():
            I[nm] = self.din(nm, shp, BF16 if nm in BF16_INPUTS else F32)
        y = self.dout("y", [T_, D])
        p_ap = self.dscr("p_scr", [T_, D_IN])
        mix_ap = self.dscr("mix_scr", [T_, D])
        qkT = self.dscr("qkT_scr", [20, 128, T_], BF16)
        xs = self.dscr("xs_scr", [T_ + 128, D])
        h2s = self.dscr("h2_scr", [T_ + 128, D], BF16)
        slot_tab = self.dscr("slot_scr", [NBLK_MAX * 512 + 128, 2])
        self.wcast = (self.dscr("wgb_scr", [NE * 11 * 128, 8192], BF16), self.dscr("wub_scr", [NE * 11 * 128, 8192], BF16),
                      self.dscr("wdb_scr", [NE * 16 * 128, 5632], BF16))
        self.wcast_t = TT()
        dbg = None
        if cfg.get("dbg"):
            dbg = self.dout("dbg", cfg["dbg_shape"])
        self.dbg2 = self.dout("dbg2", cfg["dbg2_shape"]) if cfg.get("dbg2_shape") else None
        self.dbg3 = self.dout("dbg3", cfg["dbg3_shape"]) if cfg.get("dbg3_shape") else None
        self.I = I
        with ExitStack() as st:
            sc = Sched(nc, st)
            self.sc = sc
            ident = self.sb(st, [128, 128], F32, "identf"); ident_t = TT()
            sc.dma("sp", ident[:], I["ident_f"][:, :], outs=[ident_t])
            p_t, mix_t, qkT_t = TT(), TT(), TT()
            ident_b = self.sb(st, [128, 128], BF16, "identb"); identb_t = TT()
            sc.dma("sp", ident_b[:], I["ident_b"][:, :], outs=[identb_t])
            C = I
            y_tt = [TT() for _ in range(self.NT)]
            for i in range(self.NT):
                sc.dma("sp", xs[i * 128:(i + 1) * 128, :], I["x"][i * 128:(i + 1) * 128, :], outs=[y_tt[i]])
            zt = self.sb(st, [128, D], BF16, "zt"); zt_t = TT()
            sc.op("dve", lambda: nc.vector.memset(zt[:], 0.0), outs=[zt_t])
            sc.dma("sp", h2s[T_:T_ + 128, :], zt[:], ins=[zt_t])
            y_out = y
            y = xs
            for l in layers:
                if has("inproj"):
                    self.phase_inproj(sc, l, y, y_tt, I["w_in"], I["norm1_g"], p_ap, p_t, ident, ident_t)
                if has("attn_prep"):
                    self.phase_attn_prep(sc, l, p_ap, p_t, qkT, qkT_t, C, ident_b, identb_t)
                if has("attn"):
                    self.phase_attn(sc, l, p_ap, p_t, qkT, qkT_t, mix_ap, mix_t, C)
                if has("rwkv"):
                    if l % 2 == 1 and has("ffn") and cfg.get("sparse_moe", True):
                        i_ = l // 2
                        self.pending_precast = self.precast_moe(sc, I["moe_wg_t"][i_], I["moe_wu_t"][i_], I["moe_wd_t"][i_])
                        self.precast_layer = l
                    self.phase_rwkv(sc, l, p_ap, p_t, mix_ap, mix_t, C, ident, ident_t, ident_b, identb_t)
                if has("outproj"):
                    src = mix_ap if not cfg.get("outproj_from_x") else I["x"]
                    self.phase_outproj(sc, l, src, mix_t, I["w_out"], y, y_tt, ident, ident_t)
                if has("ffn"):
                    i = l // 2
                    if l % 2 == 0:
                        ex = [(I["ffn_w_gate"][i], I["ffn_w_up"][i], I["ffn_w_down"][i])]
                        self.phase_ffn(sc, l, y, y_tt, I["norm2_g"], ex, None, ident, ident_t)
                    else:
                        ne = cfg.get("n_experts", NE)
                        if cfg.get("sparse_moe", True):
                            if not getattr(self, "precast_layer", None) == l:
                                self.pending_precast = self.precast_moe(sc, I["moe_wg_t"][i], I["moe_wu_t"][i], I["moe_wd_t"][i])
                            self.drain_precast()
                            self.phase_moe(sc, l, y, y_tt, I["norm2_g"], I["moe_wg_t"][i], I["moe_wu_t"][i], I["moe_wd_t"][i],
                                           I["moe_router"][i], C, ident, ident_t, ident_b, identb_t, h2s, slot_tab)
                        else:
                            ex = [(I["moe_w_gate"][i][e], I["moe_w_up"][i][e], I["moe_w_down"][i][e]) for e in range(ne)]
                            self.phase_ffn(sc, l, y, y_tt, I["norm2_g"], ex, I["moe_router"][i], ident, ident_t)
            if dbg is not None:
                self.emit_dbg(sc, dbg, locals())
            for i in range(self.NT):
                sc.dma("sp", y_out[i * 128:(i + 1) * 128, :], xs[i * 128:(i + 1) * 128, :], outs=[TT()], ins=[y_tt[i]])
            e_sp = sc.E["sp"]
            n_ = len(e_sp["dma_sems"])
            for slot in range(n_):
                cnt = (e_sp["dma_i"] - 1 - slot) // n_ + 1 if e_sp["dma_i"] > slot else 0
                if cnt > 0:
                    sc._wait(e_sp, ("Dsp%d" % slot, e_sp["dma_sems"][slot], 16 * cnt, "dma"))
        return nc

    @staticmethod
    def input_shapes(cfg):
        nl = cfg.get("n_layer_weights", DEPTH)
        nd = cfg.get("n_dense", 2)
        nm = cfg.get("n_moe", 2)
        nei = cfg.get("n_experts", NE)
        shp = {
            "norm1_g": [DEPTH, D], "w_in": [nl, D, D_IN], "w_out": [nl, D, D], "norm2_g": [DEPTH, D],
            "ffn_w_gate": [nd, D, D_FF], "ffn_w_up": [nd, D, D_FF], "ffn_w_down": [nd, D_FF, D],
            "moe_router": [2, D, NE], "moe_w_gate": [nm, nei, D, D_FF], "moe_w_up": [nm, nei, D, D_FF],
            "moe_w_down": [nm, nei, D_FF, D], "ident_f": [128, 128],
            "da_q_norm": [DEPTH, 64], "da_k_norm": [DEPTH, 64], "da_lambda": [DEPTH, 4, 64], "da_out_norm": [DEPTH, 128],
            "dl_q_norm": [DEPTH, 128], "dl_k_norm": [DEPTH, 128],
            "ident_b": [128, 128], "rope": [S, 192], "maskA": [128, 7, 128], "maskB": [128, 19, 128],
            "triU": [128, 128], "ones_b": [128, 128], "maskU2": [128, 256], "maskL": [128, 128],
            "rw_mu": [DEPTH, RW_IN], "rw_w0": [DEPTH, 768], "rw_w2": [DEPTH, 64, 768], "rw_a0": [DEPTH, 768],
            "rw_a2": [DEPTH, 64, 768], "rw_g2": [DEPTH, 128, 768], "rw_k_k": [DEPTH, 768], "rw_k_a": [DEPTH, 768],
            "rw_r_k": [DEPTH, 12, 64], "rw_ln_g": [DEPTH, 768], "rw_ln_b": [DEPTH, 768],
            "iota8": [128, 8], "jj": [128, NBLK_MAX], "constAD": [128, 27], "tokid": [128, NT],
            "slot_init": [NBLK_MAX * 512 + 128, 2], "cb4": [128, 16],
            "moe_wg_t": [nm, NE * 11 * 128, 16 * 512], "moe_wu_t": [nm, NE * 11 * 128, 16 * 512],
            "moe_wd_t": [nm, NE * 16 * 128, 11 * 512],
        }
        if cfg.get("sparse_moe", True):
            for nm_ in ("moe_w_gate", "moe_w_up", "moe_w_down"):
                shp[nm_] = [1, 2, 2]
        else:
            for nm_ in ("moe_wg_t", "moe_wu_t", "moe_wd_t"):
                shp[nm_] = [1, 2, 2]
        if True:
            pass
        for nm_ in cfg.get("unused", ()):
            shp[nm_] = [1, 2, 2]
        return shp

    def emit_dbg(self, sc, dbg, env):
        kind = self.cfg["dbg"]
        d_t = TT()
        rows = self.cfg["dbg_rows"]
        if kind == "p":
            for i, r in enumerate(rows):
                sc.dma("sp", dbg[i * 128:(i + 1) * 128, :], env["p_ap"][r * 128:(r + 1) * 128, :], outs=[d_t], ins=[env["p_t"]])
        if kind == "mix":
            for i, r in enumerate(rows):
                sc.dma("sp", dbg[i * 128:(i + 1) * 128, :], env["mix_ap"][r * 128:(r + 1) * 128, :], outs=[d_t], ins=[env["mix_t"]])
        if kind == "y":
            for i, r in enumerate(rows):
                sc.dma("sp", dbg[i * 128:(i + 1) * 128, :], env["y"][r * 128:(r + 1) * 128, :], outs=[d_t], ins=[env["y_tt"][r]])
        sc.wait_all("sp", [d_t])


_WEIGHT_NAMES = ("norm1_g", "w_in", "da_q_norm", "da_k_norm", "da_lambda", "da_out_norm", "dl_q_norm", "dl_k_norm",
                 "rw_mu", "rw_w0", "rw_w2", "rw_a0", "rw_a2", "rw_g2", "rw_k_k", "rw_k_a", "rw_r_k", "rw_ln_g", "rw_ln_b",
                 "w_out", "norm2_g", "ffn_w_gate", "ffn_w_up", "ffn_w_down", "moe_router", "moe_w_gate", "moe_w_up",
                 "moe_w_down")


def retile_moe(wg, wu, wd):
    n, E = wg.shape[0], wg.shape[1]
    def gu(w):
        return np.ascontiguousarray(w.reshape(n, E, 16, 128, 11, 512).transpose(0, 1, 4, 3, 2, 5)).reshape(n, E * 11 * 128, 16 * 512)
    wdt = np.ascontiguousarray(wd.reshape(n, E, 4, 11, 128, 4, 512).transpose(0, 1, 5, 2, 4, 3, 6)).reshape(n, E * 16 * 128, 11 * 512)
    return gu(wg), gu(wu), wdt


def kernel(**inputs):
    x = np.ascontiguousarray(np.asarray(inputs["x"], dtype=np.float32))
    bsz = x.shape[0]
    assert bsz == NB * NCORES and x.shape[1] == S and x.shape[2] == D
    cfg = {}
    nc = Builder(cfg).build()
    shared = dict(host_constants())
    dummy = np.zeros((1, 2, 2), np.float32)
    for nm in _WEIGHT_NAMES:
        if nm in ("moe_w_gate", "moe_w_up", "moe_w_down"):
            shared[nm] = dummy
        else:
            shared[nm] = np.ascontiguousarray(np.asarray(inputs[nm], dtype=np.float32))
    wgt, wut, wdt = retile_moe(np.asarray(inputs["moe_w_gate"], dtype=np.float32), np.asarray(inputs["moe_w_up"], dtype=np.float32),
                               np.asarray(inputs["moe_w_down"], dtype=np.float32))
    shared["moe_wg_t"], shared["moe_wu_t"], shared["moe_wd_t"] = wgt, wut, wdt
    in_maps = []
    for c in range(NCORES):
        m = dict(shared)
        m["x"] = x[c * NB:(c + 1) * NB].reshape(T, D)
        in_maps.append(m)
    res = run_bass_kernel_spmd(nc, in_maps, core_ids=list(range(NCORES)))
    out = np.stack([np.asarray(r["y"]).reshape(NB, S, D) for r in res.results], axis=0)
    return out.reshape(bsz, S, D).astype(np.float32)
```
